# Optimizing a Trainium2 kernel written in Bass

```python
import math
import jax, jax.numpy as jnp
from jax import lax
import numpy as np

D_MODEL = 1024
BATCH = 8
SEQ = 2048
DEPTH = 4

CHUNK = 64
N_MEM = 256
N_MIXERS = 3
MIXER_W = D_MODEL
GDN_DK = 128
GDN_DV = 128
GDN_HEADS = MIXER_W // GDN_DV
GDN_KW = GDN_HEADS * GDN_DK
GDN_VW = GDN_HEADS * GDN_DV
GDN_QKV = 2 * GDN_KW + GDN_VW
CONV_K = 4
ML_DQK = 64
ML_DV = 128
ML_HEADS = MIXER_W // ML_DV
ML_KW = ML_HEADS * ML_DQK
ML_VW = ML_HEADS * ML_DV
SB_DH = 64
SB_HEADS = MIXER_W // SB_DH
SB_W = SB_HEADS * SB_DH
SB_BLOCK = 128
MEM_HEADS = 4
MEM_DH = 128
MEM_W = MEM_HEADS * MEM_DH
MIX_W = MIXER_W + MEM_W
GDN_IN = GDN_QKV + GDN_VW + 2 * GDN_HEADS + MEM_W
ML_IN = 2 * ML_KW + 2 * ML_VW + 2 * ML_HEADS + MEM_W
SB_IN = 3 * SB_W + MEM_W
D_FF = -(-8 * D_MODEL // (3 * 256)) * 256
N_GDN = (DEPTH + 2) // N_MIXERS
N_ML = (DEPTH + 1) // N_MIXERS
N_SB = DEPTH // N_MIXERS
EPS = 1e-6

kernel_name = "hybrid_gdn_mlstm_stickbreak_mem_trunk"


def rms_norm(x, g):
    xf = x.astype(jnp.float32)
    y = xf * lax.rsqrt(jnp.mean(xf * xf, axis=-1, keepdims=True) + EPS)
    return (y * g.astype(jnp.float32)).astype(x.dtype)


def l2norm(x):
    return x * lax.rsqrt(jnp.sum(x * x, axis=-1, keepdims=True) + EPS)


def split_heads(t, n):
    b, s, _ = t.shape
    return t.reshape(b, s, n, -1).transpose(0, 2, 1, 3)


def merge_heads(t):
    b, h, s, d = t.shape
    return t.transpose(0, 2, 1, 3).reshape(b, s, h * d)


def to_chunks(t):
    b, h, s = t.shape[:3]
    return t.reshape(b, h, s // CHUNK, CHUNK, *t.shape[3:])


def causal_dwconv(x, w):
    k, c = w.shape
    return lax.conv_general_dilated(x, w[:, None, :].astype(x.dtype), window_strides=(1,),
                                    padding=[(k - 1, 0)], dimension_numbers=("NWC", "WIO", "NWC"),
                                    feature_group_count=c)


def gated_delta_rule(q, k, v, a, b, a_log, dt_bias):
    f32 = jnp.float32
    q, k, v, a, b = (t.astype(f32) for t in (q, k, v, a, b))
    q = l2norm(q) * GDN_DK ** -0.5
    k = l2norm(k)
    beta = jax.nn.sigmoid(b)
    g = -jnp.exp(a_log.astype(f32))[None, :, None] * jax.nn.softplus(a + dt_bias.astype(f32)[None, :, None])
    qc, kc, vc, bc, gc = (to_chunks(t) for t in (q, k, v, beta, g))
    G = jnp.cumsum(gc, axis=-1)
    incl = jnp.tril(jnp.ones((CHUNK, CHUNK), dtype=bool))
    strict = jnp.tril(jnp.ones((CHUNK, CHUNK), dtype=bool), -1)
    diff = G[..., :, None] - G[..., None, :]
    decay = jnp.where(incl, jnp.exp(jnp.where(incl, diff, 0.0)), 0.0)
    kk = jnp.einsum("bhnid,bhnjd->bhnij", kc, kc)
    lower = jnp.where(strict, bc[..., :, None] * kk * decay, 0.0)
    rhs = jnp.concatenate([vc * bc[..., None], kc * (bc * jnp.exp(G))[..., None]], axis=-1)
    sol = lax.linalg.triangular_solve(lower + jnp.eye(CHUNK, dtype=f32), rhs, left_side=True,
                                      lower=True, unit_diagonal=True)
    u, w = sol[..., :GDN_DV], sol[..., GDN_DV:]
    p = jnp.einsum("bhnid,bhnjd->bhnij", qc, kc) * decay
    qg = qc * jnp.exp(G)[..., None]
    kg = kc * jnp.exp(G[..., -1:] - G)[..., None]
    gl = jnp.exp(G[..., -1])

    def step(state, xs):
        u_n, w_n, qg_n, kg_n, p_n, gl_n = xs
        v_new = u_n - jnp.einsum("bhcd,bhde->bhce", w_n, state)
        o_n = jnp.einsum("bhcd,bhde->bhce", qg_n, state) + jnp.einsum("bhij,bhje->bhie", p_n, v_new)
        state = state * gl_n[..., None, None] + jnp.einsum("bhcd,bhce->bhde", kg_n, v_new)
        return state, o_n

    bn, hn = q.shape[:2]
    s0 = jnp.zeros((bn, hn, GDN_DK, GDN_DV), f32)
    xs = tuple(jnp.moveaxis(t, 2, 0) for t in (u, w, qg, kg, p, gl))
    _, o = lax.scan(step, s0, xs)
    return jnp.moveaxis(o, 0, 2).reshape(bn, hn, -1, GDN_DV)


def mlstm_cell(q, k, v, i_pre, f_pre):
    f32 = jnp.float32
    q, k, v, i_pre, f_pre = (t.astype(f32) for t in (q, k, v, i_pre, f_pre))
    k = k * ML_DQK ** -0.5
    lf = jax.nn.log_sigmoid(f_pre)
    qc, kc, vc, lic, lfc = (to_chunks(t) for t in (q, k, v, i_pre, lf))
    bcum = jnp.cumsum(lfc, axis=-1)
    incl = jnp.tril(jnp.ones((CHUNK, CHUNK), dtype=bool))
    dmat = jnp.where(incl, bcum[..., :, None] - bcum[..., None, :] + lic[..., None, :], -jnp.inf)
    m_intra = jnp.max(dmat, axis=-1)
    sqk = jnp.einsum("bhnid,bhnjd->bhnij", qc, kc) * jnp.exp(dmat - m_intra[..., None])
    num_intra = jnp.einsum("bhnij,bhnje->bhnie", sqk, vc)
    den_intra = jnp.sum(sqk, axis=-1)
    bl = bcum[..., -1]
    wk = bl[..., None] - bcum + lic
    m_chunk = jnp.max(wk, axis=-1)
    e = jnp.exp(wk - m_chunk[..., None])
    kv_c = jnp.einsum("bhnc,bhncd,bhnce->bhnde", e, kc, vc)
    ks_c = jnp.einsum("bhnc,bhncd->bhnd", e, kc)

    def step(carry, xs):
        c_s, n_s, m_s = carry
        kv_n, ks_n, bl_n, mc_n = xs
        m_new = jnp.maximum(bl_n + m_s, mc_n)
        a = jnp.exp(bl_n + m_s - m_new)
        c = jnp.exp(mc_n - m_new)
        c_new = a[..., None, None] * c_s + c[..., None, None] * kv_n
        n_new = a[..., None] * n_s + c[..., None] * ks_n
        return (c_new, n_new, m_new), (c_s, n_s, m_s)

    bn, hn = q.shape[:2]
    init = (jnp.zeros((bn, hn, ML_DQK, ML_DV), f32), jnp.zeros((bn, hn, ML_DQK), f32),
            jnp.zeros((bn, hn), f32))
    xs = tuple(jnp.moveaxis(t, 2, 0) for t in (kv_c, ks_c, bl, m_chunk))
    _, (c_prev, n_prev, m_prev) = lax.scan(step, init, xs)
    c_prev = jnp.moveaxis(c_prev, 0, 2)
    n_prev = jnp.moveaxis(n_prev, 0, 2)
    m_prev = jnp.moveaxis(m_prev, 0, 2)
    a_inter = bcum + m_prev[..., None]
    m_t = jnp.maximum(a_inter, m_intra)
    s_inter = jnp.exp(a_inter - m_t)
    s_intra = jnp.exp(m_intra - m_t)
    num = s_inter[..., None] * jnp.einsum("bhncd,bhnde->bhnce", qc, c_prev) + s_intra[..., None] * num_intra
    den = s_inter * jnp.einsum("bhncd,bhnd->bhnc", qc, n_prev) + s_intra * den_intra
    h = num / jnp.maximum(jnp.abs(den), jnp.exp(-m_t))[..., None]
    return h.reshape(bn, hn, -1, ML_DV)


def stick_breaking(q, k, v):
    seq = q.shape[2]
    scale = SB_DH ** -0.5
    outs = []
    for blk in range(seq // SB_BLOCK):
        q0 = blk * SB_BLOCK
        kend = q0 + SB_BLOCK
        z = jnp.einsum("bhtd,bhsd->bhts", q[:, :, q0:kend], k[:, :, :kend]).astype(jnp.float32) * scale
        t_idx = q0 + jnp.arange(SB_BLOCK)[:, None]
        s_idx = jnp.arange(kend)[None, :]
        causal = s_idx < t_idx
        log_1mb = jnp.where(causal, jax.nn.log_sigmoid(-z), 0.0)
        tail = lax.cumsum(log_1mb, axis=3, reverse=True) - log_1mb
        att = jnp.where(causal, jnp.exp(jax.nn.log_sigmoid(z) + tail), 0.0)
        outs.append(jnp.einsum("bhts,bhsd->bhtd", att.astype(v.dtype), v[:, :, :kend]))
    return jnp.concatenate(outs, axis=2)


def memory_attention(q_mem, mem_k, mem_v):
    q = split_heads(q_mem, MEM_HEADS)
    k = split_heads(mem_k, MEM_HEADS)
    v = split_heads(mem_v, MEM_HEADS)
    s = jnp.einsum("bhtd,bhmd->bhtm", q, k).astype(jnp.float32) * MEM_DH ** -0.5
    p = jax.nn.softmax(s, axis=-1).astype(v.dtype)
    return merge_heads(jnp.einsum("bhtm,bhmd->bhtd", p, v))


def gdn_branch(proj, conv_w, a_log, dt_bias, out_norm):
    qkv, z, a, b, q_mem = jnp.split(proj, [GDN_QKV, GDN_QKV + GDN_VW, GDN_QKV + GDN_VW + GDN_HEADS,
                                           GDN_QKV + GDN_VW + 2 * GDN_HEADS], axis=-1)
    qkv = jax.nn.silu(causal_dwconv(qkv, conv_w))
    q, k, v = jnp.split(qkv, [GDN_KW, 2 * GDN_KW], axis=-1)
    o = gated_delta_rule(split_heads(q, GDN_HEADS), split_heads(k, GDN_HEADS), split_heads(v, GDN_HEADS),
                         a.transpose(0, 2, 1), b.transpose(0, 2, 1), a_log, dt_bias)
    o = rms_norm(o, out_norm) * jax.nn.silu(split_heads(z, GDN_HEADS).astype(jnp.float32))
    return merge_heads(o).astype(proj.dtype), q_mem


def mlstm_branch(proj, i_bias, f_bias, out_norm):
    q, k, v, o, i_pre, f_pre, q_mem = jnp.split(
        proj, [ML_KW, 2 * ML_KW, 2 * ML_KW + ML_VW, 2 * ML_KW + 2 * ML_VW,
               2 * ML_KW + 2 * ML_VW + ML_HEADS, 2 * ML_KW + 2 * ML_VW + 2 * ML_HEADS], axis=-1)
    i_pre = (i_pre + i_bias).transpose(0, 2, 1)
    f_pre = (f_pre + f_bias).transpose(0, 2, 1)
    h = mlstm_cell(split_heads(q, ML_HEADS), split_heads(k, ML_HEADS), split_heads(v, ML_HEADS), i_pre, f_pre)
    h = rms_norm(h, out_norm) * jax.nn.sigmoid(split_heads(o, ML_HEADS).astype(jnp.float32))
    return merge_heads(h).astype(proj.dtype), q_mem


def sb_branch(proj):
    q, k, v, q_mem = jnp.split(proj, [SB_W, 2 * SB_W, 3 * SB_W], axis=-1)
    o = stick_breaking(split_heads(q, SB_HEADS), split_heads(k, SB_HEADS), split_heads(v, SB_HEADS))
    return merge_heads(o), q_mem


def swiglu(x, w_in, w_out):
    g, u = jnp.split(x @ w_in, 2, axis=-1)
    return (jax.nn.silu(g) * u) @ w_out


def _w(key, shape, fan_in):
    return jax.random.normal(key, shape, jnp.float32) * fan_in ** -0.5


def _gain(key, shape):
    return 1.0 + 0.02 * jax.random.normal(key, shape, jnp.float32)


def setup_inputs(seed: int = 0) -> dict:
    key = jax.random.key(seed)
    ks = jax.random.split(key, 24)
    dt = jnp.exp(jax.random.uniform(ks[14], (N_GDN, GDN_HEADS), jnp.float32,
                                    minval=math.log(1e-3), maxval=math.log(1e-1)))
    return {
        "x": jax.random.normal(ks[0], (BATCH, SEQ, D_MODEL), jnp.float32),
        "mem": jax.random.normal(ks[1], (BATCH, N_MEM, D_MODEL), jnp.float32),
        "mem_norm": _gain(ks[2], (D_MODEL,)),
        "norm_pre_mix": _gain(ks[3], (DEPTH, D_MODEL)),
        "norm_post_mix": _gain(ks[4], (DEPTH, D_MODEL)),
        "norm_pre_ffn": _gain(ks[5], (DEPTH, D_MODEL)),
        "norm_post_ffn": _gain(ks[6], (DEPTH, D_MODEL)),
        "w_mem_kv": _w(ks[7], (DEPTH, D_MODEL, 2 * MEM_W), D_MODEL),
        "w_out": _w(ks[8], (DEPTH, MIX_W, D_MODEL), MIX_W),
        "w_ffn_in": _w(ks[9], (DEPTH, D_MODEL, 2 * D_FF), D_MODEL),
        "w_ffn_out": _w(ks[10], (DEPTH, D_FF, D_MODEL), D_FF),
        "gdn_w_in": _w(ks[11], (N_GDN, D_MODEL, GDN_IN), D_MODEL),
        "gdn_conv": _w(ks[12], (N_GDN, CONV_K, GDN_QKV), CONV_K),
        "gdn_a_log": jnp.log(jax.random.uniform(ks[13], (N_GDN, GDN_HEADS), jnp.float32, minval=1.0, maxval=16.0)),
        "gdn_dt_bias": dt + jnp.log(-jnp.expm1(-dt)),
        "gdn_out_norm": _gain(ks[15], (N_GDN, GDN_DV)),
        "ml_w_in": _w(ks[16], (N_ML, D_MODEL, ML_IN), D_MODEL),
        "ml_i_bias": 0.1 * jax.random.normal(ks[17], (N_ML, ML_HEADS), jnp.float32),
        "ml_f_bias": jax.random.uniform(ks[18], (N_ML, ML_HEADS), jnp.float32, minval=3.0, maxval=6.0),
        "ml_out_norm": _gain(ks[19], (N_ML, ML_DV)),
        "sb_w_in": _w(ks[20], (N_SB, D_MODEL, SB_IN), D_MODEL),
    }


def reference(x, mem, mem_norm, norm_pre_mix, norm_post_mix, norm_pre_ffn, norm_post_ffn, w_mem_kv,
              w_out, w_ffn_in, w_ffn_out, gdn_w_in, gdn_conv, gdn_a_log, gdn_dt_bias, gdn_out_norm,
              ml_w_in, ml_i_bias, ml_f_bias, ml_out_norm, sb_w_in):
    h = x
    mem_n = rms_norm(mem, mem_norm)
    for layer in range(DEPTH):
        kind = layer % N_MIXERS
        j = layer // N_MIXERS
        u = rms_norm(h, norm_pre_mix[layer])
        if kind == 0:
            y_mix, q_mem = gdn_branch(u @ gdn_w_in[j], gdn_conv[j], gdn_a_log[j], gdn_dt_bias[j], gdn_out_norm[j])
        elif kind == 1:
            y_mix, q_mem = mlstm_branch(u @ ml_w_in[j], ml_i_bias[j], ml_f_bias[j], ml_out_norm[j])
        else:
            y_mix, q_mem = sb_branch(u @ sb_w_in[j])
        mem_k, mem_v = jnp.split(mem_n @ w_mem_kv[layer], 2, axis=-1)
        y_mem = memory_attention(q_mem, mem_k, mem_v)
        y = jnp.concatenate([y_mix, y_mem], axis=-1) @ w_out[layer]
        h = h + rms_norm(y, norm_post_mix[layer])
        f = swiglu(rms_norm(h, norm_pre_ffn[layer]), w_ffn_in[layer], w_ffn_out[layer])
        h = h + rms_norm(f, norm_post_ffn[layer])
    return h
```

```python
import numpy as np
import ml_dtypes
import concourse.bass as bass
import concourse.mybir as mybir
from concourse.bass_utils import run_bass_kernel_spmd

F32 = mybir.dt.float32
BF16 = mybir.dt.bfloat16
F32R = mybir.dt.float32r
AF = mybir.ActivationFunctionType
ALU = mybir.AluOpType

S = 2048
D = 1024
NB = 4
TB = 512
DEPTH = 4
N_MEM = 256
D_FF = 2816
EPS = 1e-6
SEM_LIMIT = 30000


class Counter:
    def __init__(self, tr, name, inorder):
        self.tr = tr
        self.name = name
        self.inorder = inorder
        self.sems = []
        self.final = {}
        self.epoch = -1
        self.val = 0
        self._new_epoch()

    def _new_epoch(self):
        if self.epoch >= 0:
            self.final[self.epoch] = self.val
        self.epoch += 1
        self.val = 0
        self.sems.append(self.tr.new_sem(f"{self.name}_{self.epoch}"))

    def reserve(self, amount):
        if self.val + amount > SEM_LIMIT:
            self._new_epoch()
        self.val += amount
        return (self.epoch, self.val)

    def peek_next(self, amount=1):
        if self.val + amount > SEM_LIMIT:
            return (self.epoch + 1, amount)
        return (self.epoch, self.val + amount)


class Tracker:
    def __init__(self, nc):
        self.nc = nc
        self._sem_stack = []
        self.eng = {"pe": nc.tensor, "act": nc.scalar, "dve": nc.vector, "pool": nc.gpsimd, "sp": nc.sync}
        self.cnt = {k: Counter(self, k, True) for k in self.eng}
        self.waited = {k: {} for k in self.eng}
        self.last_w = {}
        self.readers = {}
        self.pending_noinc = {k: False for k in self.eng}
        self.n_inst = {k: 0 for k in self.eng}
        self.dry = False

    def new_sem(self, name):
        cm = self.nc.semaphore(name)
        h = cm.__enter__()
        self._sem_stack.append(cm)
        return h

    def new_counter(self, name):
        c = Counter(self, name, False)
        self.cnt[name] = c
        return c

    def _need(self, deps, cnt, tok):
        k = (cnt.name, tok[0])
        if k not in deps or deps[k][1] < tok[1]:
            deps[k] = (cnt, tok[1])

    def _wait_all(self, e, rd, wr):
        deps = {}
        for r in rd:
            lw = self.last_w.get(r)
            if lw is not None:
                self._need(deps, lw[0], lw[1])
        for w in wr:
            lw = self.last_w.get(w)
            if lw is not None:
                self._need(deps, lw[0], lw[1])
            for (c, tok) in self.readers.get(w, {}).values():
                self._need(deps, c, tok)
        wd = self.waited[e]
        for (cname, ep), (cnt, val) in deps.items():
            if cname == e and e == "pe":
                continue
            if wd.get((cname, ep), 0) >= val:
                continue
            if cnt.inorder:
                if any(k[0] == cname and k[1] > ep for k in wd):
                    continue
            self.eng[e].wait_ge(cnt.sems[ep], val)
            wd[(cname, ep)] = val

    def _record(self, cnt, tok, rd, wr):
        for r in rd:
            self.readers.setdefault(r, {})[cnt.name] = (cnt, tok)
        for w in wr:
            self.last_w[w] = (cnt, tok)
            self.readers[w] = {}

    def op(self, e, fn, rd=(), wr=(), inc=True):
        if self.dry:
            return None
        self._wait_all(e, rd, wr)
        inst = fn()
        self.n_inst[e] += 1
        cnt = self.cnt[e]
        if inc:
            tok = cnt.reserve(1)
            inst.then_inc(cnt.sems[tok[0]], 1)
        else:
            tok = cnt.peek_next(1)
        self._record(cnt, tok, rd, wr)
        return inst

    def dma(self, q, cnt, out, in_, rd=(), wr=()):
        if self.dry:
            return None
        self._wait_all(q, rd, wr)
        inst = self.eng[q].dma_start(out=out, in_=in_)
        tok = cnt.reserve(16)
        inst.then_inc(cnt.sems[tok[0]], 16)
        self.n_inst[q] += 1
        self._record(cnt, tok, rd, wr)
        return inst

    def alias(self, dst_keys, src_keys):
        if self.dry:
            return
        acc = {}
        for s in src_keys:
            lw = self.last_w.get(s)
            items = list(self.readers.get(s, {}).values())
            if lw is not None:
                items.append(lw)
            for (c, tok) in items:
                k = (c.name, tok[0])
                if k not in acc or acc[k][1][1] < tok[1]:
                    acc[k] = (c, tok)
        for d in dst_keys:
            rd = self.readers.setdefault(d, {})
            for (cname, ep), (c, tok) in acc.items():
                nm = f"{cname}@{ep}"
                if nm not in rd or rd[nm][1][1] < tok[1]:
                    rd[nm] = (c, tok)

    def barrier(self):
        if self.dry:
            return
        for e in self.eng:
            wd = self.waited[e]
            for c in self.cnt.values():
                eps = range(c.epoch + 1) if not c.inorder else [c.epoch]
                for ep in eps:
                    val = c.val if ep == c.epoch else c.final[ep]
                    if val <= 0 or wd.get((c.name, ep), 0) >= val:
                        continue
                    if c.name == e and e == "pe":
                        continue
                    self.eng[e].wait_ge(c.sems[ep], val)
                    wd[(c.name, ep)] = val

    def wait_key(self, e, keys):
        if self.dry:
            return
        self._wait_all(e, keys, ())

    def close(self):
        for cm in reversed(self._sem_stack):
            cm.__exit__(None, None, None)


class Prog:
    def __init__(self, layers, load_x=True):
        self.layers = layers
        self.nc = bass.Bass("TRN2", target_bir_lowering=False)
        self._stack = []
        self.T = Tracker(self.nc)

    def dram(self, name, shape, dt, kind):
        return self.nc.dram_tensor(name, list(shape), dt, kind=kind).ap()

    def sb(self, name, shape, dt):
        cm = self.nc.sbuf_tensor(name, list(shape), dt)
        t = cm.__enter__()
        self._stack.append(cm)
        return t

    def psum(self, name, shape, dt):
        cm = self.nc.psum_tensor(name, list(shape), dt)
        t = cm.__enter__()
        self._stack.append(cm)
        return t

    def finish(self):
        for cm in reversed(self._stack):
            cm.__exit__(None, None, None)
        self.T.close()


GDN_IN = 4624
ML_IN = 3600
SB_IN = 3584
NWS = 3
NWST = 2
NEGBIG = -30000.0


def build(layers, first=True, last=True):
    P = Prog(layers)
    nc, T = P.nc, P.T
    kinds = [l % 3 for l in layers]
    x_d = P.dram("x", [S, D], F32, "ExternalInput")
    out_d = P.dram("out", [S, D], F32, "ExternalOutput")
    mem_d = P.dram("mem", [N_MEM, D], F32, "ExternalInput")
    identf_d = P.dram("identf", [128, 128], F32, "ExternalInput")
    cb_d = P.dram("cbf", [128, NCB], BF16, "ExternalInput")
    gains_d = P.dram("gains", [128, NG], F32, "ExternalInput")
    W = {
        "w_ffn_in": P.dram("w_ffn_in", [DEPTH, D, 2 * D_FF], F32, "ExternalInput"),
        "w_ffn_out": P.dram("w_ffn_out", [DEPTH, D_FF, D], F32, "ExternalInput"),
        "w_mem_kv": P.dram("w_mem_kv", [DEPTH, D, 1024], F32, "ExternalInput"),
        "w_out": P.dram("w_out", [DEPTH, 1536, D], F32, "ExternalInput"),
        "gdn_w_in": P.dram("gdn_w_in", [2, D, GDN_IN], F32, "ExternalInput"),
        "ml_w_in": P.dram("ml_w_in", [1, D, ML_IN], F32, "ExternalInput"),
        "sb_w_in": P.dram("sb_w_in", [1, D, SB_IN], F32, "ExternalInput"),
    }

    hT = P.sb("hT", [128, 8, S], F32)
    uT = P.sb("uT", [128, 8, TB], BF16)
    sq = P.sb("sq", [128, 2, TB], BF16)
    rstd = P.sb("rstd", [128, TB], F32)
    ysb_raw = P.sb("ysb", [128, 8 * TB], F32)
    ysb = ysb_raw[:, :].rearrange("p (c n) -> p c n", c=8)
    R1 = P.sb("R1", [128, 22 * TB], BF16)
    aT = R1[:, :].rearrange("p (j n) -> p j n", j=22)
    sg = P.sb("sg", [128, 1, TB], BF16)
    tmpn = P.sb("tmpn", [128, 1, TB], F32)
    LS = {}
    identf = P.sb("identf_sb", [128, 128], F32)
    cb = P.sb("cb_sb", [128, NCB], BF16)
    gains = P.sb("gains_sb", [128, NG], F32)
    wst = P.sb("wst", [128, NWST, 2048], F32)
    wbf = P.sb("wbf", [128, NWS, 2048], BF16)
    catT = P.sb("catT", [128, 12, TB], BF16)
    qmT = P.sb("qmT", [128, 4, TB], BF16)
    memnT = P.sb("memnT", [128, 8, N_MEM], BF16)
    memkT = P.sb("memkT", [128, 4, N_MEM], BF16)
    memv = P.sb("memv", [128, 2, 512], BF16)
    negkmax = P.sb("negkmax", [128, 4], F32)
    cq = rstd
    negc = P.sb("negc", [1, TB], BF16)
    pT = P.sb("pT", [128, 2, TB], BF16)
    rden = P.sb("rden", [128, TB], F32)
    epsb = P.sb("epsb", [128, 1], F32)
    oneb = P.sb("oneb", [128, 1], F32)
    mhalf = P.sb("mhalf", [128, TB], BF16)
    ps = [P.psum(f"ps{i}", [128, TB], F32) for i in range(8)]

    onesb = cb[:, CB_ONES:CB_ONES + 128]
    negones = cb[:, CB_NEGONES:CB_NEGONES + 128]
    identb = cb[:, CB_IDENT:CB_IDENT + 128]
    negMincl = cb[:, CB_NEGMINCL:CB_NEGMINCL + 128]

    c_io = T.new_counter("io")
    c_w = [T.new_counter(f"w{i}") for i in range(NWST)]

    def gain(l, which, c):
        i = (l * 4 + which) * 8 + c
        return gains[:, i:i + 1]

    def xin(sl):
        return ysb[:, 2 * sl:2 * sl + 2, :].rearrange("p a b -> p (a b)")

    def xink(sl):
        return [("ysb", 2 * sl), ("ysb", 2 * sl + 1)]

    ring = {"i": 0}

    def nextbank():
        ring["i"] = (ring["i"] + 1) % 5
        return ring["i"]

    wplan = []
    wstate = {"issued": 0, "next": 0, "lb": None, "idx": 0}
    NWC = 72
    wcache_d = P.dram("wcache", [NWC, 128, 2048], BF16, "Internal")
    c_wb = [T.new_counter(f"wb{i}") for i in range(NWS + 2 * NWST)]
    c_wc = T.new_counter("wc")

    wst_b = wst.bitcast(BF16)
    NSLOT = NWS + 2 * NWST

    def WT(slot):
        if slot < NWS:
            return wbf[:, slot, :]
        j = slot - NWS
        return wst_b[:, j // 2, (j % 2) * 2048:(j % 2 + 1) * 2048]

    def WK(slot):
        if slot < NWS:
            return ("wbf", slot)
        j = slot - NWS
        return ("wst", j // 2, (j % 2) * 1024)

    def w_issue(i):
        req, lb, idx = wplan[i]
        n = sum(kc * ncols for (_, _, _, kc, _, ncols) in req)
        if lb is not None and lb[1] > 0:
            slot = wstate["nb16"] % NSLOT
            wstate["nb16"] += 1
            wstate["slot_of"][i] = slot
            T.dma("sp", c_wb[slot], WT(slot)[:, 0:n], wcache_d[idx, :, 0:n], rd=[("wc", idx)], wr=[WK(slot)])
            return
        slot = wstate["nf32"] % NWS
        st = wstate["nf32"] % NWST
        wstate["nf32"] += 1
        wstate["slot_of"][i] = slot
        off = 0
        for pi, (name, l, r0, kc, c0, ncols) in enumerate(req):
            dst = wst[:, st, off:off + kc * ncols].rearrange("p (k n) -> p k n", k=kc)
            src = W[name][l, r0:r0 + kc * 128, c0:c0 + ncols].rearrange("(k p) n -> p k n", p=128)
            if len(req) == 1:
                wk = [("wst", st, 0), ("wst", st, 1024)]
            else:
                assert kc * ncols <= 1024 and off == pi * 1024
                wk = [("wst", st, pi * 1024)]
            T.dma("sp", c_w[st], dst, src, wr=wk)
            off += kc * ncols
        T.op("act", lambda: nc.scalar.copy(out=wbf[:, slot, 0:n], in_=wst[:, st, 0:n]),
             rd=[("wst", st, 0), ("wst", st, 1024)], wr=[("wbf", slot)])
        if lb is not None:
            T.dma("pool", c_wc, wcache_d[idx, :, 0:n], wbf[:, slot, 0:n], rd=[("wbf", slot)], wr=[("wc", idx)])

    def w_get(req):
        req = tuple(req)
        if T.dry:
            wplan.append((req, wstate["lb"], wstate["idx"]))
            wstate["idx"] += 1
            return 0
        i = wstate["next"]
        assert wplan[i][0] == req, (wplan[i], req)

        def depth(k):
            lb = wplan[k][1]
            return (NSLOT - 1) if (lb is not None and lb[1] > 0) else (NWS - 1)
        def isb16(k):
            lb = wplan[k][1]
            return lb is not None and lb[1] > 0
        while wstate["issued"] < len(wplan):
            k = wstate["issued"]
            if k > i + depth(k) - 1:
                break
            if k > i and isb16(k) != isb16(i):
                break
            w_issue(k)
            wstate["issued"] += 1
        wstate["next"] += 1
        return wstate["slot_of"][i]

    def w_block(l, b):
        wstate["lb"] = (l, b) if b is not None else None
        wstate["idx"] = 0

    def wview(slot, off, kc, ncols):
        return WT(slot)[:, off:off + kc * ncols].rearrange("p (k n) -> p k n", k=kc)

    lcount = {"n": 0}

    def layer_alloc(kind):
        lcount["n"] += 1
        tag = lcount["n"]
        cms = []

        def mk(name, shape, dt):
            cm = nc.sbuf_tensor(f"{name}_{tag}", list(shape), dt)
            t = cm.__enter__()
            cms.append(cm)
            return t
        if kind == 0:
            LS["gF"] = mk("gF", [128, 3584], F32)
            LS["gR2"] = mk("gR2", [128, 2944], F32)
            gdn_alloc()
        else:
            LS["R2"] = mk("R2", [128, 12544], BF16)
            LS["R2f"] = LS["R2"].bitcast(F32)
            if kind == 2:
                sb_alloc()
            else:
                ml_alloc()
        LS["cms"] = cms

    def layer_free():
        for cm in reversed(LS["cms"]):
            cm.__exit__(None, None, None)
        LS["cms"] = []

    def rms_stats(src_fn, src_keys, n):
        pk = ("ps", 7)
        for c in range(8):
            k = c % 2
            T.op("act", lambda: nc.scalar.activation(out=sq[:, k, 0:n], in_=src_fn(c), func=AF.Square),
                 rd=[src_keys(c)], wr=[("sq", k)])
            T.op("pe", lambda: nc.tensor.matmul(ps[7][:, 0:n], lhsT=onesb, rhs=sq[:, k, 0:n],
                                                start=(c == 0), stop=(c == 7)),
                 rd=["cb", ("sq", k)], wr=[pk])
        T.op("act", lambda: nc.scalar.activation(out=rstd[:, 0:n], in_=ps[7][:, 0:n], func=AF.Identity,
                                                 scale=1.0 / D, bias=epsb[:]),
             rd=[pk, "epsb"], wr=["rstd"])
        T.op("pool", lambda: nc.gpsimd.tensor_tensor(out=rstd[:, 0:n], in0=rstd[:, 0:n], in1=mhalf[:, 0:n], op=ALU.pow),
             rd=["rstd", "mhalf"], wr=["rstd"])

    def pre_norm(l, which, b):
        rms_stats(lambda c: hT[:, c, b * TB:(b + 1) * TB], lambda c: ("hT", c, b), TB)
        for c in range(8):
            T.op("dve", lambda: nc.vector.scalar_tensor_tensor(
                out=uT[:, c, :], in0=hT[:, c, b * TB:(b + 1) * TB], scalar=gain(l, which, c),
                in1=rstd[:], op0=ALU.mult, op1=ALU.mult),
                rd=[("hT", c, b), "gains", "rstd"], wr=[("uT", c)])

    def post_norm_add(l, which, b):
        rms_stats(lambda c: ysb[:, c, :], lambda c: ("ysb", c), TB)
        for c in range(8):
            k = 0
            T.op("dve", lambda: nc.vector.scalar_tensor_tensor(
                out=tmpn[:, k, :], in0=ysb[:, c, :], scalar=gain(l, which, c),
                in1=rstd[:], op0=ALU.mult, op1=ALU.mult),
                rd=[("ysb", c), "gains", "rstd"], wr=[("tmpn", k)])
            T.op("pool", lambda: nc.gpsimd.tensor_tensor(
                out=hT[:, c, b * TB:(b + 1) * TB], in0=hT[:, c, b * TB:(b + 1) * TB],
                in1=tmpn[:, k, :], op=ALU.add),
                rd=[("hT", c, b), ("tmpn", k)], wr=[("hT", c, b)])

    def proj_fm(wname, wl, c0, nchunks, xT, xkey, n, handler):
        j = 0
        while j < nchunks:
            nn = min(2, nchunks - j)
            slot = w_get([(wname, wl, 0, 8, c0 + j * 128, nn * 128)])
            wv = wview(slot, 0, 8, nn * 128)
            for jj in range(nn):
                pb = nextbank()
                for kc in range(8):
                    T.op("pe", lambda: nc.tensor.matmul(ps[pb][:, 0:n], lhsT=wv[:, kc, jj * 128:(jj + 1) * 128],
                                                        rhs=xT[:, kc, 0:n], start=(kc == 0), stop=(kc == 7)),
                         rd=[WK(slot), (xkey, kc)], wr=[("ps", pb)], inc=(kc == 7))
                handler(j + jj, pb)
            j += nn

    def proj_tm(wname, wl, c0, ncols, xT, xkey, ntt, handler):
        slot = w_get([(wname, wl, 0, 8, c0, ncols)])
        wv = wview(slot, 0, 8, ncols)
        for tt in range(ntt):
            pb = nextbank()
            for kc in range(8):
                T.op("pe", lambda: nc.tensor.matmul(ps[pb][:, 0:ncols], lhsT=xT[:, kc, tt * 128:(tt + 1) * 128],
                                                    rhs=wv[:, kc, :], start=(kc == 0), stop=(kc == 7)),
                     rd=[WK(slot), (xkey, kc)], wr=[("ps", pb)], inc=(kc == 7))
            handler(tt, pb)

    mixer_keys = []
    ysb_overlay = []

    def ffn(l, b):
        T.alias([("aT", j) for j in range(22)], mixer_keys)
        pre_norm(l, 2, b)
        for j in range(22):
            slot = w_get([("w_ffn_in", l, 0, 8, j * 128, 128), ("w_ffn_in", l, 0, 8, D_FF + j * 128, 128)])
            wv = WT(slot).rearrange("p (g k n) -> p g k n", g=2, k=8)
            pg, pu = nextbank(), nextbank()
            for gi, pbank in ((0, pg), (1, pu)):
                for kc in range(8):
                    T.op("pe", lambda: nc.tensor.matmul(ps[pbank][:], lhsT=wv[:, gi, kc, :], rhs=uT[:, kc, :],
                                                        start=(kc == 0), stop=(kc == 7)),
                         rd=[WK(slot), ("uT", kc)], wr=[("ps", pbank)], inc=(kc == 7))
            k = 0
            T.op("act", lambda: nc.scalar.activation(out=sg[:, k, :], in_=ps[pg][:], func=AF.Silu),
                 rd=[("ps", pg)], wr=[("sg", k)])
            T.op("dve", lambda: nc.vector.tensor_tensor(out=aT[:, j, :], in0=sg[:, k, :], in1=ps[pu][:],
                                                        op=ALU.mult),
                 rd=[("sg", k), ("ps", pu)], wr=[("aT", j)])
        for c in range(8):
            pbank = nextbank()
            for hh in range(2):
                slot = w_get([("w_ffn_out", l, hh * 1408, 11, c * 128, 128)])
                wv = wview(slot, 0, 11, 128)
                for kc in range(11):
                    kk = hh * 11 + kc
                    T.op("pe", lambda: nc.tensor.matmul(ps[pbank][:], lhsT=wv[:, kc, :], rhs=aT[:, kk, :],
                                                        start=(kk == 0), stop=(kk == 21)),
                         rd=[WK(slot), ("aT", kk)], wr=[("ps", pbank)], inc=(kc == 10))
            T.op("act", lambda: nc.scalar.copy(out=ysb[:, c, :], in_=ps[pbank][:]),
                 rd=[("ps", pbank)], wr=[("ysb", c)])
        post_norm_add(l, 3, b)

    def out_proj(l, b):
        T.alias([("ysb", c) for c in range(8)], ysb_overlay)
        for c in range(8):
            slot = w_get([("w_out", l, 0, 12, c * 128, 128)])
            wv = wview(slot, 0, 12, 128)
            pbank = nextbank()
            for kc in range(12):
                T.op("pe", lambda: nc.tensor.matmul(ps[pbank][:], lhsT=wv[:, kc, :], rhs=catT[:, kc, :],
                                                    start=(kc == 0), stop=(kc == 11)),
                     rd=[WK(slot), ("catT", kc)], wr=[("ps", pbank)], inc=(kc == 11))
            T.op("act", lambda: nc.scalar.copy(out=ysb[:, c, :], in_=ps[pbank][:]),
                 rd=[("ps", pbank)], wr=[("ysb", c)])
        post_norm_add(l, 1, b)

    def prep_mem():
        for t in range(2):
            sl = t % 2
            T.dma("sp", c_io, xin(sl), mem_d[t * 128:(t + 1) * 128, :], wr=xink(sl))
            for half in range(2):
                pb = nextbank()
                pk = ("ps", pb)
                for q in range(4):
                    c = half * 4 + q
                    T.op("pe", lambda: nc.tensor.transpose(out=ps[pb][:, q * 128:(q + 1) * 128],
                                                           in_=xin(sl)[:, c * 128:(c + 1) * 128],
                                                           identity=identf[:]),
                         rd=xink(sl) + ["identf"], wr=[pk], inc=(q == 3))
                T.op("dve", lambda: nc.vector.tensor_copy(
                    out=hT[:, half * 4:(half + 1) * 4, t * 128:(t + 1) * 128],
                    in_=ps[pb][:].rearrange("p (q n) -> p q n", q=4)), rd=[pk],
                    wr=[("hT", c, 0) for c in range(half * 4, half * 4 + 4)])
        rms_stats(lambda c: hT[:, c, 0:N_MEM], lambda c: ("hT", c, 0), N_MEM)
        for c in range(8):
            T.op("dve", lambda: nc.vector.scalar_tensor_tensor(
                out=memnT[:, c, :], in0=hT[:, c, 0:N_MEM], scalar=gains[:, G_MEM + c:G_MEM + c + 1],
                in1=rstd[:, 0:N_MEM], op0=ALU.mult, op1=ALU.mult),
                rd=[("hT", c, 0), "gains", "rstd"], wr=[("memnT", c)])

    def mem_kv(l):
        def hk(j, pb):
            T.op("act", lambda: nc.scalar.copy(out=memkT[:, j, :], in_=ps[pb][:, 0:N_MEM]),
                 rd=[("ps", pb)], wr=[("memkT", j)])
            T.op("act", lambda: nc.scalar.activation(out=sq[:, j % 2, 0:N_MEM], in_=ps[pb][:, 0:N_MEM], func=AF.Square),
                 rd=[("ps", pb)], wr=[("sq", j % 2)])
            T.op("pe", lambda: nc.tensor.matmul(ps[6][:, 0:N_MEM], lhsT=onesb, rhs=sq[:, j % 2, 0:N_MEM],
                                                start=True, stop=True),
                 rd=["cb", ("sq", j % 2)], wr=[("ps", 6)])
            T.op("dve", lambda: nc.vector.reduce_max(out=negkmax[:, j:j + 1], in_=ps[6][:, 0:N_MEM],
                                                     axis=mybir.AxisListType.X),
                 rd=[("ps", 6)], wr=[("negkmax", j)])
            T.op("act", lambda: nc.scalar.activation(out=negkmax[:, j:j + 1], in_=negkmax[:, j:j + 1], func=AF.Sqrt),
                 rd=[("negkmax", j)], wr=[("negkmax", j)])
            T.op("dve", lambda: nc.vector.tensor_scalar(out=negkmax[:, j:j + 1], in0=negkmax[:, j:j + 1],
                                                        scalar1=-1.0, scalar2=None, op0=ALU.mult),
                 rd=[("negkmax", j)], wr=[("negkmax", j)])
        proj_fm("w_mem_kv", l, 0, 4, memnT, "memnT", N_MEM, hk)
        for half in range(2):
            def hv(tt, pb):
                T.op("act", lambda: nc.scalar.copy(out=memv[:, tt, half * 256:(half + 1) * 256], in_=ps[pb][:, 0:256]),
                     rd=[("ps", pb)], wr=[("memv", tt, half)])
            proj_tm("w_mem_kv", l, 512 + half * 256, 256, memnT, "memnT", 2, hv)

    def qmem_handler(j, pb):
        T.op("act", lambda: nc.scalar.activation(out=qmT[:, j, :], in_=ps[pb][:], func=AF.Copy, scale=128.0 ** -0.5),
             rd=[("ps", pb)], wr=[("qmT", j)])

    def mem_attn(b):
        for hm in range(4):
            T.op("act", lambda: nc.scalar.activation(out=sq[:, hm % 2, :], in_=qmT[:, hm, :], func=AF.Square),
                 rd=[("qmT", hm)], wr=[("sq", hm % 2)])
            T.op("pe", lambda: nc.tensor.matmul(ps[6][:], lhsT=onesb, rhs=sq[:, hm % 2, :], start=True, stop=True),
                 rd=["cb", ("sq", hm % 2)], wr=[("ps", 6)])
            T.op("act", lambda: nc.scalar.activation(out=cq[0:1, :], in_=ps[6][0:1, :], func=AF.Identity, bias=epsb[0:1, :]),
                 rd=[("ps", 6), "epsb"], wr=["rstd"])
            T.op("pool", lambda: nc.gpsimd.tensor_tensor(out=rden[0:1, :], in0=cq[0:1, :], in1=mhalf[0:1, :], op=ALU.pow),
                 rd=["rstd", "mhalf"], wr=["rden"])
            T.op("dve", lambda: nc.vector.scalar_tensor_tensor(out=negc[0:1, :], in0=cq[0:1, :],
                                                               scalar=negkmax[0:1, hm:hm + 1], in1=rden[0:1, :],
                                                               op0=ALU.mult, op1=ALU.mult),
                 rd=["rstd", ("negkmax", hm), "rden"], wr=["negc"])
            for mt in range(2):
                pb = nextbank()
                T.op("pe", lambda: nc.tensor.matmul(ps[pb][:], lhsT=memkT[:, hm, mt * 128:(mt + 1) * 128],
                                                    rhs=qmT[:, hm, :], start=True, stop=False),
                     rd=[("memkT", hm), ("qmT", hm)], wr=[("ps", pb)], inc=False)
                T.op("pe", lambda: nc.tensor.matmul(ps[pb][:], lhsT=onesb[0:1, :], rhs=negc[0:1, :],
                                                    start=False, stop=True),
                     rd=["cb", "negc"], wr=[("ps", pb)])
                T.op("act", lambda: nc.scalar.activation(out=pT[:, mt, :], in_=ps[pb][:], func=AF.Exp),
                     rd=[("ps", pb)], wr=[("pT", mt)])
            po, pd = nextbank(), 6
            for mt in range(2):
                T.op("pe", lambda: nc.tensor.matmul(ps[po][:], lhsT=memv[:, mt, hm * 128:(hm + 1) * 128],
                                                    rhs=pT[:, mt, :], start=(mt == 0), stop=(mt == 1)),
                     rd=[("memv", mt, hm // 2), ("pT", mt)], wr=[("ps", po)], inc=(mt == 1))
            for mt in range(2):
                T.op("pe", lambda: nc.tensor.matmul(ps[pd][:], lhsT=onesb, rhs=pT[:, mt, :],
                                                    start=(mt == 0), stop=(mt == 1)),
                     rd=["cb", ("pT", mt)], wr=[("ps", pd)], inc=(mt == 1))
            T.op("act", lambda: nc.scalar.activation(out=rden[:], in_=ps[pd][:], func=AF.Square), rd=[("ps", pd)], wr=["rden"])
            T.op("pool", lambda: nc.gpsimd.tensor_tensor(out=rden[:], in0=rden[:], in1=mhalf[:], op=ALU.pow),
                 rd=["rden", "mhalf"], wr=["rden"])
            T.op("dve", lambda: nc.vector.tensor_tensor(out=catT[:, 8 + hm, :], in0=ps[po][:], in1=rden[:],
                                                        op=ALU.mult),
                 rd=[("ps", po), "rden"], wr=[("catT", 8 + hm)])

    sbst = {}
    kT_d = P.dram("sb_kT_scr", [8, 128, S], BF16, "Internal")
    v_d = P.dram("sb_v_scr", [8, S, 128], BF16, "Internal")
    c_kv = T.new_counter("kv")

    def sb_alloc():
        R2, R2f = LS["R2"], LS["R2f"]
        def v3(lo):
            return R2[:, lo:lo + 1536].rearrange("p (a n) -> p a n", a=3)
        sbst["qT"] = R2[:, 0:4096].rearrange("p (a n) -> p a n", a=8)
        sbst["Lp"] = v3(4096)
        sbst["attB"] = v3(5632)
        sbst["Pm"] = v3(7168)
        sbst["att"] = v3(8704)
        sbst["e"] = v3(10240)
        sbst["kc"] = R1[:, 0:4096].rearrange("p (a n) -> p a n", a=2)
        sbst["vc"] = R1[:, 4096:8192].rearrange("p (a k n) -> p a k n", a=2, k=16)
        sbst["kst"] = R1[:, 8192:9216].rearrange("p (a n) -> p a n", a=2)
        sbst["vst"] = R1[:, 9216:9728].rearrange("p (a n) -> p a n", a=2)

    def sb_setup():
        T.dma("sp", c_io, cb[:, CB_MASK01:CB_MASK01 + 4 * TB], cb_d[:, CB_MASK01:CB_MASK01 + 4 * TB], wr=["cb"])

    def sb_inproj(l, b):
        qT, kst, vst = sbst["qT"], sbst["kst"], sbst["vst"]
        mixer_keys[:] = [("sb_kc", 0), ("sb_kc", 1), ("sb_vc", 0), ("sb_vc", 1), ("sb_kst", 0), ("sb_kst", 1), ("sb_vst", 0), ("sb_vst", 1)]
        T.alias(mixer_keys, [("aT", j) for j in range(22)])

        def hq(j, pb):
            T.op("act", lambda: nc.scalar.activation(out=qT[:, j, :], in_=ps[pb][:], func=AF.Copy, scale=0.125),
                 rd=[("ps", pb)], wr=[("sb_qT", j)])

        def hk(j, pb):
            k2 = j % 2
            T.op("dve", lambda: nc.vector.tensor_copy(out=kst[:, k2, :], in_=ps[pb][:]),
                 rd=[("ps", pb)], wr=[("sb_kst", k2)])
            T.dma("sp", c_kv, kT_d[j, :, b * TB:(b + 1) * TB], kst[:, k2, :], rd=[("sb_kst", k2)],
                  wr=[("kT_d", j, b)])
        proj_fm("sb_w_in", 0, 0, 8, uT, "uT", TB, hq)
        proj_fm("sb_w_in", 0, 1024, 8, uT, "uT", TB, hk)
        for q4 in range(4):
            def hv(tt, pb):
                k2 = tt % 2
                T.op("act", lambda: nc.scalar.copy(out=vst[:, k2, :], in_=ps[pb][:, 0:256]),
                     rd=[("ps", pb)], wr=[("sb_vst", k2)])
                for pp in range(2):
                    cpair = 2 * q4 + pp
                    t0 = b * TB + tt * 128
                    T.dma("sp", c_kv, v_d[cpair, t0:t0 + 128, :], vst[:, k2, pp * 128:(pp + 1) * 128],
                          rd=[("sb_vst", k2)], wr=[("v_d", cpair, b)])
            proj_tm("sb_w_in", 0, 2048 + q4 * 256, 256, uT, "uT", 4, hv)
        proj_fm("sb_w_in", 0, 3072, 4, uT, "uT", TB, qmem_handler)

    def sb_load_pair(c, b):
        sl = c % 2
        n = (b + 1) * TB
        T.dma("sp", c_kv, sbst["kc"][:, sl, 0:n], kT_d[c, :, 0:n], rd=[("kT_d", c, bb) for bb in range(b + 1)],
              wr=[("sb_kc", sl)])
        T.dma("sp", c_kv, sbst["vc"][:, sl, 0:4 * (b + 1), :],
              v_d[c, 0:n, :].rearrange("(k p) n -> p k n", p=128),
              rd=[("v_d", c, bb) for bb in range(b + 1)], wr=[("sb_vc", sl)])

    def sb_attn(b):
        qT = sbst["qT"]
        e, Lp, attB, Pm, att = sbst["e"], sbst["Lp"], sbst["attB"], sbst["Pm"], sbst["att"]
        sb_load_pair(0, b)
        nk = 4 * b + 4
        rot = {"i": 0}

        def sbbank():
            rot["i"] = (rot["i"] + 1) % 6
            return (0, 1, 2, 3, 4, 7)[rot["i"]]
        for c in range(8):
            if c + 1 < 8:
                sb_load_pair(c + 1, b)
            sl = c % 2
            kc, vc = sbst["kc"], sbst["vc"]
            pO = 5
            for hh in range(2):
                po = hh * 64
                qview = qT[po:po + 64, c, :]
                order = list(range(nk - 1, -1, -1))
                for g0 in range(0, nk, 3):
                    grp = [(g0 + u, order[g0 + u]) for u in range(3) if g0 + u < nk]
                    banks_a, banks_b = {}, {}
                    for u, (idx, kb) in enumerate(grp):
                        kview = kc[po:po + 64, sl, kb * 128:(kb + 1) * 128]
                        pa = sbbank()
                        banks_a[u] = pa
                        T.op("pe", lambda: nc.tensor.matmul(ps[pa][:], lhsT=kview, rhs=qview, start=True, stop=True),
                             rd=[("sb_kc", sl), ("sb_qT", c)], wr=[("ps", pa)])
                    for u, (idx, kb) in enumerate(grp):
                        pa = banks_a[u]
                        T.op("act", lambda: nc.scalar.activation(out=e[:, u, :], in_=ps[pa][:], func=AF.Exp),
                             rd=[("ps", pa)], wr=[("sb_e", u)])
                    for u, (idx, kb) in enumerate(grp):
                        T.op("act", lambda: nc.scalar.activation(out=Lp[:, u, :], in_=e[:, u, :], func=AF.Ln,
                                                                 bias=oneb[:]),
                             rd=[("sb_e", u), "oneb"], wr=[("sb_Lp", u)])
                        i = kb - 4 * b
                        if i >= 0:
                            m01 = cb[:, CB_MASK01 + i * TB:CB_MASK01 + (i + 1) * TB]
                            T.op("pool", lambda: nc.gpsimd.tensor_tensor(out=Lp[:, u, :], in0=Lp[:, u, :], in1=m01,
                                                                         op=ALU.mult),
                                 rd=[("sb_Lp", u), "cb"], wr=[("sb_Lp", u)])
                    for u, (idx, kb) in enumerate(grp):
                        kview = kc[po:po + 64, sl, kb * 128:(kb + 1) * 128]
                        pbk = sbbank()
                        banks_b[u] = pbk
                        T.op("pe", lambda: nc.tensor.matmul(ps[pbk][:], lhsT=kview, rhs=qview, start=True, stop=False),
                             rd=[("sb_kc", sl), ("sb_qT", c)], wr=[("ps", pbk)], inc=False)
                        T.op("pe", lambda: nc.tensor.matmul(ps[pbk][:], lhsT=negMincl, rhs=Lp[:, u, :],
                                                            start=False, stop=True),
                             rd=["cb", ("sb_Lp", u)], wr=[("ps", pbk)])
                    for u, (idx, kb) in enumerate(grp):
                        pbk = banks_b[u]
                        T.op("act", lambda: nc.scalar.activation(out=attB[:, u, :], in_=ps[pbk][:], func=AF.Exp),
                             rd=[("ps", pbk)], wr=[("sb_attB", u)])
                        i = kb - 4 * b
                        if i >= 0:
                            m01 = cb[:, CB_MASK01 + i * TB:CB_MASK01 + (i + 1) * TB]
                            T.op("pool", lambda: nc.gpsimd.tensor_tensor(out=attB[:, u, :], in0=attB[:, u, :], in1=m01,
                                                                         op=ALU.mult),
                                 rd=[("sb_attB", u), "cb"], wr=[("sb_attB", u)])
                    for u, (idx, kb) in enumerate(grp):
                        if idx > 0:
                            T.op("act", lambda: nc.scalar.activation(out=Pm[:, u, :], in_=ps[6][:], func=AF.Exp),
                                 rd=[("ps", 6)], wr=[("sb_Pm", u)])
                            T.op("dve", lambda: nc.vector.tensor_tensor(out=att[:, u, :], in0=attB[:, u, :],
                                                                        in1=Pm[:, u, :], op=ALU.mult),
                                 rd=[("sb_attB", u), ("sb_Pm", u)], wr=[("sb_att", u)])
                            a_ap, a_key = att[:, u, :], ("sb_att", u)
                        else:
                            a_ap, a_key = attB[:, u, :], ("sb_attB", u)
                        if idx < nk - 1:
                            T.op("pe", lambda: nc.tensor.matmul(ps[6][:], lhsT=negones, rhs=Lp[:, u, :],
                                                                start=(idx == 0), stop=True),
                                 rd=["cb", ("sb_Lp", u)], wr=[("ps", 6)])
                        T.op("pe", lambda: nc.tensor.matmul(ps[pO][po:po + 64, :], lhsT=vc[:, sl, kb, po:po + 64],
                                                            rhs=a_ap, start=(idx == 0), stop=(idx == nk - 1),
                                                            tile_position=(0, po)),
                             rd=[("sb_vc", sl), a_key], wr=[("ps", pO, hh)])
            T.op("dve", lambda: nc.vector.tensor_copy(out=catT[:, c, :], in_=ps[pO][:]),
                 rd=[("ps", pO, 0), ("ps", pO, 1)], wr=[("catT", c)])

    mlst = {}
    sel_d = P.dram("selc", [16, 8 * 128], F32, "ExternalInput")
    mlb_d = P.dram("mlbias", [16, 1], F32, "ExternalInput")
    mlg_d = P.dram("mlgain", [128, 1], F32, "ExternalInput")
    m01i_d = P.dram("mask01incl", [128, 4 * TB], BF16, "ExternalInput")

    def ml_alloc():
        R2, R2f = LS["R2"], LS["R2f"]
        mlst["qT"] = R2[:, 0:2048].rearrange("p (a n) -> p a n", a=4)
        mlst["sigo"] = R2[:, 2048:2560]
        mlst["Dm"] = R2[:, 2560:3584].rearrange("p (a n) -> p a n", a=2)
        mlst["Wt"] = R2[:, 3584:4608].rearrange("p (a n) -> p a n", a=2)
        mlst["hsb"] = R2f[:, 2304:2816]
        mlst["gsb"] = R2f[0:16, 2816:3328]
        mlst["lp"] = R2f[0:16, 3328:3840]
        mlst["cum"] = R2f[0:16, 3840:4352]
        mlst["NF"] = R2f[0:16, 4352:4864]
        mlst["sel"] = R2f[0:16, 4864:5888]
        mlst["Acol"] = R2f[:, 5888:6016].rearrange("p (a n) -> p a n", a=16)
        mlst["tT"] = R2f[:, 6016:6048].rearrange("p (a n) -> p a n", a=2)
        mlst["carry"] = R2f[0:16, 6048:6049]
        mlst["bias"] = R2f[0:16, 6049:6050]
        mlst["gain"] = R2f[:, 6050:6051]
        mlst["kc"] = R1[:, 0:4096].rearrange("p (a n) -> p a n", a=2)
        mlst["vc"] = R1[:, 4096:8192].rearrange("p (a k n) -> p a k n", a=2, k=16)
        mlst["kst"] = R1[:, 8192:9216].rearrange("p (a n) -> p a n", a=2)
        mlst["vst"] = R1[:, 9216:9728].rearrange("p (a n) -> p a n", a=2)

    def ml_setup():
        T.dma("sp", c_io, mlst["sel"][:], sel_d, wr=["ml_sel"])
        T.dma("sp", c_io, mlst["bias"][:], mlb_d, wr=["ml_bias"])
        T.dma("sp", c_io, mlst["gain"][:], mlg_d, wr=["ml_gain"])
        T.dma("sp", c_io, cb[:, CB_MASK01:CB_MASK01 + 4 * TB], m01i_d, wr=["cb"])

    def ml_inproj(l, b):
        qT, kst, vst = mlst["qT"], mlst["kst"], mlst["vst"]
        gsb, lp, cum, carry, NF, Acol, tT = (mlst[k] for k in ("gsb", "lp", "cum", "carry", "NF", "Acol", "tT"))
        mixer_keys[:] = [("sb_kc", 0), ("sb_kc", 1), ("sb_vc", 0), ("sb_vc", 1), ("sb_kst", 0), ("sb_kst", 1),
                         ("sb_vst", 0), ("sb_vst", 1)]
        T.alias(mixer_keys, [("aT", j) for j in range(22)])

        def hq(j, pb):
            T.op("act", lambda: nc.scalar.copy(out=qT[:, j, :], in_=ps[pb][:]), rd=[("ps", pb)], wr=[("ml_qT", j)])

        def hk(j, pb):
            k2 = j % 2
            T.op("dve", lambda: nc.vector.tensor_scalar(out=kst[:, k2, :], in0=ps[pb][:], scalar1=0.125, scalar2=None,
                                                        op0=ALU.mult),
                 rd=[("ps", pb)], wr=[("sb_kst", k2)])
            T.dma("sp", c_kv, kT_d[j, :, b * TB:(b + 1) * TB], kst[:, k2, :], rd=[("sb_kst", k2)],
                  wr=[("kT_d", j, b)])
        proj_fm("ml_w_in", 0, 0, 4, uT, "uT", TB, hq)
        proj_fm("ml_w_in", 0, 512, 4, uT, "uT", TB, hk)
        for q4 in range(4):
            def hv(tt, pb):
                k2 = tt % 2
                T.op("act", lambda: nc.scalar.copy(out=vst[:, k2, :], in_=ps[pb][:, 0:256]),
                     rd=[("ps", pb)], wr=[("sb_vst", k2)])
                for pp in range(2):
                    hd = 2 * q4 + pp
                    t0 = b * TB + tt * 128
                    T.dma("sp", c_kv, v_d[hd, t0:t0 + 128, :], vst[:, k2, pp * 128:(pp + 1) * 128],
                          rd=[("sb_vst", k2)], wr=[("v_d", hd, b)])
            proj_tm("ml_w_in", 0, 1024 + q4 * 256, 256, uT, "uT", 4, hv)
        slot = w_get([("ml_w_in", 0, 0, 8, 3072, 16)])
        wv = wview(slot, 0, 8, 16)
        pg = nextbank()
        for kc in range(8):
            T.op("pe", lambda: nc.tensor.matmul(ps[pg][0:16, :], lhsT=wv[:, kc, :], rhs=uT[:, kc, :],
                                                start=(kc == 0), stop=(kc == 7)),
                 rd=[WK(slot), ("uT", kc)], wr=[("ps", pg)], inc=(kc == 7))
        T.op("act", lambda: nc.scalar.activation(out=gsb[:], in_=ps[pg][0:16, :], func=AF.Identity,
                                                 bias=mlst["bias"][:]),
             rd=[("ps", pg), "ml_bias"], wr=["ml_gsb"])
        T.op("act", lambda: nc.scalar.activation(out=lp[:], in_=gsb[:], func=AF.Exp, scale=-1.0),
             rd=["ml_gsb"], wr=["ml_lp"])
        T.op("act", lambda: nc.scalar.activation(out=lp[:], in_=lp[:], func=AF.Ln, bias=oneb[0:16, :]),
             rd=["ml_lp", "oneb"], wr=["ml_lp"])
        if b == 0:
            T.op("dve", lambda: nc.vector.memset(carry[:], 0.0), wr=["ml_carry"])
        T.op("dve", lambda: nc.vector.tensor_tensor_scan(out=cum[:], data0=lp[:], data1=lp[:], initial=carry[:, 0:1],
                                                         op0=ALU.add, op1=ALU.max),
             rd=["ml_lp", "ml_carry"], wr=["ml_cum"])
        T.op("dve", lambda: nc.vector.tensor_copy(out=carry[:], in_=cum[:, TB - 1:TB]), rd=["ml_cum"], wr=["ml_carry"])
        T.op("dve", lambda: nc.vector.tensor_scalar(out=NF[:], in0=cum[:], scalar1=-1.0, scalar2=None, op0=ALU.mult),
             rd=["ml_cum"], wr=["ml_NF"])
        for tt in range(4):
            pt = nextbank()
            T.op("pe", lambda: nc.tensor.transpose(out=ps[pt][:, 0:16], in_=gsb[:, tt * 128:(tt + 1) * 128],
                                                   identity=identf[0:16, 0:16]),
                 rd=["ml_gsb", "identf"], wr=[("ps", pt)], inc=False)
            T.op("pe", lambda: nc.tensor.transpose(out=ps[pt][:, 16:32], in_=cum[:, tt * 128:(tt + 1) * 128],
                                                   identity=identf[0:16, 0:16]),
                 rd=["ml_cum", "identf"], wr=[("ps", pt)])
            T.op("act", lambda: nc.scalar.copy(out=tT[:, tt % 2, :], in_=ps[pt][:, 16:32]),
                 rd=[("ps", pt)], wr=[("ml_tT", tt % 2)])
            T.op("dve", lambda: nc.vector.tensor_tensor(out=Acol[:, b * 4 + tt, :], in0=ps[pt][:, 0:8],
                                                        in1=tT[:, tt % 2, 8:16], op=ALU.add),
                 rd=[("ps", pt), ("ml_tT", tt % 2)], wr=[("ml_Acol", b * 4 + tt)])
        proj_fm("ml_w_in", 0, 3088, 4, uT, "uT", TB, qmem_handler)

    def ml_attn(l, b):
        qT, Dm, Wt, hsb, Acol, NF, sel, sigo = (mlst[k] for k in ("qT", "Dm", "Wt", "hsb", "Acol", "NF", "sel", "sigo"))
        kc, vc = mlst["kc"], mlst["vc"]
        it = 0
        nk = 4 * b + 4

        def load(h):
            sl = h % 2
            n = (b + 1) * TB
            if h % 2 == 0:
                T.dma("sp", c_kv, kc[:, (h // 2) % 2, 0:n], kT_d[h // 2, :, 0:n],
                      rd=[("kT_d", h // 2, bb) for bb in range(b + 1)], wr=[("sb_kc", (h // 2) % 2)])
            T.dma("sp", c_kv, vc[:, sl, 0:4 * (b + 1), :], v_d[h, 0:n, :].rearrange("(k p) n -> p k n", p=128),
                  rd=[("v_d", h, bb) for bb in range(b + 1)], wr=[("sb_vc", sl)])
        load(0)
        for h in range(8):
            if h + 1 < 8:
                load(h + 1)
            c, po, sl, ksl = h // 2, (h % 2) * 64, h % 2, (h // 2) % 2
            T.op("pe", lambda: nc.tensor.matmul(ps[6][:], lhsT=sel[:, h * 128:(h + 1) * 128], rhs=NF[:],
                                                start=True, stop=True),
                 rd=["ml_sel", "ml_NF"], wr=[("ps", 6)])
            pN, pD = 5, 7
            for idx, kb in enumerate(range(nk - 1, -1, -1)):
                i = kb - 4 * b
                k2 = it % 2
                it += 1
                pa = nextbank()
                T.op("pe", lambda: nc.tensor.matmul(ps[pa][:], lhsT=kc[po:po + 64, ksl, kb * 128:(kb + 1) * 128],
                                                    rhs=qT[po:po + 64, c, :], start=True, stop=True),
                     rd=[("sb_kc", ksl), ("ml_qT", c)], wr=[("ps", pa)])
                T.op("act", lambda: nc.scalar.activation(out=Dm[:, k2, :], in_=ps[6][:], func=AF.Exp,
                                                         bias=Acol[:, kb, h:h + 1]),
                     rd=[("ps", 6), ("ml_Acol", kb)], wr=[("ml_Dm", k2)])
                T.op("dve", lambda: nc.vector.tensor_tensor(out=Wt[:, k2, :], in0=ps[pa][:], in1=Dm[:, k2, :],
                                                            op=ALU.mult),
                     rd=[("ps", pa), ("ml_Dm", k2)], wr=[("ml_Wt", k2)])
                if i >= 0:
                    m01 = cb[:, CB_MASK01 + i * TB:CB_MASK01 + (i + 1) * TB]
                    T.op("pool", lambda: nc.gpsimd.tensor_tensor(out=Wt[:, k2, :], in0=Wt[:, k2, :], in1=m01,
                                                                 op=ALU.mult),
                         rd=[("ml_Wt", k2), "cb"], wr=[("ml_Wt", k2)])
                T.op("pe", lambda: nc.tensor.matmul(ps[pN][:], lhsT=vc[:, sl, kb, :], rhs=Wt[:, k2, :],
                                                    start=(idx == 0), stop=(idx == nk - 1)),
                     rd=[("sb_vc", sl), ("ml_Wt", k2)], wr=[("ps", pN)], inc=False)
                T.op("pe", lambda: nc.tensor.matmul(ps[pD][:], lhsT=onesb, rhs=Wt[:, k2, :],
                                                    start=(idx == 0), stop=(idx == nk - 1)),
                     rd=["cb", ("ml_Wt", k2)], wr=[("ps", pD)])
            T.op("act", lambda: nc.scalar.activation(out=rden[:], in_=ps[pD][:], func=AF.Square),
                 rd=[("ps", pD)], wr=["rden"])
            T.op("dve", lambda: nc.vector.tensor_scalar(out=rden[:], in0=rden[:], scalar1=1.0, scalar2=None,
                                                        op0=ALU.max),
                 rd=["rden"], wr=["rden"])
            T.op("pool", lambda: nc.gpsimd.tensor_tensor(out=rden[:], in0=rden[:], in1=mhalf[:], op=ALU.pow),
                 rd=["rden", "mhalf"], wr=["rden"])
            T.op("dve", lambda: nc.vector.tensor_tensor(out=hsb[:], in0=ps[pN][:], in1=rden[:], op=ALU.mult),
                 rd=[("ps", pN), "rden"], wr=["ml_hsb"])
            T.op("act", lambda: nc.scalar.activation(out=sq[:, 0, :], in_=hsb[:], func=AF.Square),
                 rd=["ml_hsb"], wr=[("sq", 0)])
            T.op("pe", lambda: nc.tensor.matmul(ps[pD][:], lhsT=onesb, rhs=sq[:, 0, :], start=True, stop=True),
                 rd=["cb", ("sq", 0)], wr=[("ps", pD)])
            T.op("act", lambda: nc.scalar.activation(out=rden[:], in_=ps[pD][:], func=AF.Identity, scale=1.0 / 128,
                                                     bias=epsb[:]),
                 rd=[("ps", pD), "epsb"], wr=["rden"])
            T.op("pool", lambda: nc.gpsimd.tensor_tensor(out=rden[:], in0=rden[:], in1=mhalf[:], op=ALU.pow),
                 rd=["rden", "mhalf"], wr=["rden"])
            T.op("dve", lambda: nc.vector.scalar_tensor_tensor(out=hsb[:], in0=hsb[:], scalar=mlst["gain"][:, 0:1],
                                                               in1=rden[:], op0=ALU.mult, op1=ALU.mult),
                 rd=["ml_hsb", "ml_gain", "rden"], wr=["ml_hsb"])
            slot = w_get([("ml_w_in", 0, 0, 8, 2048 + h * 128, 128)])
            wv = wview(slot, 0, 8, 128)
            pg = nextbank()
            for kcc in range(8):
                T.op("pe", lambda: nc.tensor.matmul(ps[pg][:], lhsT=wv[:, kcc, :], rhs=uT[:, kcc, :],
                                                    start=(kcc == 0), stop=(kcc == 7)),
                     rd=[WK(slot), ("uT", kcc)], wr=[("ps", pg)], inc=(kcc == 7))
            T.op("act", lambda: nc.scalar.activation(out=sigo[:], in_=ps[pg][:], func=AF.Sigmoid),
                 rd=[("ps", pg)], wr=["ml_sigo"])
            T.op("dve", lambda: nc.vector.tensor_tensor(out=catT[:, h, :], in0=hsb[:], in1=sigo[:], op=ALU.mult),
                 rd=["ml_hsb", "ml_sigo"], wr=[("catT", h)])

    gd = {}
    gcw_d = P.dram("gdn_cw", [2, 128, 96], F32, "ExternalInput")
    gdtb_d = P.dram("gdn_dtb", [2, 8, 1], F32, "ExternalInput")
    galog_d = P.dram("gdn_alog", [2, 8, 1], F32, "ExternalInput")
    ggain_d = P.dram("gdn_gain", [2, 128, 1], F32, "ExternalInput")
    sel8_d = P.dram("sel8", [8, 8 * 128], F32, "ExternalInput")
    gmask_d = P.dram("gdn_masks", [128, 3 * TB], BF16, "ExternalInput")
    R3 = ysb_raw.bitcast(BF16)
    R1f = R1.bitcast(F32)

    def gdn_alloc():
        gF, gR2 = LS["gF"], LS["gR2"]
        gFr = gF.bitcast(F32R)
        gd["A"] = gF[:, 0:1024].rearrange("p (s n) -> p s n", s=2)
        gd["AT"] = gF[:, 1024:2048].rearrange("p (s n) -> p s n", s=2)
        gd["X"] = gF[:, 2048:2560]
        gd["Ru"] = gF[:, 2560:3072]
        gd["Rw"] = gF[:, 3072:3584]
        gd["Ar"] = gFr[:, 0:1024].rearrange("p (s n) -> p s n", s=2)
        gd["ATr"] = gFr[:, 1024:2048].rearrange("p (s n) -> p s n", s=2)
        gd["Xr"] = gFr[:, 2048:2560]
        gd["Rur"] = gFr[:, 2560:3072]
        gd["Rwr"] = gFr[:, 3072:3584]
        o1 = 0

        def f1(n, parts=128):
            nonlocal o1
            v = R1f[0:parts, o1:o1 + n]
            o1 += n
            return v
        gd["u"] = f1(512)
        gd["knf"] = f1(512)
        gd["vsf"] = f1(512)
        gd["ra"] = f1(512, 8)
        gd["rb"] = f1(512, 8)
        gd["G"] = f1(512, 8)
        gd["NG"] = f1(512, 8)
        gd["GB"] = f1(512)
        gd["xc"] = f1(516)
        gd["xc2"] = f1(516)
        assert o1 <= 5632
        o = 0

        def f2(n, parts=128):
            nonlocal o
            v = gR2[0:parts, o:o + n]
            o += n
            return v
        gd["sel8"] = f2(1024, 8)
        gd["acc"] = f2(512)
        gd["S"] = f2(1024).rearrange("p (h n) -> p h n", h=8)
        gd["gcol"] = f2(64).rearrange("p (t n) -> p t n", t=4)
        gd["ngcol"] = f2(64).rearrange("p (t n) -> p t n", t=4)
        gd["bgc"] = f2(32).rearrange("p (t n) -> p t n", t=4)
        gd["egc"] = f2(32).rearrange("p (t n) -> p t n", t=4)
        gd["kgc"] = f2(4)
        gd["glc"] = f2(4)
        gd["cw"] = f2(96).rearrange("p (c k) -> p c k", k=4)
        gd["halo"] = f2(72).rearrange("p (c k) -> p c k", k=3)
        gd["gain"] = f2(1)
        gd["dtb"] = f2(1, 8)
        gd["negA"] = f2(1, 8)
        gd["rr"] = rden[:, :]
        assert o <= 2944, o
        o3 = 0

        def f3(n):
            nonlocal o3
            v = R3[:, o3:o3 + n]
            o3 += n
            return v
        for nm in ("qnT", "qgT", "knT", "kbT", "zs", "EG", "Bb", "E1", "E2", "E3", "kg", "wT", "pTg"):
            gd[nm] = f3(512)
        gd["vnb"] = f3(256).rearrange("p (s n) -> p s n", s=2)
        gd["Sb"] = f3(128)
        assert o3 <= 8192

    G_R1KEYS = ["g_u", "g_knf", "g_vsf", "g_ra", "g_rb", "g_G", "g_NG", "g_GB", "g_xc", "g_xc2"]

    def gdn_setup(l):
        jg = l // 3
        T.dma("sp", c_io, gd["cw"].rearrange("p c k -> p (c k)"), gcw_d[jg], wr=["g_cw"])
        T.dma("sp", c_io, gd["dtb"], gdtb_d[jg], wr=["g_dtb"])
        T.dma("sp", c_io, gd["negA"], galog_d[jg], wr=["g_negA"])
        T.dma("sp", c_io, gd["gain"], ggain_d[jg], wr=["g_gain"])
        T.dma("sp", c_io, gd["sel8"], sel8_d, wr=["g_sel8"])
        T.dma("sp", c_io, cb[:, CB_MASK01:CB_MASK01 + 3 * TB], gmask_d, wr=["cb"])
        T.op("act", lambda: nc.scalar.activation(out=gd["negA"], in_=gd["negA"], func=AF.Exp), rd=["g_negA"], wr=["g_negA"])
        T.op("dve", lambda: nc.vector.tensor_scalar(out=gd["negA"], in0=gd["negA"], scalar1=-1.0, scalar2=None,
                                                    op0=ALU.mult), rd=["g_negA"], wr=["g_negA"])
        T.op("pool", lambda: nc.gpsimd.memset(gd["S"].rearrange("p h n -> p (h n)"), 0.0), wr=[("g_S", h) for h in range(8)])

    def gdn_block(l, b):
        jg = l // 3
        g = gd
        M1 = cb[:, CB_MASK01:CB_MASK01 + TB]
        M2 = cb[:, CB_MASK01 + TB:CB_MASK01 + 2 * TB]
        M3 = cb[:, CB_MASK01 + 2 * TB:CB_MASK01 + 3 * TB]
        mixer_keys[:] = G_R1KEYS
        T.alias(mixer_keys, [("aT", j) for j in range(22)])
        ysb_overlay[:] = ["g_qnT", "g_qgT", "g_knT", "g_kbT", "g_zs", "g_EG", "g_Bb", "g_E1", "g_E2", "g_E3", "g_kg", "g_wT", "g_pTg", ("g_vnb", 0), ("g_vnb", 1), "g_Sb"]
        T.alias(ysb_overlay, [("ysb", c) for c in range(8)])
        slot = w_get([("gdn_w_in", jg, 0, 8, 4096, 16)])
        wv = wview(slot, 0, 8, 16)
        pa, pb_ = nextbank(), nextbank()
        for (pbank, c0) in ((pa, 0), (pb_, 8)):
            for kc in range(8):
                T.op("pe", lambda: nc.tensor.matmul(ps[pbank][0:8, :], lhsT=wv[:, kc, c0:c0 + 8], rhs=uT[:, kc, :],
                                                    start=(kc == 0), stop=(kc == 7)),
                     rd=[WK(slot), ("uT", kc)], wr=[("ps", pbank)], inc=(kc == 7))
        T.op("act", lambda: nc.scalar.activation(out=g["ra"], in_=ps[pa][0:8, :], func=AF.Exp, bias=g["dtb"]),
             rd=[("ps", pa), "g_dtb"], wr=["g_ra"])
        T.op("act", lambda: nc.scalar.activation(out=g["ra"], in_=g["ra"], func=AF.Ln, bias=oneb[0:8, :]),
             rd=["g_ra", "oneb"], wr=["g_ra"])
        T.op("dve", lambda: nc.vector.tensor_scalar(out=g["ra"], in0=g["ra"], scalar1=g["negA"], scalar2=None,
                                                    op0=ALU.mult), rd=["g_ra", "g_negA"], wr=["g_ra"])
        T.op("act", lambda: nc.scalar.activation(out=g["rb"], in_=ps[pb_][0:8, :], func=AF.Sigmoid),
             rd=[("ps", pb_)], wr=["g_rb"])
        for n in range(4):
            cs = slice(n * 128, (n + 1) * 128)
            T.op("dve", lambda: nc.vector.tensor_tensor_scan(out=g["G"][:, cs], data0=g["ra"][:, cs], data1=g["ra"][:, cs],
                                                             initial=0.0, op0=ALU.add, op1=ALU.min),
                 rd=["g_ra"], wr=["g_G"])
        T.op("dve", lambda: nc.vector.tensor_scalar(out=g["NG"], in0=g["G"], scalar1=-1.0, scalar2=None, op0=ALU.mult),
             rd=["g_G"], wr=["g_NG"])
        for tt in range(4):
            cs = slice(tt * 128, (tt + 1) * 128)
            pt = nextbank()
            T.op("pe", lambda: nc.tensor.transpose(out=ps[pt][:, 0:8], in_=g["G"][:, cs], identity=identf[0:8, 0:8]),
                 rd=["g_G", "identf"], wr=[("ps", pt)], inc=False)
            T.op("pe", lambda: nc.tensor.transpose(out=ps[pt][:, 8:16], in_=g["rb"][:, cs], identity=identf[0:8, 0:8]),
                 rd=["g_rb", "identf"], wr=[("ps", pt)])
            T.op("act", lambda: nc.scalar.copy(out=g["gcol"][:, tt, :], in_=ps[pt][:, 0:16]),
                 rd=[("ps", pt)], wr=["g_gcol"])
        T.op("dve", lambda: nc.vector.tensor_scalar(out=g["ngcol"], in0=g["gcol"], scalar1=-1.0, scalar2=None,
                                                    op0=ALU.mult), rd=["g_gcol"], wr=["g_ngcol"])
        T.op("act", lambda: nc.scalar.activation(out=g["egc"], in_=g["gcol"][:, :, 0:8], func=AF.Exp),
             rd=["g_gcol"], wr=["g_egc"])
        T.op("dve", lambda: nc.vector.tensor_tensor(out=g["bgc"], in0=g["egc"], in1=g["gcol"][:, :, 8:16], op=ALU.mult),
             rd=["g_egc", "g_gcol"], wr=["g_bgc"])

        def conv_silu(cj, pbank, dst, dkey):
            xc, acc, cw, halo = g["xc"], g["acc"], g["cw"], g["halo"]
            T.op("act", lambda: nc.scalar.copy(out=xc[:, 3:515], in_=ps[pbank][:]), rd=[("ps", pbank)], wr=["g_xc"])
            if b == 0:
                T.op("dve", lambda: nc.vector.memset(xc[:, 0:3], 0.0), wr=["g_xc"])
            else:
                T.op("dve", lambda: nc.vector.tensor_copy(out=xc[:, 0:3], in_=halo[:, cj, :]), rd=[("g_halo", cj)],
                     wr=["g_xc"])
            T.op("dve", lambda: nc.vector.tensor_copy(out=halo[:, cj, :], in_=xc[:, 512:515]), rd=["g_xc"],
                 wr=[("g_halo", cj)])
            T.op("act", lambda: nc.scalar.activation(out=acc, in_=xc[:, 0:512], func=AF.Copy, scale=cw[:, cj, 0:1]),
                 rd=["g_xc", "g_cw"], wr=["g_acc"])
            for k in range(1, 4):
                T.op("dve", lambda: nc.vector.scalar_tensor_tensor(out=acc, in0=xc[:, k:k + 512], scalar=cw[:, cj, k:k + 1],
                                                                   in1=acc, op0=ALU.mult, op1=ALU.add),
                     rd=["g_xc", "g_cw", "g_acc"], wr=["g_acc"])
            T.op("act", lambda: nc.scalar.activation(out=dst, in_=acc, func=AF.Silu), rd=["g_acc"], wr=[dkey])

        def l2_rstd(src, skey):
            T.op("act", lambda: nc.scalar.activation(out=sq[:, 0, :], in_=src, func=AF.Square), rd=[skey], wr=[("sq", 0)])
            T.op("pe", lambda: nc.tensor.matmul(ps[7][:], lhsT=onesb, rhs=sq[:, 0, :], start=True, stop=True),
                 rd=["cb", ("sq", 0)], wr=[("ps", 7)])
            T.op("act", lambda: nc.scalar.activation(out=g["rr"], in_=ps[7][:], func=AF.Sqrt, bias=epsb[:]),
                 rd=[("ps", 7), "epsb"], wr=["rden"])
            T.op("dve", lambda: nc.vector.reciprocal(out=g["rr"], in_=g["rr"]), rd=["rden"], wr=["rden"])

        for h in range(8):
            s8 = g["sel8"][:, h * 128:(h + 1) * 128]
            pG = nextbank()
            T.op("pe", lambda: nc.tensor.matmul(ps[pG][:], lhsT=s8, rhs=g["G"], start=True, stop=True),
                 rd=["g_sel8", "g_G"], wr=[("ps", pG)])
            T.op("act", lambda: nc.scalar.copy(out=g["GB"], in_=ps[pG][:]), rd=[("ps", pG)], wr=["g_GB"])
            T.op("act", lambda: nc.scalar.activation(out=g["EG"], in_=ps[pG][:], func=AF.Exp), rd=[("ps", pG)], wr=["g_EG"])
            pB = nextbank()
            T.op("pe", lambda: nc.tensor.matmul(ps[pB][:], lhsT=s8, rhs=g["rb"], start=True, stop=True),
                 rd=["g_sel8", "g_rb"], wr=[("ps", pB)])
            T.op("act", lambda: nc.scalar.copy(out=g["Bb"], in_=ps[pB][:]), rd=[("ps", pB)], wr=["g_Bb"])
            for (nm, rows, rkey, Mk, colt, off) in (("E1", g["NG"], "g_NG", M1, g["gcol"], 0),
                                                     ("E2", g["G"], "g_G", M2, g["ngcol"], 0),
                                                     ("E3", g["G"], "g_G", M3, g["ngcol"], 0)):
                pe_ = nextbank()
                T.op("pe", lambda: nc.tensor.matmul(ps[pe_][:], lhsT=s8, rhs=rows, start=True, stop=False),
                     rd=["g_sel8", rkey], wr=[("ps", pe_)], inc=False)
                T.op("pe", lambda: nc.tensor.matmul(ps[pe_][:], lhsT=identb, rhs=Mk, start=False, stop=True),
                     rd=["cb"], wr=[("ps", pe_)])
                for n in range(4):
                    cs = slice(n * 128, (n + 1) * 128)
                    T.op("act", lambda: nc.scalar.activation(out=g[nm][:, cs], in_=ps[pe_][:, cs], func=AF.Exp,
                                                             bias=colt[:, n, h:h + 1]),
                         rd=[("ps", pe_), "g_gcol", "g_ngcol"], wr=["g_" + nm])
            for n in range(4):
                T.op("act", lambda: nc.scalar.activation(out=g["kgc"][:, n:n + 1], in_=g["gcol"][:, n, h:h + 1], func=AF.Exp,
                                                         scale=-1.0, bias=g["GB"][:, n * 128 + 127:n * 128 + 128]),
                     rd=["g_gcol", "g_GB"], wr=["g_kgc"])
                T.op("act", lambda: nc.scalar.activation(out=g["glc"][:, n:n + 1],
                                                         in_=g["GB"][:, n * 128 + 127:n * 128 + 128], func=AF.Exp),
                     rd=["g_GB"], wr=["g_glc"])
            slot = w_get([("gdn_w_in", jg, 0, 8, h * 128, 128), ("gdn_w_in", jg, 0, 8, 1024 + h * 128, 128)])
            slot2 = w_get([("gdn_w_in", jg, 0, 8, 2048 + h * 128, 128), ("gdn_w_in", jg, 0, 8, 3072 + h * 128, 128)])
            banks = []
            for (sl_, piece) in ((slot, 0), (slot, 1), (slot2, 0), (slot2, 1)):
                wv4 = WT(sl_).rearrange("p (g k n) -> p g k n", g=2, k=8)
                pbank = nextbank()
                for kc in range(8):
                    T.op("pe", lambda: nc.tensor.matmul(ps[pbank][:], lhsT=wv4[:, piece, kc, :], rhs=uT[:, kc, :],
                                                        start=(kc == 0), stop=(kc == 7)),
                         rd=[WK(sl_), ("uT", kc)], wr=[("ps", pbank)], inc=(kc == 7))
                banks.append(pbank)
            SA = dict(xc=g["xc"], xk="g_xc", acc=g["acc"], ak="g_acc", rr=g["rr"], rk="rden", sqs=0, pb=7, cj=h, bank=banks[0])
            SB_ = dict(xc=g["xc2"], xk="g_xc2", acc=tmpn[:, 0, :], ak=("tmpn", 0), rr=rstd[:, :], rk="rstd", sqs=1, pb=6,
                       cj=8 + h, bank=banks[1])
            cw, halo = g["cw"], g["halo"]

            def st_load(S_):
                T.op("act", lambda: nc.scalar.copy(out=S_["xc"][:, 3:515], in_=ps[S_["bank"]][:]), rd=[("ps", S_["bank"])],
                     wr=[S_["xk"]])
                if b == 0:
                    T.op("dve", lambda: nc.vector.memset(S_["xc"][:, 0:3], 0.0), wr=[S_["xk"]])
                else:
                    T.op("dve", lambda: nc.vector.tensor_copy(out=S_["xc"][:, 0:3], in_=halo[:, S_["cj"], :]),
                         rd=[("g_halo", S_["cj"])], wr=[S_["xk"]])
                T.op("dve", lambda: nc.vector.tensor_copy(out=halo[:, S_["cj"], :], in_=S_["xc"][:, 512:515]), rd=[S_["xk"]],
                     wr=[("g_halo", S_["cj"])])

            def st_tap0(S_):
                T.op("act", lambda: nc.scalar.activation(out=S_["acc"], in_=S_["xc"][:, 0:512], func=AF.Copy,
                                                         scale=cw[:, S_["cj"], 0:1]),
                     rd=[S_["xk"], "g_cw"], wr=[S_["ak"]])

            def st_taps(S_):
                for k in range(1, 4):
                    T.op("dve", lambda: nc.vector.scalar_tensor_tensor(out=S_["acc"], in0=S_["xc"][:, k:k + 512],
                                                                       scalar=cw[:, S_["cj"], k:k + 1], in1=S_["acc"],
                                                                       op0=ALU.mult, op1=ALU.add),
                         rd=[S_["xk"], "g_cw", S_["ak"]], wr=[S_["ak"]])

            def st_silu(S_, dst=None, dkey=None):
                d_ = S_["acc"] if dst is None else dst
                k_ = S_["ak"] if dkey is None else dkey
                T.op("act", lambda: nc.scalar.activation(out=d_, in_=S_["acc"], func=AF.Silu), rd=[S_["ak"]], wr=[k_])

            def st_sq(S_):
                T.op("act", lambda: nc.scalar.activation(out=sq[:, S_["sqs"], :], in_=S_["acc"], func=AF.Square),
                     rd=[S_["ak"]], wr=[("sq", S_["sqs"])])
                T.op("pe", lambda: nc.tensor.matmul(ps[S_["pb"]][:], lhsT=onesb, rhs=sq[:, S_["sqs"], :], start=True, stop=True),
                     rd=["cb", ("sq", S_["sqs"])], wr=[("ps", S_["pb"])])

            def st_sqrt(S_):
                T.op("act", lambda: nc.scalar.activation(out=S_["rr"], in_=ps[S_["pb"]][:], func=AF.Identity, bias=epsb[:]),
                     rd=[("ps", S_["pb"]), "epsb"], wr=[S_["rk"]])

            def st_recip(S_):
                T.op("pool", lambda: nc.gpsimd.tensor_tensor(out=S_["rr"], in0=S_["rr"], in1=mhalf[:], op=ALU.pow),
                     rd=[S_["rk"], "mhalf"], wr=[S_["rk"]])

            for fn_ in (st_load, st_tap0, st_taps, st_silu, st_sq, st_sqrt, st_recip):
                fn_(SA)
                fn_(SB_)
            T.op("dve", lambda: nc.vector.scalar_tensor_tensor(out=g["qnT"], in0=SA["acc"], scalar=128.0 ** -0.5, in1=SA["rr"],
                                                               op0=ALU.mult, op1=ALU.mult),
                 rd=[SA["ak"], SA["rk"]], wr=["g_qnT"])
            T.op("dve", lambda: nc.vector.tensor_tensor(out=g["knf"], in0=SB_["acc"], in1=SB_["rr"], op=ALU.mult),
                 rd=[SB_["ak"], SB_["rk"]], wr=["g_knf"])
            T.op("pool", lambda: nc.gpsimd.tensor_tensor(out=g["qgT"], in0=g["qnT"], in1=g["EG"], op=ALU.mult),
                 rd=["g_qnT", "g_EG"], wr=["g_qgT"])
            T.op("act", lambda: nc.scalar.copy(out=g["knT"], in_=g["knf"]), rd=["g_knf"], wr=["g_knT"])
            T.op("pool", lambda: nc.gpsimd.tensor_tensor(out=g["kbT"], in0=g["knf"], in1=g["Bb"], op=ALU.mult),
                 rd=["g_knf", "g_Bb"], wr=["g_kbT"])
            SV = dict(SA)
            SV["cj"] = 16 + h
            SV["bank"] = banks[2]
            st_load(SV)
            st_tap0(SV)
            st_taps(SV)
            st_silu(SV, g["vsf"], "g_vsf")
            T.op("act", lambda: nc.scalar.activation(out=g["zs"], in_=ps[banks[3]][:], func=AF.Silu),
                 rd=[("ps", banks[3])], wr=["g_zs"])
            pL, pLT = nextbank(), nextbank()
            for n in range(4):
                cs = slice(n * 128, (n + 1) * 128)
                T.op("pe", lambda: nc.tensor.matmul(ps[pL][:, cs], lhsT=g["kbT"][:, cs], rhs=g["knT"][:, cs],
                                                    start=True, stop=True),
                     rd=["g_kbT", "g_knT"], wr=[("ps", pL)], inc=(n == 3))
            for n in range(4):
                cs = slice(n * 128, (n + 1) * 128)
                T.op("pe", lambda: nc.tensor.matmul(ps[pLT][:, cs], lhsT=g["knT"][:, cs], rhs=g["kbT"][:, cs],
                                                    start=True, stop=True),
                     rd=["g_kbT", "g_knT"], wr=[("ps", pLT)], inc=(n == 3))
            T.op("dve", lambda: nc.vector.tensor_tensor(out=g["Ar"][:, 0, :], in0=ps[pL][:], in1=g["E1"], op=ALU.mult),
                 rd=[("ps", pL), "g_E1"], wr=[("g_A", 0)])
            T.op("dve", lambda: nc.vector.tensor_tensor(out=g["ATr"][:, 0, :], in0=ps[pLT][:], in1=g["E2"], op=ALU.mult),
                 rd=[("ps", pLT), "g_E2"], wr=[("g_AT", 0)])
            for n in range(4):
                cs = slice(n * 128, (n + 1) * 128)
                T.op("dve", lambda: nc.vector.tensor_tensor(out=g["Xr"][:, cs], in0=identf[:], in1=g["AT"][:, 0, cs],
                                                            op=ALU.subtract),
                     rd=["identf", ("g_AT", 0)], wr=["g_X"])
            for k in range(1, 7):
                cur, prv = k % 2, (k - 1) % 2
                p1 = nextbank()
                for n in range(4):
                    cs = slice(n * 128, (n + 1) * 128)
                    T.op("pe", lambda: nc.tensor.matmul(ps[p1][:, cs], lhsT=g["ATr"][:, prv, cs], rhs=g["Ar"][:, prv, cs],
                                                        start=True, stop=True),
                         rd=[("g_A", prv), ("g_AT", prv)], wr=[("ps", p1)], inc=(n == 3))
                if k < 6:
                    p2 = nextbank()
                    for n in range(4):
                        cs = slice(n * 128, (n + 1) * 128)
                        T.op("pe", lambda: nc.tensor.matmul(ps[p2][:, cs], lhsT=g["Ar"][:, prv, cs], rhs=g["ATr"][:, prv, cs],
                                                            start=True, stop=True),
                             rd=[("g_A", prv), ("g_AT", prv)], wr=[("ps", p2)], inc=(n == 3))
                T.op("act", lambda: nc.scalar.copy(out=g["Ar"][:, cur, :], in_=ps[p1][:]), rd=[("ps", p1)], wr=[("g_A", cur)])
                if k < 6:
                    T.op("dve", lambda: nc.vector.tensor_copy(out=g["ATr"][:, cur, :], in_=ps[p2][:]), rd=[("ps", p2)],
                         wr=[("g_AT", cur)])
                p3 = nextbank()
                for n in range(4):
                    cs = slice(n * 128, (n + 1) * 128)
                    T.op("pe", lambda: nc.tensor.matmul(ps[p3][:, cs], lhsT=g["Ar"][:, cur, cs], rhs=g["Xr"][:, cs],
                                                        start=True, stop=True),
                         rd=[("g_A", cur), "g_X"], wr=[("ps", p3)], inc=(n == 3))
                T.op("dve", lambda: nc.vector.tensor_tensor(out=g["Xr"], in0=g["X"], in1=ps[p3][:], op=ALU.add),
                     rd=["g_X", ("ps", p3)], wr=["g_X"])
            pk_, pv_ = nextbank(), nextbank()
            for n in range(4):
                cs = slice(n * 128, (n + 1) * 128)
                T.op("pe", lambda: nc.tensor.transpose(out=ps[pk_][:, cs], in_=g["knf"][:, cs], identity=identf[:]),
                     rd=["g_knf", "identf"], wr=[("ps", pk_)], inc=(n == 3))
            for n in range(4):
                cs = slice(n * 128, (n + 1) * 128)
                T.op("pe", lambda: nc.tensor.transpose(out=ps[pv_][:, cs], in_=g["vsf"][:, cs], identity=identf[:]),
                     rd=["g_vsf", "identf"], wr=[("ps", pv_)], inc=(n == 3))
            for n in range(4):
                cs = slice(n * 128, (n + 1) * 128)
                T.op("dve", lambda: nc.vector.tensor_scalar(out=g["Rwr"][:, cs], in0=ps[pk_][:, cs], scalar1=g["bgc"][:, n, h:h + 1],
                                                            scalar2=None, op0=ALU.mult),
                     rd=[("ps", pk_), "g_bgc"], wr=["g_Rw"])
                T.op("dve", lambda: nc.vector.tensor_scalar(out=g["kg"][:, cs], in0=ps[pk_][:, cs], scalar1=g["kgc"][:, n:n + 1],
                                                            scalar2=None, op0=ALU.mult),
                     rd=[("ps", pk_), "g_kgc"], wr=["g_kg"])
                T.op("act", lambda: nc.scalar.activation(out=g["Rur"][:, cs], in_=ps[pv_][:, cs], func=AF.Copy,
                                                         scale=g["gcol"][:, n, 8 + h:9 + h]),
                     rd=[("ps", pv_), "g_gcol"], wr=["g_Ru"])
            pu, pw = nextbank(), nextbank()
            for n in range(4):
                cs = slice(n * 128, (n + 1) * 128)
                T.op("pe", lambda: nc.tensor.matmul(ps[pu][:, cs], lhsT=g["Xr"][:, cs], rhs=g["Rur"][:, cs], start=True, stop=True),
                     rd=["g_X", "g_Ru"], wr=[("ps", pu)], inc=(n == 3))
            for n in range(4):
                cs = slice(n * 128, (n + 1) * 128)
                T.op("pe", lambda: nc.tensor.matmul(ps[pw][:, cs], lhsT=g["Rwr"][:, cs], rhs=g["Xr"][:, cs], start=True, stop=True),
                     rd=["g_X", "g_Rw"], wr=[("ps", pw)], inc=(n == 3))
            T.op("act", lambda: nc.scalar.copy(out=g["u"], in_=ps[pu][:]), rd=[("ps", pu)], wr=["g_u"])
            T.op("dve", lambda: nc.vector.tensor_copy(out=g["wT"], in_=ps[pw][:]), rd=[("ps", pw)], wr=["g_wT"])
            pp = nextbank()
            for n in range(4):
                cs = slice(n * 128, (n + 1) * 128)
                T.op("pe", lambda: nc.tensor.matmul(ps[pp][:, cs], lhsT=g["knT"][:, cs], rhs=g["qnT"][:, cs], start=True, stop=True),
                     rd=["g_knT", "g_qnT"], wr=[("ps", pp)], inc=(n == 3))
            T.op("dve", lambda: nc.vector.tensor_tensor(out=g["pTg"], in0=ps[pp][:], in1=g["E3"], op=ALU.mult),
                 rd=[("ps", pp), "g_E3"], wr=["g_pTg"])
            pO = 5
            T.op("act", lambda: nc.scalar.copy(out=g["Sb"], in_=g["S"][:, h, :]), rd=[("g_S", h)], wr=["g_Sb"])
            for n in range(4):
                cs = slice(n * 128, (n + 1) * 128)
                v2 = n % 2
                pvn = nextbank()
                T.op("pe", lambda: nc.tensor.matmul(ps[pvn][:, 0:128], lhsT=g["wT"][:, cs], rhs=g["Sb"],
                                                    start=True, stop=True),
                     rd=["g_wT", "g_Sb"], wr=[("ps", pvn)])
                T.op("dve", lambda: nc.vector.tensor_tensor(out=g["vnb"][:, v2, :], in0=g["u"][:, cs], in1=ps[pvn][:, 0:128],
                                                            op=ALU.subtract),
                     rd=["g_u", ("ps", pvn)], wr=[("g_vnb", v2)])
                T.op("pe", lambda: nc.tensor.matmul(ps[pO][:, cs], lhsT=g["Sb"], rhs=g["qgT"][:, cs],
                                                    start=True, stop=False),
                     rd=["g_Sb", "g_qgT"], wr=[("ps", pO)], inc=False)
                T.op("pe", lambda: nc.tensor.matmul(ps[pO][:, cs], lhsT=g["vnb"][:, v2, :], rhs=g["pTg"][:, cs],
                                                    start=False, stop=True),
                     rd=[("g_vnb", v2), "g_pTg"], wr=[("ps", pO)])
                pst = nextbank()
                T.op("pe", lambda: nc.tensor.matmul(ps[pst][:, 0:128], lhsT=g["kg"][:, cs], rhs=g["vnb"][:, v2, :],
                                                    start=True, stop=True),
                     rd=["g_kg", ("g_vnb", v2)], wr=[("ps", pst)])
                T.op("dve", lambda: nc.vector.scalar_tensor_tensor(out=g["S"][:, h, :], in0=g["S"][:, h, :],
                                                                   scalar=g["glc"][:, n:n + 1], in1=ps[pst][:, 0:128],
                                                                   op0=ALU.mult, op1=ALU.add),
                     rd=[("g_S", h), "g_glc", ("ps", pst)], wr=[("g_S", h)])
                T.op("act", lambda: nc.scalar.copy(out=g["Sb"], in_=g["S"][:, h, :]), rd=[("g_S", h)],
                     wr=["g_Sb"])
            T.op("act", lambda: nc.scalar.activation(out=sq[:, 0, :], in_=ps[pO][:], func=AF.Square), rd=[("ps", pO)],
                 wr=[("sq", 0)])
            T.op("pe", lambda: nc.tensor.matmul(ps[7][:], lhsT=onesb, rhs=sq[:, 0, :], start=True, stop=True),
                 rd=["cb", ("sq", 0)], wr=[("ps", 7)])
            T.op("act", lambda: nc.scalar.activation(out=g["rr"], in_=ps[7][:], func=AF.Identity, scale=1.0 / 128, bias=epsb[:]),
                 rd=[("ps", 7), "epsb"], wr=["rden"])
            T.op("pool", lambda: nc.gpsimd.tensor_tensor(out=g["rr"], in0=g["rr"], in1=mhalf[:], op=ALU.pow),
                 rd=["rden", "mhalf"], wr=["rden"])
            T.op("dve", lambda: nc.vector.scalar_tensor_tensor(out=g["acc"], in0=ps[pO][:], scalar=g["gain"], in1=g["rr"],
                                                               op0=ALU.mult, op1=ALU.mult),
                 rd=[("ps", pO), "g_gain", "rden"], wr=["g_acc"])
            T.op("dve", lambda: nc.vector.tensor_tensor(out=catT[:, h, :], in0=g["acc"], in1=g["zs"], op=ALU.mult),
                 rd=["g_acc", "g_zs"], wr=[("catT", h)])
        proj_fm("gdn_w_in", jg, 4112, 4, uT, "uT", TB, qmem_handler)

    def emit():
        T.dma("sp", c_io, identf[:], identf_d, wr=["identf"])
        T.dma("sp", c_io, gains[:], gains_d, wr=["gains"])
        T.dma("sp", c_io, cb[:], cb_d, wr=["cb"])
        T.op("pool", lambda: nc.gpsimd.memset(epsb[:], EPS), wr=["epsb"])
        T.op("pool", lambda: nc.gpsimd.memset(oneb[:], 1.0), wr=["oneb"])
        T.op("pool", lambda: nc.gpsimd.memset(mhalf[:], -0.5), wr=["mhalf"])
        prep_mem()
        for t in range(16):
            sl = t % 2
            T.dma("sp", c_io, xin(sl), x_d[t * 128:(t + 1) * 128, :], wr=xink(sl))
            for half in range(2):
                pbn = nextbank()
                pk = ("ps", pbn)
                for q in range(4):
                    c = half * 4 + q
                    T.op("pe", lambda: nc.tensor.transpose(out=ps[pbn][:, q * 128:(q + 1) * 128],
                                                           in_=xin(sl)[:, c * 128:(c + 1) * 128],
                                                           identity=identf[:]),
                         rd=xink(sl) + ["identf"], wr=[pk], inc=(q == 3))
                dst = hT[:, half * 4:(half + 1) * 4, t * 128:(t + 1) * 128]
                src = ps[pbn][:].rearrange("p (q n) -> p q n", q=4)
                wr = [("hT", c, t // 4) for c in range(half * 4, half * 4 + 4)]
                if half == 0:
                    T.op("act", lambda: nc.scalar.copy(out=dst, in_=src), rd=[pk], wr=wr)
                else:
                    T.op("dve", lambda: nc.vector.tensor_copy(out=dst, in_=src), rd=[pk], wr=wr)
        for l in layers:
            kind = l % 3
            T.barrier()
            layer_alloc(kind)
            w_block(l, None)
            mem_kv(l)
            for b in range(NB):
                w_block(l, b)
                pre_norm(l, 0, b)
                if kind == 2:
                    if b == 0:
                        sb_setup()
                    sb_inproj(l, b)
                    sb_attn(b)
                elif kind == 1:
                    if b == 0:
                        ml_setup()
                    ml_inproj(l, b)
                    ml_attn(l, b)
                else:
                    if b == 0:
                        gdn_setup(l)
                    gdn_block(l, b)
                mem_attn(b)
                out_proj(l, b)
                ffn(l, b)
            T.barrier()
            layer_free()
        for t in range(16):
            sl = t % 2
            for half in range(2):
                pbn = nextbank()
                pk = ("ps", pbn)
                for q in range(4):
                    c = half * 4 + q
                    T.op("pe", lambda: nc.tensor.transpose(out=ps[pbn][:, q * 128:(q + 1) * 128],
                                                           in_=hT[:, c, t * 128:(t + 1) * 128],
                                                           identity=identf[:]),
                         rd=[("hT", c, t // 4), "identf"], wr=[pk], inc=(q == 3))
                dst = xin(sl)[:, half * 512:(half + 1) * 512]
                if half == 0:
                    T.op("act", lambda: nc.scalar.copy(out=dst, in_=ps[pbn][:]), rd=[pk], wr=[("ysb", 2 * sl + half)])
                else:
                    T.op("dve", lambda: nc.vector.tensor_copy(out=dst, in_=ps[pbn][:]), rd=[pk],
                         wr=[("ysb", 2 * sl + half)])
            T.dma("sp", c_io, out_d[t * 128:(t + 1) * 128, :], xin(sl), rd=xink(sl), wr=[("out", t)])
        T.wait_key("sp", [("out", t) for t in range(16)])

    T.dry = True
    emit()
    T.dry = False
    ring["i"] = 0
    wstate["nf32"] = 0
    wstate["nb16"] = 0
    wstate["slot_of"] = {}
    assert max(x[2] for x in wplan) < NWC
    emit()
    P.finish()
    return nc, T


CB_ONES = 0
CB_NEGONES = 128
CB_IDENT = 256
CB_NEGMINCL = 384
CB_MASK01 = 512
NCB = CB_MASK01 + 4 * TB
G_MEM = DEPTH * 4 * 8
NG = G_MEM + 8


def host_consts(inputs):
    g = np.stack([inputs["norm_pre_mix"], inputs["norm_post_mix"], inputs["norm_pre_ffn"],
                  inputs["norm_post_ffn"]], axis=1)
    g = g.reshape(DEPTH, 4, 8, 128).transpose(3, 0, 1, 2).reshape(128, DEPTH * 4 * 8)
    gm = inputs["mem_norm"].reshape(8, 128).T
    gains = np.concatenate([g, gm], axis=1).astype(np.float32)
    cbf = np.zeros((128, NCB), np.float32)
    cbf[:, CB_ONES:CB_ONES + 128] = 1.0
    cbf[:, CB_NEGONES:CB_NEGONES + 128] = -1.0
    cbf[:, CB_IDENT:CB_IDENT + 128] = np.eye(128)
    jj, ss = np.meshgrid(np.arange(128), np.arange(128), indexing="ij")
    cbf[:, CB_NEGMINCL:CB_NEGMINCL + 128] = -(jj >= ss).astype(np.float32)
    sidx = np.arange(128)[:, None]
    tidx = np.arange(TB)[None, :]
    for i in range(4):
        m = ((sidx + 128 * i) < tidx).astype(np.float32)
        cbf[:, CB_MASK01 + i * TB:CB_MASK01 + (i + 1) * TB] = m
    sel = np.zeros((16, 8, 128), np.float32)
    for h in range(8):
        sel[8 + h, h, :] = 1.0
    m01i = np.zeros((128, 4 * TB), np.float32)
    for i in range(4):
        m01i[:, i * TB:(i + 1) * TB] = ((sidx + 128 * i) <= tidx)
    mlbias = np.concatenate([inputs["ml_i_bias"][0], inputs["ml_f_bias"][0]]).reshape(16, 1).astype(np.float32)
    sel8 = np.zeros((8, 8, 128), np.float32)
    for h in range(8):
        sel8[h, h, :] = 1.0
    pidx = np.arange(128)[:, None]
    fidx = np.arange(128)[None, :]
    gm = np.concatenate([np.tile(np.where(fidx < pidx, 0.0, NEGBIG), (1, 4)),
                         np.tile(np.where(pidx < fidx, 0.0, NEGBIG), (1, 4)),
                         np.tile(np.where(pidx <= fidx, 0.0, NEGBIG), (1, 4))], axis=1).astype(np.float32)
    gcw = inputs["gdn_conv"].reshape(2, 4, 24, 128).transpose(0, 3, 2, 1).reshape(2, 128, 96)
    return {
        "sel8": sel8.reshape(8, 8 * 128),
        "gdn_masks": gm.astype(ml_dtypes.bfloat16),
        "gdn_cw": np.ascontiguousarray(gcw.astype(np.float32)),
        "gdn_dtb": np.ascontiguousarray(inputs["gdn_dt_bias"].reshape(2, 8, 1).astype(np.float32)),
        "gdn_alog": np.ascontiguousarray(inputs["gdn_a_log"].reshape(2, 8, 1).astype(np.float32)),
        "gdn_gain": np.ascontiguousarray(inputs["gdn_out_norm"].reshape(2, 128, 1).astype(np.float32)),
        "selc": sel.reshape(16, 8 * 128),
        "mlbias": mlbias,
        "mlgain": np.ascontiguousarray(inputs["ml_out_norm"][0].reshape(128, 1).astype(np.float32)),
        "mask01incl": m01i.astype(ml_dtypes.bfloat16),
        "identf": np.eye(128, dtype=np.float32),
        "gains": np.ascontiguousarray(gains),
        "cbf": cbf.astype(ml_dtypes.bfloat16),
    }


WNAMES = ["w_ffn_in", "w_ffn_out", "w_mem_kv", "w_out", "gdn_w_in", "ml_w_in", "sb_w_in"]


def make_in_maps(inputs, cores, x_override=None):
    consts = host_consts(inputs)
    in_maps = []
    for core in cores:
        m = dict(consts)
        m["x"] = np.ascontiguousarray(inputs["x"][core] if x_override is None else x_override[core])
        m["mem"] = np.ascontiguousarray(inputs["mem"][core])
        for w in WNAMES:
            m[w] = inputs[w]
        in_maps.append(m)
    return in_maps


def kernel(**inputs):
    inputs = {k: np.asarray(v) for k, v in inputs.items()}
    nc, _ = build(list(range(DEPTH)))
    in_maps = make_in_maps(inputs, list(range(8)))
    res = run_bass_kernel_spmd(nc, in_maps, core_ids=list(range(8)))
    return np.stack([r["out"] for r in res.results], axis=0)
```

```python
import numpy as np
import ml_dtypes
import concourse.bass as bass
import concourse.mybir as mybir
from concourse.bass_utils import run_bass_kernel_spmd

F32 = mybir.dt.float32
BF16 = mybir.dt.bfloat16
F32R = mybir.dt.float32r
AF = mybir.ActivationFunctionType
ALU = mybir.AluOpType

S = 2048
D = 1024
NB = 4
TB = 512
DEPTH = 4
N_MEM = 256
D_FF = 2816
EPS = 1e-6
SEM_LIMIT = 30000


class Counter:
    def __init__(self, tr, name, inorder):
        self.tr = tr
        self.name = name
        self.inorder = inorder
        self.sems = []
        self.final = {}
        self.epoch = -1
        self.val = 0
        self._new_epoch()

    def _new_epoch(self):
        if self.epoch >= 0:
            self.final[self.epoch] = self.val
        self.epoch += 1
        self.val = 0
        self.sems.append(self.tr.new_sem(f"{self.name}_{self.epoch}"))

    def reserve(self, amount):
        if self.val + amount > SEM_LIMIT:
            self._new_epoch()
        self.val += amount
        return (self.epoch, self.val)

    def peek_next(self, amount=1):
        if self.val + amount > SEM_LIMIT:
            return (self.epoch + 1, amount)
        return (self.epoch, self.val + amount)


class Tracker:
    def __init__(self, nc):
        self.nc = nc
        self._sem_stack = []
        self.eng = {"pe": nc.tensor, "act": nc.scalar, "dve": nc.vector, "pool": nc.gpsimd, "sp": nc.sync}
        self.cnt = {k: Counter(self, k, True) for k in self.eng}
        self.waited = {k: {} for k in self.eng}
        self.last_w = {}
        self.readers = {}
        self.pending_noinc = {k: False for k in self.eng}
        self.n_inst = {k: 0 for k in self.eng}
        self.dry = False

    def new_sem(self, name):
        cm = self.nc.semaphore(name)
        h = cm.__enter__()
        self._sem_stack.append(cm)
        return h

    def new_counter(self, name):
        c = Counter(self, name, False)
        self.cnt[name] = c
        return c

    def _need(self, deps, cnt, tok):
        k = (cnt.name, tok[0])
        if k not in deps or deps[k][1] < tok[1]:
            deps[k] = (cnt, tok[1])

    def _wait_all(self, e, rd, wr):
        deps = {}
        for r in rd:
            lw = self.last_w.get(r)
            if lw is not None:
                self._need(deps, lw[0], lw[1])
        for w in wr:
            lw = self.last_w.get(w)
            if lw is not None:
                self._need(deps, lw[0], lw[1])
            for (c, tok) in self.readers.get(w, {}).values():
                self._need(deps, c, tok)
        wd = self.waited[e]
        for (cname, ep), (cnt, val) in deps.items():
            if cname == e and e == "pe":
                continue
            if wd.get((cname, ep), 0) >= val:
                continue
            if cnt.inorder:
                if any(k[0] == cname and k[1] > ep for k in wd):
                    continue
            self.eng[e].wait_ge(cnt.sems[ep], val)
            wd[(cname, ep)] = val

    def _record(self, cnt, tok, rd, wr):
        for r in rd:
            self.readers.setdefault(r, {})[cnt.name] = (cnt, tok)
        for w in wr:
            self.last_w[w] = (cnt, tok)
            self.readers[w] = {}

    def op(self, e, fn, rd=(), wr=(), inc=True):
        if self.dry:
            return None
        self._wait_all(e, rd, wr)
        inst = fn()
        self.n_inst[e] += 1
        cnt = self.cnt[e]
        if inc:
            tok = cnt.reserve(1)
            inst.then_inc(cnt.sems[tok[0]], 1)
        else:
            tok = cnt.peek_next(1)
        self._record(cnt, tok, rd, wr)
        return inst

    def dma(self, q, cnt, out, in_, rd=(), wr=()):
        if self.dry:
            return None
        self._wait_all(q, rd, wr)
        inst = self.eng[q].dma_start(out=out, in_=in_)
        tok = cnt.reserve(16)
        inst.then_inc(cnt.sems[tok[0]], 16)
        self.n_inst[q] += 1
        self._record(cnt, tok, rd, wr)
        return inst

    def alias(self, dst_keys, src_keys):
        if self.dry:
            return
        acc = {}
        for s in src_keys:
            lw = self.last_w.get(s)
            items = list(self.readers.get(s, {}).values())
            if lw is not None:
                items.append(lw)
            for (c, tok) in items:
                k = (c.name, tok[0])
                if k not in acc or acc[k][1][1] < tok[1]:
                    acc[k] = (c, tok)
        for d in dst_keys:
            rd = self.readers.setdefault(d, {})
            for (cname, ep), (c, tok) in acc.items():
                nm = f"{cname}@{ep}"
                if nm not in rd or rd[nm][1][1] < tok[1]:
                    rd[nm] = (c, tok)

    def barrier(self):
        if self.dry:
            return
        for e in self.eng:
            wd = self.waited[e]
            for c in self.cnt.values():
                eps = range(c.epoch + 1) if not c.inorder else [c.epoch]
                for ep in eps:
                    val = c.val if ep == c.epoch else c.final[ep]
                    if val <= 0 or wd.get((c.name, ep), 0) >= val:
                        continue
                    if c.name == e and e == "pe":
                        continue
                    self.eng[e].wait_ge(c.sems[ep], val)
                    wd[(c.name, ep)] = val

    def wait_key(self, e, keys):
        if self.dry:
            return
        self._wait_all(e, keys, ())

    def close(self):
        for cm in reversed(self._sem_stack):
            cm.__exit__(None, None, None)


class Prog:
    def __init__(self, layers, load_x=True):
        self.layers = layers
        self.nc = bass.Bass("TRN2", target_bir_lowering=False)
        self._stack = []
        self.T = Tracker(self.nc)

    def dram(self, name, shape, dt, kind):
        return self.nc.dram_tensor(name, list(shape), dt, kind=kind).ap()

    def sb(self, name, shape, dt):
        cm = self.nc.sbuf_tensor(name, list(shape), dt)
        t = cm.__enter__()
        self._stack.append(cm)
        return t

    def psum(self, name, shape, dt):
        cm = self.nc.psum_tensor(name, list(shape), dt)
        t = cm.__enter__()
        self._stack.append(cm)
        return t

    def finish(self):
        for cm in reversed(self._stack):
            cm.__exit__(None, None, None)
        self.T.close()


GDN_IN = 4624
ML_IN = 3600
SB_IN = 3584
NWS = 3
NWST = 2
NEGBIG = -30000.0


def build(layers, first=True, last=True):
    P = Prog(layers)
    nc, T = P.nc, P.T
    kinds = [l % 3 for l in layers]
    x_d = P.dram("x", [S, D], F32, "ExternalInput")
    out_d = P.dram("out", [S, D], F32, "ExternalOutput")
    mem_d = P.dram("mem", [N_MEM, D], F32, "ExternalInput")
    identf_d = P.dram("identf", [128, 128], F32, "ExternalInput")
    cb_d = P.dram("cbf", [128, NCB], BF16, "ExternalInput")
    gains_d = P.dram("gains", [128, NG], F32, "ExternalInput")
    W = {
        "w_ffn_in": P.dram("w_ffn_in", [DEPTH, D, 2 * D_FF], F32, "ExternalInput"),
        "w_ffn_out": P.dram("w_ffn_out", [DEPTH, D_FF, D], F32, "ExternalInput"),
        "w_mem_kv": P.dram("w_mem_kv", [DEPTH, D, 1024], F32, "ExternalInput"),
        "w_out": P.dram("w_out", [DEPTH, 1536, D], F32, "ExternalInput"),
        "gdn_w_in": P.dram("gdn_w_in", [2, D, GDN_IN], F32, "ExternalInput"),
        "ml_w_in": P.dram("ml_w_in", [1, D, ML_IN], F32, "ExternalInput"),
        "sb_w_in": P.dram("sb_w_in", [1, D, SB_IN], F32, "ExternalInput"),
    }

    hT = P.sb("hT", [128, 8, S], F32)
    uT = P.sb("uT", [128, 8, TB], BF16)
    sq = P.sb("sq", [128, 2, TB], BF16)
    rstd = P.sb("rstd", [128, TB], F32)
    ysb_raw = P.sb("ysb", [128, 8 * TB], F32)
    ysb = ysb_raw[:, :].rearrange("p (c n) -> p c n", c=8)
    R1 = P.sb("R1", [128, 22 * TB], BF16)
    aT = R1[:, :].rearrange("p (j n) -> p j n", j=22)
    sg = P.sb("sg", [128, 1, TB], BF16)
    tmpn = P.sb("tmpn", [128, 1, TB], F32)
    LS = {}
    identf = P.sb("identf_sb", [128, 128], F32)
    cb = P.sb("cb_sb", [128, NCB], BF16)
    gains = P.sb("gains_sb", [128, NG], F32)
    wst = P.sb("wst", [128, NWST, 2048], F32)
    wbf = P.sb("wbf", [128, NWS, 2048], BF16)
    catT = P.sb("catT", [128, 12, TB], BF16)
    qmT = P.sb("qmT", [128, 4, TB], BF16)
    memnT = P.sb("memnT", [128, 8, N_MEM], BF16)
    memkT = P.sb("memkT", [128, 4, N_MEM], BF16)
    memv = P.sb("memv", [128, 2, 512], BF16)
    negkmax = P.sb("negkmax", [128, 4], F32)
    cq = P.sb("cq", [1, TB], F32)
    negc = P.sb("negc", [1, TB], BF16)
    pT = P.sb("pT", [128, 2, TB], BF16)
    rden = P.sb("rden", [128, TB], F32)
    epsb = P.sb("epsb", [128, 1], F32)
    oneb = P.sb("oneb", [128, 1], F32)
    ps = [P.psum(f"ps{i}", [128, TB], F32) for i in range(8)]

    onesb = cb[:, CB_ONES:CB_ONES + 128]
    negones = cb[:, CB_NEGONES:CB_NEGONES + 128]
    identb = cb[:, CB_IDENT:CB_IDENT + 128]
    negMincl = cb[:, CB_NEGMINCL:CB_NEGMINCL + 128]

    c_io = T.new_counter("io")
    c_w = [T.new_counter(f"w{i}") for i in range(NWST)]

    def gain(l, which, c):
        i = (l * 4 + which) * 8 + c
        return gains[:, i:i + 1]

    def xin(sl):
        return ysb[:, 2 * sl:2 * sl + 2, :].rearrange("p a b -> p (a b)")

    def xink(sl):
        return [("ysb", 2 * sl), ("ysb", 2 * sl + 1)]

    ring = {"i": 0}

    def nextbank():
        ring["i"] = (ring["i"] + 1) % 5
        return ring["i"]

    wplan = []
    wstate = {"issued": 0, "next": 0, "lb": None, "idx": 0}
    NWC = 72
    wcache_d = P.dram("wcache", [NWC, 128, 2048], BF16, "Internal")
    c_wb = [T.new_counter(f"wb{i}") for i in range(NWS + 2 * NWST)]
    c_wc = T.new_counter("wc")

    wst_b = wst.bitcast(BF16)
    NSLOT = NWS + 2 * NWST

    def WT(slot):
        if slot < NWS:
            return wbf[:, slot, :]
        j = slot - NWS
        return wst_b[:, j // 2, (j % 2) * 2048:(j % 2 + 1) * 2048]

    def WK(slot):
        if slot < NWS:
            return ("wbf", slot)
        j = slot - NWS
        return ("wst", j // 2, (j % 2) * 1024)

    def w_issue(i):
        req, lb, idx = wplan[i]
        n = sum(kc * ncols for (_, _, _, kc, _, ncols) in req)
        if lb is not None and lb[1] > 0:
            slot = wstate["nb16"] % NSLOT
            wstate["nb16"] += 1
            wstate["slot_of"][i] = slot
            T.dma("sp", c_wb[slot], WT(slot)[:, 0:n], wcache_d[idx, :, 0:n], rd=[("wc", idx)], wr=[WK(slot)])
            return
        slot = wstate["nf32"] % NWS
        st = wstate["nf32"] % NWST
        wstate["nf32"] += 1
        wstate["slot_of"][i] = slot
        off = 0
        for pi, (name, l, r0, kc, c0, ncols) in enumerate(req):
            dst = wst[:, st, off:off + kc * ncols].rearrange("p (k n) -> p k n", k=kc)
            src = W[name][l, r0:r0 + kc * 128, c0:c0 + ncols].rearrange("(k p) n -> p k n", p=128)
            if len(req) == 1:
                wk = [("wst", st, 0), ("wst", st, 1024)]
            else:
                assert kc * ncols <= 1024 and off == pi * 1024
                wk = [("wst", st, pi * 1024)]
            T.dma("sp", c_w[st], dst, src, wr=wk)
            off += kc * ncols
        T.op("act", lambda: nc.scalar.copy(out=wbf[:, slot, 0:n], in_=wst[:, st, 0:n]),
             rd=[("wst", st, 0), ("wst", st, 1024)], wr=[("wbf", slot)])
        if lb is not None:
            T.dma("pool", c_wc, wcache_d[idx, :, 0:n], wbf[:, slot, 0:n], rd=[("wbf", slot)], wr=[("wc", idx)])

    def w_get(req):
        req = tuple(req)
        if T.dry:
            wplan.append((req, wstate["lb"], wstate["idx"]))
            wstate["idx"] += 1
            return 0
        i = wstate["next"]
        assert wplan[i][0] == req, (wplan[i], req)

        def depth(k):
            lb = wplan[k][1]
            return (NSLOT - 1) if (lb is not None and lb[1] > 0) else (NWS - 1)
        def isb16(k):
            lb = wplan[k][1]
            return lb is not None and lb[1] > 0
        while wstate["issued"] < len(wplan):
            k = wstate["issued"]
            if k > i + depth(k) - 1:
                break
            if k > i and isb16(k) != isb16(i):
                break
            w_issue(k)
            wstate["issued"] += 1
        wstate["next"] += 1
        return wstate["slot_of"][i]

    def w_block(l, b):
        wstate["lb"] = (l, b) if b is not None else None
        wstate["idx"] = 0

    def wview(slot, off, kc, ncols):
        return WT(slot)[:, off:off + kc * ncols].rearrange("p (k n) -> p k n", k=kc)

    lcount = {"n": 0}

    def layer_alloc(kind):
        lcount["n"] += 1
        tag = lcount["n"]
        cms = []

        def mk(name, shape, dt):
            cm = nc.sbuf_tensor(f"{name}_{tag}", list(shape), dt)
            t = cm.__enter__()
            cms.append(cm)
            return t
        if kind == 0:
            LS["gF"] = mk("gF", [128, 3584], F32)
            LS["gR2"] = mk("gR2", [128, 2944], F32)
            gdn_alloc()
        else:
            LS["R2"] = mk("R2", [128, 12544], BF16)
            LS["R2f"] = LS["R2"].bitcast(F32)
            if kind == 2:
                sb_alloc()
            else:
                ml_alloc()
        LS["cms"] = cms

    def layer_free():
        for cm in reversed(LS["cms"]):
            cm.__exit__(None, None, None)
        LS["cms"] = []

    def rms_stats(src_fn, src_keys, n):
        pk = ("ps", 7)
        for c in range(8):
            k = c % 2
            T.op("act", lambda: nc.scalar.activation(out=sq[:, k, 0:n], in_=src_fn(c), func=AF.Square),
                 rd=[src_keys(c)], wr=[("sq", k)])
            T.op("pe", lambda: nc.tensor.matmul(ps[7][:, 0:n], lhsT=onesb, rhs=sq[:, k, 0:n],
                                                start=(c == 0), stop=(c == 7)),
                 rd=["cb", ("sq", k)], wr=[pk])
        T.op("act", lambda: nc.scalar.activation(out=rstd[:, 0:n], in_=ps[7][:, 0:n], func=AF.Sqrt,
                                                 scale=1.0 / D, bias=epsb[:]),
             rd=[pk, "epsb"], wr=["rstd"])
        T.op("dve", lambda: nc.vector.reciprocal(out=rstd[:, 0:n], in_=rstd[:, 0:n]), rd=["rstd"], wr=["rstd"])

    def pre_norm(l, which, b):
        rms_stats(lambda c: hT[:, c, b * TB:(b + 1) * TB], lambda c: ("hT", c, b), TB)
        for c in range(8):
            T.op("dve", lambda: nc.vector.scalar_tensor_tensor(
                out=uT[:, c, :], in0=hT[:, c, b * TB:(b + 1) * TB], scalar=gain(l, which, c),
                in1=rstd[:], op0=ALU.mult, op1=ALU.mult),
                rd=[("hT", c, b), "gains", "rstd"], wr=[("uT", c)])

    def post_norm_add(l, which, b):
        rms_stats(lambda c: ysb[:, c, :], lambda c: ("ysb", c), TB)
        for c in range(8):
            k = 0
            T.op("dve", lambda: nc.vector.scalar_tensor_tensor(
                out=tmpn[:, k, :], in0=ysb[:, c, :], scalar=gain(l, which, c),
                in1=rstd[:], op0=ALU.mult, op1=ALU.mult),
                rd=[("ysb", c), "gains", "rstd"], wr=[("tmpn", k)])
            T.op("pool", lambda: nc.gpsimd.tensor_tensor(
                out=hT[:, c, b * TB:(b + 1) * TB], in0=hT[:, c, b * TB:(b + 1) * TB],
                in1=tmpn[:, k, :], op=ALU.add),
                rd=[("hT", c, b), ("tmpn", k)], wr=[("hT", c, b)])

    def proj_fm(wname, wl, c0, nchunks, xT, xkey, n, handler):
        j = 0
        while j < nchunks:
            nn = min(2, nchunks - j)
            slot = w_get([(wname, wl, 0, 8, c0 + j * 128, nn * 128)])
            wv = wview(slot, 0, 8, nn * 128)
            for jj in range(nn):
                pb = nextbank()
                for kc in range(8):
                    T.op("pe", lambda: nc.tensor.matmul(ps[pb][:, 0:n], lhsT=wv[:, kc, jj * 128:(jj + 1) * 128],
                                                        rhs=xT[:, kc, 0:n], start=(kc == 0), stop=(kc == 7)),
                         rd=[WK(slot), (xkey, kc)], wr=[("ps", pb)], inc=(kc == 7))
                handler(j + jj, pb)
            j += nn

    def proj_tm(wname, wl, c0, ncols, xT, xkey, ntt, handler):
        slot = w_get([(wname, wl, 0, 8, c0, ncols)])
        wv = wview(slot, 0, 8, ncols)
        for tt in range(ntt):
            pb = nextbank()
            for kc in range(8):
                T.op("pe", lambda: nc.tensor.matmul(ps[pb][:, 0:ncols], lhsT=xT[:, kc, tt * 128:(tt + 1) * 128],
                                                    rhs=wv[:, kc, :], start=(kc == 0), stop=(kc == 7)),
                     rd=[WK(slot), (xkey, kc)], wr=[("ps", pb)], inc=(kc == 7))
            handler(tt, pb)

    mixer_keys = []
    ysb_overlay = []

    def ffn(l, b):
        T.alias([("aT", j) for j in range(22)], mixer_keys)
        pre_norm(l, 2, b)
        for j in range(22):
            slot = w_get([("w_ffn_in", l, 0, 8, j * 128, 128), ("w_ffn_in", l, 0, 8, D_FF + j * 128, 128)])
            wv = WT(slot).rearrange("p (g k n) -> p g k n", g=2, k=8)
            pg, pu = nextbank(), nextbank()
            for gi, pbank in ((0, pg), (1, pu)):
                for kc in range(8):
                    T.op("pe", lambda: nc.tensor.matmul(ps[pbank][:], lhsT=wv[:, gi, kc, :], rhs=uT[:, kc, :],
                                                        start=(kc == 0), stop=(kc == 7)),
                         rd=[WK(slot), ("uT", kc)], wr=[("ps", pbank)], inc=(kc == 7))
            k = 0
            T.op("act", lambda: nc.scalar.activation(out=sg[:, k, :], in_=ps[pg][:], func=AF.Silu),
                 rd=[("ps", pg)], wr=[("sg", k)])
            T.op("dve", lambda: nc.vector.tensor_tensor(out=aT[:, j, :], in0=sg[:, k, :], in1=ps[pu][:],
                                                        op=ALU.mult),
                 rd=[("sg", k), ("ps", pu)], wr=[("aT", j)])
        for c in range(8):
            pbank = nextbank()
            for hh in range(2):
                slot = w_get([("w_ffn_out", l, hh * 1408, 11, c * 128, 128)])
                wv = wview(slot, 0, 11, 128)
                for kc in range(11):
                    kk = hh * 11 + kc
                    T.op("pe", lambda: nc.tensor.matmul(ps[pbank][:], lhsT=wv[:, kc, :], rhs=aT[:, kk, :],
                                                        start=(kk == 0), stop=(kk == 21)),
                         rd=[WK(slot), ("aT", kk)], wr=[("ps", pbank)], inc=(kc == 10))
            T.op("act", lambda: nc.scalar.copy(out=ysb[:, c, :], in_=ps[pbank][:]),
                 rd=[("ps", pbank)], wr=[("ysb", c)])
        post_norm_add(l, 3, b)

    def out_proj(l, b):
        T.alias([("ysb", c) for c in range(8)], ysb_overlay)
        for c in range(8):
            slot = w_get([("w_out", l, 0, 12, c * 128, 128)])
            wv = wview(slot, 0, 12, 128)
            pbank = nextbank()
            for kc in range(12):
                T.op("pe", lambda: nc.tensor.matmul(ps[pbank][:], lhsT=wv[:, kc, :], rhs=catT[:, kc, :],
                                                    start=(kc == 0), stop=(kc == 11)),
                     rd=[WK(slot), ("catT", kc)], wr=[("ps", pbank)], inc=(kc == 11))
            T.op("act", lambda: nc.scalar.copy(out=ysb[:, c, :], in_=ps[pbank][:]),
                 rd=[("ps", pbank)], wr=[("ysb", c)])
        post_norm_add(l, 1, b)

    def prep_mem():
        for t in range(2):
            sl = t % 2
            T.dma("sp", c_io, xin(sl), mem_d[t * 128:(t + 1) * 128, :], wr=xink(sl))
            for half in range(2):
                pb = nextbank()
                pk = ("ps", pb)
                for q in range(4):
                    c = half * 4 + q
                    T.op("pe", lambda: nc.tensor.transpose(out=ps[pb][:, q * 128:(q + 1) * 128],
                                                           in_=xin(sl)[:, c * 128:(c + 1) * 128],
                                                           identity=identf[:]),
                         rd=xink(sl) + ["identf"], wr=[pk], inc=(q == 3))
                T.op("dve", lambda: nc.vector.tensor_copy(
                    out=hT[:, half * 4:(half + 1) * 4, t * 128:(t + 1) * 128],
                    in_=ps[pb][:].rearrange("p (q n) -> p q n", q=4)), rd=[pk],
                    wr=[("hT", c, 0) for c in range(half * 4, half * 4 + 4)])
        rms_stats(lambda c: hT[:, c, 0:N_MEM], lambda c: ("hT", c, 0), N_MEM)
        for c in range(8):
            T.op("dve", lambda: nc.vector.scalar_tensor_tensor(
                out=memnT[:, c, :], in0=hT[:, c, 0:N_MEM], scalar=gains[:, G_MEM + c:G_MEM + c + 1],
                in1=rstd[:, 0:N_MEM], op0=ALU.mult, op1=ALU.mult),
                rd=[("hT", c, 0), "gains", "rstd"], wr=[("memnT", c)])

    def mem_kv(l):
        def hk(j, pb):
            T.op("act", lambda: nc.scalar.copy(out=memkT[:, j, :], in_=ps[pb][:, 0:N_MEM]),
                 rd=[("ps", pb)], wr=[("memkT", j)])
            T.op("act", lambda: nc.scalar.activation(out=sq[:, j % 2, 0:N_MEM], in_=ps[pb][:, 0:N_MEM], func=AF.Square),
                 rd=[("ps", pb)], wr=[("sq", j % 2)])
            T.op("pe", lambda: nc.tensor.matmul(ps[6][:, 0:N_MEM], lhsT=onesb, rhs=sq[:, j % 2, 0:N_MEM],
                                                start=True, stop=True),
                 rd=["cb", ("sq", j % 2)], wr=[("ps", 6)])
            T.op("dve", lambda: nc.vector.reduce_max(out=negkmax[:, j:j + 1], in_=ps[6][:, 0:N_MEM],
                                                     axis=mybir.AxisListType.X),
                 rd=[("ps", 6)], wr=[("negkmax", j)])
            T.op("act", lambda: nc.scalar.activation(out=negkmax[:, j:j + 1], in_=negkmax[:, j:j + 1], func=AF.Sqrt),
                 rd=[("negkmax", j)], wr=[("negkmax", j)])
            T.op("dve", lambda: nc.vector.tensor_scalar(out=negkmax[:, j:j + 1], in0=negkmax[:, j:j + 1],
                                                        scalar1=-1.0, scalar2=None, op0=ALU.mult),
                 rd=[("negkmax", j)], wr=[("negkmax", j)])
        proj_fm("w_mem_kv", l, 0, 4, memnT, "memnT", N_MEM, hk)
        for half in range(2):
            def hv(tt, pb):
                T.op("act", lambda: nc.scalar.copy(out=memv[:, tt, half * 256:(half + 1) * 256], in_=ps[pb][:, 0:256]),
                     rd=[("ps", pb)], wr=[("memv", tt, half)])
            proj_tm("w_mem_kv", l, 512 + half * 256, 256, memnT, "memnT", 2, hv)

    def qmem_handler(j, pb):
        T.op("act", lambda: nc.scalar.activation(out=qmT[:, j, :], in_=ps[pb][:], func=AF.Copy, scale=128.0 ** -0.5),
             rd=[("ps", pb)], wr=[("qmT", j)])

    def mem_attn(b):
        for hm in range(4):
            T.op("act", lambda: nc.scalar.activation(out=sq[:, hm % 2, :], in_=qmT[:, hm, :], func=AF.Square),
                 rd=[("qmT", hm)], wr=[("sq", hm % 2)])
            T.op("pe", lambda: nc.tensor.matmul(ps[6][:], lhsT=onesb, rhs=sq[:, hm % 2, :], start=True, stop=True),
                 rd=["cb", ("sq", hm % 2)], wr=[("ps", 6)])
            T.op("act", lambda: nc.scalar.activation(out=cq[0:1, :], in_=ps[6][0:1, :], func=AF.Sqrt),
                 rd=[("ps", 6)], wr=["cq"])
            T.op("dve", lambda: nc.vector.tensor_scalar(out=negc[0:1, :], in0=cq[0:1, :],
                                                        scalar1=negkmax[0:1, hm:hm + 1], scalar2=None, op0=ALU.mult),
                 rd=["cq", ("negkmax", hm)], wr=["negc"])
            for mt in range(2):
                pb = nextbank()
                T.op("pe", lambda: nc.tensor.matmul(ps[pb][:], lhsT=memkT[:, hm, mt * 128:(mt + 1) * 128],
                                                    rhs=qmT[:, hm, :], start=True, stop=False),
                     rd=[("memkT", hm), ("qmT", hm)], wr=[("ps", pb)], inc=False)
                T.op("pe", lambda: nc.tensor.matmul(ps[pb][:], lhsT=onesb[0:1, :], rhs=negc[0:1, :],
                                                    start=False, stop=True),
                     rd=["cb", "negc"], wr=[("ps", pb)])
                T.op("act", lambda: nc.scalar.activation(out=pT[:, mt, :], in_=ps[pb][:], func=AF.Exp),
                     rd=[("ps", pb)], wr=[("pT", mt)])
            po, pd = nextbank(), 6
            for mt in range(2):
                T.op("pe", lambda: nc.tensor.matmul(ps[po][:], lhsT=memv[:, mt, hm * 128:(hm + 1) * 128],
                                                    rhs=pT[:, mt, :], start=(mt == 0), stop=(mt == 1)),
                     rd=[("memv", mt, hm // 2), ("pT", mt)], wr=[("ps", po)], inc=(mt == 1))
            for mt in range(2):
                T.op("pe", lambda: nc.tensor.matmul(ps[pd][:], lhsT=onesb, rhs=pT[:, mt, :],
                                                    start=(mt == 0), stop=(mt == 1)),
                     rd=["cb", ("pT", mt)], wr=[("ps", pd)], inc=(mt == 1))
            T.op("dve", lambda: nc.vector.reciprocal(out=rden[:], in_=ps[pd][:]), rd=[("ps", pd)], wr=["rden"])
            T.op("dve", lambda: nc.vector.tensor_tensor(out=catT[:, 8 + hm, :], in0=ps[po][:], in1=rden[:],
                                                        op=ALU.mult),
                 rd=[("ps", po), "rden"], wr=[("catT", 8 + hm)])

    sbst = {}
    kT_d = P.dram("sb_kT_scr", [8, 128, S], BF16, "Internal")
    v_d = P.dram("sb_v_scr", [8, S, 128], BF16, "Internal")
    c_kv = T.new_counter("kv")

    def sb_alloc():
        R2, R2f = LS["R2"], LS["R2f"]
        def v3(lo):
            return R2[:, lo:lo + 1536].rearrange("p (a n) -> p a n", a=3)
        sbst["qT"] = R2[:, 0:4096].rearrange("p (a n) -> p a n", a=8)
        sbst["Lp"] = v3(4096)
        sbst["attB"] = v3(5632)
        sbst["Pm"] = v3(7168)
        sbst["att"] = v3(8704)
        sbst["e"] = v3(10240)
        sbst["kc"] = R1[:, 0:4096].rearrange("p (a n) -> p a n", a=2)
        sbst["vc"] = R1[:, 4096:8192].rearrange("p (a k n) -> p a k n", a=2, k=16)
        sbst["kst"] = R1[:, 8192:9216].rearrange("p (a n) -> p a n", a=2)
        sbst["vst"] = R1[:, 9216:9728].rearrange("p (a n) -> p a n", a=2)

    def sb_setup():
        T.dma("sp", c_io, cb[:, CB_MASK01:CB_MASK01 + 4 * TB], cb_d[:, CB_MASK01:CB_MASK01 + 4 * TB], wr=["cb"])

    def sb_inproj(l, b):
        qT, kst, vst = sbst["qT"], sbst["kst"], sbst["vst"]
        mixer_keys[:] = [("sb_kc", 0), ("sb_kc", 1), ("sb_vc", 0), ("sb_vc", 1), ("sb_kst", 0), ("sb_kst", 1), ("sb_vst", 0), ("sb_vst", 1)]
        T.alias(mixer_keys, [("aT", j) for j in range(22)])

        def hq(j, pb):
            T.op("act", lambda: nc.scalar.activation(out=qT[:, j, :], in_=ps[pb][:], func=AF.Copy, scale=0.125),
                 rd=[("ps", pb)], wr=[("sb_qT", j)])

        def hk(j, pb):
            k2 = j % 2
            T.op("dve", lambda: nc.vector.tensor_copy(out=kst[:, k2, :], in_=ps[pb][:]),
                 rd=[("ps", pb)], wr=[("sb_kst", k2)])
            T.dma("sp", c_kv, kT_d[j, :, b * TB:(b + 1) * TB], kst[:, k2, :], rd=[("sb_kst", k2)],
                  wr=[("kT_d", j, b)])
        proj_fm("sb_w_in", 0, 0, 8, uT, "uT", TB, hq)
        proj_fm("sb_w_in", 0, 1024, 8, uT, "uT", TB, hk)
        for q4 in range(4):
            def hv(tt, pb):
                k2 = tt % 2
                T.op("act", lambda: nc.scalar.copy(out=vst[:, k2, :], in_=ps[pb][:, 0:256]),
                     rd=[("ps", pb)], wr=[("sb_vst", k2)])
                for pp in range(2):
                    cpair = 2 * q4 + pp
                    t0 = b * TB + tt * 128
                    T.dma("sp", c_kv, v_d[cpair, t0:t0 + 128, :], vst[:, k2, pp * 128:(pp + 1) * 128],
                          rd=[("sb_vst", k2)], wr=[("v_d", cpair, b)])
            proj_tm("sb_w_in", 0, 2048 + q4 * 256, 256, uT, "uT", 4, hv)
        proj_fm("sb_w_in", 0, 3072, 4, uT, "uT", TB, qmem_handler)

    def sb_load_pair(c, b):
        sl = c % 2
        n = (b + 1) * TB
        T.dma("sp", c_kv, sbst["kc"][:, sl, 0:n], kT_d[c, :, 0:n], rd=[("kT_d", c, bb) for bb in range(b + 1)],
              wr=[("sb_kc", sl)])
        T.dma("sp", c_kv, sbst["vc"][:, sl, 0:4 * (b + 1), :],
              v_d[c, 0:n, :].rearrange("(k p) n -> p k n", p=128),
              rd=[("v_d", c, bb) for bb in range(b + 1)], wr=[("sb_vc", sl)])

    def sb_attn(b):
        qT = sbst["qT"]
        e, Lp, attB, Pm, att = sbst["e"], sbst["Lp"], sbst["attB"], sbst["Pm"], sbst["att"]
        sb_load_pair(0, b)
        nk = 4 * b + 4
        rot = {"i": 0}

        def sbbank():
            rot["i"] = (rot["i"] + 1) % 6
            return (0, 1, 2, 3, 4, 7)[rot["i"]]
        for c in range(8):
            if c + 1 < 8:
                sb_load_pair(c + 1, b)
            sl = c % 2
            kc, vc = sbst["kc"], sbst["vc"]
            pO = 5
            for hh in range(2):
                po = hh * 64
                qview = qT[po:po + 64, c, :]
                order = list(range(nk - 1, -1, -1))
                for g0 in range(0, nk, 3):
                    grp = [(g0 + u, order[g0 + u]) for u in range(3) if g0 + u < nk]
                    banks_a, banks_b = {}, {}
                    for u, (idx, kb) in enumerate(grp):
                        kview = kc[po:po + 64, sl, kb * 128:(kb + 1) * 128]
                        pa = sbbank()
                        banks_a[u] = pa
                        T.op("pe", lambda: nc.tensor.matmul(ps[pa][:], lhsT=kview, rhs=qview, start=True, stop=True),
                             rd=[("sb_kc", sl), ("sb_qT", c)], wr=[("ps", pa)])
                    for u, (idx, kb) in enumerate(grp):
                        pa = banks_a[u]
                        T.op("act", lambda: nc.scalar.activation(out=e[:, u, :], in_=ps[pa][:], func=AF.Exp),
                             rd=[("ps", pa)], wr=[("sb_e", u)])
                    for u, (idx, kb) in enumerate(grp):
                        T.op("act", lambda: nc.scalar.activation(out=Lp[:, u, :], in_=e[:, u, :], func=AF.Ln,
                                                                 bias=oneb[:]),
                             rd=[("sb_e", u), "oneb"], wr=[("sb_Lp", u)])
                        i = kb - 4 * b
                        if i >= 0:
                            m01 = cb[:, CB_MASK01 + i * TB:CB_MASK01 + (i + 1) * TB]
                            T.op("pool", lambda: nc.gpsimd.tensor_tensor(out=Lp[:, u, :], in0=Lp[:, u, :], in1=m01,
                                                                         op=ALU.mult),
                                 rd=[("sb_Lp", u), "cb"], wr=[("sb_Lp", u)])
                    for u, (idx, kb) in enumerate(grp):
                        kview = kc[po:po + 64, sl, kb * 128:(kb + 1) * 128]
                        pbk = sbbank()
                        banks_b[u] = pbk
                        T.op("pe", lambda: nc.tensor.matmul(ps[pbk][:], lhsT=kview, rhs=qview, start=True, stop=False),
                             rd=[("sb_kc", sl), ("sb_qT", c)], wr=[("ps", pbk)], inc=False)
                        T.op("pe", lambda: nc.tensor.matmul(ps[pbk][:], lhsT=negMincl, rhs=Lp[:, u, :],
                                                            start=False, stop=True),
                             rd=["cb", ("sb_Lp", u)], wr=[("ps", pbk)])
                    for u, (idx, kb) in enumerate(grp):
                        pbk = banks_b[u]
                        T.op("act", lambda: nc.scalar.activation(out=attB[:, u, :], in_=ps[pbk][:], func=AF.Exp),
                             rd=[("ps", pbk)], wr=[("sb_attB", u)])
                        i = kb - 4 * b
                        if i >= 0:
                            m01 = cb[:, CB_MASK01 + i * TB:CB_MASK01 + (i + 1) * TB]
                            T.op("pool", lambda: nc.gpsimd.tensor_tensor(out=attB[:, u, :], in0=attB[:, u, :], in1=m01,
                                                                         op=ALU.mult),
                                 rd=[("sb_attB", u), "cb"], wr=[("sb_attB", u)])
                    for u, (idx, kb) in enumerate(grp):
                        if idx > 0:
                            T.op("act", lambda: nc.scalar.activation(out=Pm[:, u, :], in_=ps[6][:], func=AF.Exp),
                                 rd=[("ps", 6)], wr=[("sb_Pm", u)])
                            T.op("dve", lambda: nc.vector.tensor_tensor(out=att[:, u, :], in0=attB[:, u, :],
                                                                        in1=Pm[:, u, :], op=ALU.mult),
                                 rd=[("sb_attB", u), ("sb_Pm", u)], wr=[("sb_att", u)])
                            a_ap, a_key = att[:, u, :], ("sb_att", u)
                        else:
                            a_ap, a_key = attB[:, u, :], ("sb_attB", u)
                        if idx < nk - 1:
                            T.op("pe", lambda: nc.tensor.matmul(ps[6][:], lhsT=negones, rhs=Lp[:, u, :],
                                                                start=(idx == 0), stop=True),
                                 rd=["cb", ("sb_Lp", u)], wr=[("ps", 6)])
                        T.op("pe", lambda: nc.tensor.matmul(ps[pO][po:po + 64, :], lhsT=vc[:, sl, kb, po:po + 64],
                                                            rhs=a_ap, start=(idx == 0), stop=(idx == nk - 1),
                                                            tile_position=(0, po)),
                             rd=[("sb_vc", sl), a_key], wr=[("ps", pO, hh)])
            T.op("dve", lambda: nc.vector.tensor_copy(out=catT[:, c, :], in_=ps[pO][:]),
                 rd=[("ps", pO, 0), ("ps", pO, 1)], wr=[("catT", c)])

    mlst = {}
    sel_d = P.dram("selc", [16, 8 * 128], F32, "ExternalInput")
    mlb_d = P.dram("mlbias", [16, 1], F32, "ExternalInput")
    mlg_d = P.dram("mlgain", [128, 1], F32, "ExternalInput")
    m01i_d = P.dram("mask01incl", [128, 4 * TB], BF16, "ExternalInput")

    def ml_alloc():
        R2, R2f = LS["R2"], LS["R2f"]
        mlst["qT"] = R2[:, 0:2048].rearrange("p (a n) -> p a n", a=4)
        mlst["sigo"] = R2[:, 2048:2560]
        mlst["Dm"] = R2[:, 2560:3584].rearrange("p (a n) -> p a n", a=2)
        mlst["Wt"] = R2[:, 3584:4608].rearrange("p (a n) -> p a n", a=2)
        mlst["hsb"] = R2f[:, 2304:2816]
        mlst["gsb"] = R2f[0:16, 2816:3328]
        mlst["lp"] = R2f[0:16, 3328:3840]
        mlst["cum"] = R2f[0:16, 3840:4352]
        mlst["NF"] = R2f[0:16, 4352:4864]
        mlst["sel"] = R2f[0:16, 4864:5888]
        mlst["Acol"] = R2f[:, 5888:6016].rearrange("p (a n) -> p a n", a=16)
        mlst["tT"] = R2f[:, 6016:6048].rearrange("p (a n) -> p a n", a=2)
        mlst["carry"] = R2f[0:16, 6048:6049]
        mlst["bias"] = R2f[0:16, 6049:6050]
        mlst["gain"] = R2f[:, 6050:6051]
        mlst["kc"] = R1[:, 0:4096].rearrange("p (a n) -> p a n", a=2)
        mlst["vc"] = R1[:, 4096:8192].rearrange("p (a k n) -> p a k n", a=2, k=16)
        mlst["kst"] = R1[:, 8192:9216].rearrange("p (a n) -> p a n", a=2)
        mlst["vst"] = R1[:, 9216:9728].rearrange("p (a n) -> p a n", a=2)

    def ml_setup():
        T.dma("sp", c_io, mlst["sel"][:], sel_d, wr=["ml_sel"])
        T.dma("sp", c_io, mlst["bias"][:], mlb_d, wr=["ml_bias"])
        T.dma("sp", c_io, mlst["gain"][:], mlg_d, wr=["ml_gain"])
        T.dma("sp", c_io, cb[:, CB_MASK01:CB_MASK01 + 4 * TB], m01i_d, wr=["cb"])

    def ml_inproj(l, b):
        qT, kst, vst = mlst["qT"], mlst["kst"], mlst["vst"]
        gsb, lp, cum, carry, NF, Acol, tT = (mlst[k] for k in ("gsb", "lp", "cum", "carry", "NF", "Acol", "tT"))
        mixer_keys[:] = [("sb_kc", 0), ("sb_kc", 1), ("sb_vc", 0), ("sb_vc", 1), ("sb_kst", 0), ("sb_kst", 1),
                         ("sb_vst", 0), ("sb_vst", 1)]
        T.alias(mixer_keys, [("aT", j) for j in range(22)])

        def hq(j, pb):
            T.op("act", lambda: nc.scalar.copy(out=qT[:, j, :], in_=ps[pb][:]), rd=[("ps", pb)], wr=[("ml_qT", j)])

        def hk(j, pb):
            k2 = j % 2
            T.op("dve", lambda: nc.vector.tensor_scalar(out=kst[:, k2, :], in0=ps[pb][:], scalar1=0.125, scalar2=None,
                                                        op0=ALU.mult),
                 rd=[("ps", pb)], wr=[("sb_kst", k2)])
            T.dma("sp", c_kv, kT_d[j, :, b * TB:(b + 1) * TB], kst[:, k2, :], rd=[("sb_kst", k2)],
                  wr=[("kT_d", j, b)])
        proj_fm("ml_w_in", 0, 0, 4, uT, "uT", TB, hq)
        proj_fm("ml_w_in", 0, 512, 4, uT, "uT", TB, hk)
        for q4 in range(4):
            def hv(tt, pb):
                k2 = tt % 2
                T.op("act", lambda: nc.scalar.copy(out=vst[:, k2, :], in_=ps[pb][:, 0:256]),
                     rd=[("ps", pb)], wr=[("sb_vst", k2)])
                for pp in range(2):
                    hd = 2 * q4 + pp
                    t0 = b * TB + tt * 128
                    T.dma("sp", c_kv, v_d[hd, t0:t0 + 128, :], vst[:, k2, pp * 128:(pp + 1) * 128],
                          rd=[("sb_vst", k2)], wr=[("v_d", hd, b)])
            proj_tm("ml_w_in", 0, 1024 + q4 * 256, 256, uT, "uT", 4, hv)
        slot = w_get([("ml_w_in", 0, 0, 8, 3072, 16)])
        wv = wview(slot, 0, 8, 16)
        pg = nextbank()
        for kc in range(8):
            T.op("pe", lambda: nc.tensor.matmul(ps[pg][0:16, :], lhsT=wv[:, kc, :], rhs=uT[:, kc, :],
                                                start=(kc == 0), stop=(kc == 7)),
                 rd=[WK(slot), ("uT", kc)], wr=[("ps", pg)], inc=(kc == 7))
        T.op("act", lambda: nc.scalar.activation(out=gsb[:], in_=ps[pg][0:16, :], func=AF.Identity,
                                                 bias=mlst["bias"][:]),
             rd=[("ps", pg), "ml_bias"], wr=["ml_gsb"])
        T.op("act", lambda: nc.scalar.activation(out=lp[:], in_=gsb[:], func=AF.Exp, scale=-1.0),
             rd=["ml_gsb"], wr=["ml_lp"])
        T.op("act", lambda: nc.scalar.activation(out=lp[:], in_=lp[:], func=AF.Ln, bias=oneb[0:16, :]),
             rd=["ml_lp", "oneb"], wr=["ml_lp"])
        if b == 0:
            T.op("dve", lambda: nc.vector.memset(carry[:], 0.0), wr=["ml_carry"])
        T.op("dve", lambda: nc.vector.tensor_tensor_scan(out=cum[:], data0=lp[:], data1=lp[:], initial=carry[:, 0:1],
                                                         op0=ALU.add, op1=ALU.max),
             rd=["ml_lp", "ml_carry"], wr=["ml_cum"])
        T.op("dve", lambda: nc.vector.tensor_copy(out=carry[:], in_=cum[:, TB - 1:TB]), rd=["ml_cum"], wr=["ml_carry"])
        T.op("dve", lambda: nc.vector.tensor_scalar(out=NF[:], in0=cum[:], scalar1=-1.0, scalar2=None, op0=ALU.mult),
             rd=["ml_cum"], wr=["ml_NF"])
        for tt in range(4):
            pt = nextbank()
            T.op("pe", lambda: nc.tensor.transpose(out=ps[pt][:, 0:16], in_=gsb[:, tt * 128:(tt + 1) * 128],
                                                   identity=identf[0:16, 0:16]),
                 rd=["ml_gsb", "identf"], wr=[("ps", pt)], inc=False)
            T.op("pe", lambda: nc.tensor.transpose(out=ps[pt][:, 16:32], in_=cum[:, tt * 128:(tt + 1) * 128],
                                                   identity=identf[0:16, 0:16]),
                 rd=["ml_cum", "identf"], wr=[("ps", pt)])
            T.op("act", lambda: nc.scalar.copy(out=tT[:, tt % 2, :], in_=ps[pt][:, 16:32]),
                 rd=[("ps", pt)], wr=[("ml_tT", tt % 2)])
            T.op("dve", lambda: nc.vector.tensor_tensor(out=Acol[:, b * 4 + tt, :], in0=ps[pt][:, 0:8],
                                                        in1=tT[:, tt % 2, 8:16], op=ALU.add),
                 rd=[("ps", pt), ("ml_tT", tt % 2)], wr=[("ml_Acol", b * 4 + tt)])
        proj_fm("ml_w_in", 0, 3088, 4, uT, "uT", TB, qmem_handler)

    def ml_attn(l, b):
        qT, Dm, Wt, hsb, Acol, NF, sel, sigo = (mlst[k] for k in ("qT", "Dm", "Wt", "hsb", "Acol", "NF", "sel", "sigo"))
        kc, vc = mlst["kc"], mlst["vc"]
        it = 0
        nk = 4 * b + 4

        def load(h):
            sl = h % 2
            n = (b + 1) * TB
            if h % 2 == 0:
                T.dma("sp", c_kv, kc[:, (h // 2) % 2, 0:n], kT_d[h // 2, :, 0:n],
                      rd=[("kT_d", h // 2, bb) for bb in range(b + 1)], wr=[("sb_kc", (h // 2) % 2)])
            T.dma("sp", c_kv, vc[:, sl, 0:4 * (b + 1), :], v_d[h, 0:n, :].rearrange("(k p) n -> p k n", p=128),
                  rd=[("v_d", h, bb) for bb in range(b + 1)], wr=[("sb_vc", sl)])
        load(0)
        for h in range(8):
            if h + 1 < 8:
                load(h + 1)
            c, po, sl, ksl = h // 2, (h % 2) * 64, h % 2, (h // 2) % 2
            T.op("pe", lambda: nc.tensor.matmul(ps[6][:], lhsT=sel[:, h * 128:(h + 1) * 128], rhs=NF[:],
                                                start=True, stop=True),
                 rd=["ml_sel", "ml_NF"], wr=[("ps", 6)])
            pN, pD = 5, 7
            order = list(range(nk - 1, -1, -1))
            for g0 in range(0, nk, 2):
                grp = [(g0 + u, order[g0 + u]) for u in range(2) if g0 + u < nk]
                pas = {}
                for u, (idx, kb) in enumerate(grp):
                    pa = nextbank()
                    pas[u] = pa
                    T.op("pe", lambda: nc.tensor.matmul(ps[pa][:], lhsT=kc[po:po + 64, ksl, kb * 128:(kb + 1) * 128],
                                                        rhs=qT[po:po + 64, c, :], start=True, stop=True),
                         rd=[("sb_kc", ksl), ("ml_qT", c)], wr=[("ps", pa)])
                for u, (idx, kb) in enumerate(grp):
                    T.op("act", lambda: nc.scalar.activation(out=Dm[:, u, :], in_=ps[6][:], func=AF.Exp,
                                                             bias=Acol[:, kb, h:h + 1]),
                         rd=[("ps", 6), ("ml_Acol", kb)], wr=[("ml_Dm", u)])
                for u, (idx, kb) in enumerate(grp):
                    pa = pas[u]
                    i = kb - 4 * b
                    T.op("dve", lambda: nc.vector.tensor_tensor(out=Wt[:, u, :], in0=ps[pa][:], in1=Dm[:, u, :],
                                                                op=ALU.mult),
                         rd=[("ps", pa), ("ml_Dm", u)], wr=[("ml_Wt", u)])
                    if i >= 0:
                        m01 = cb[:, CB_MASK01 + i * TB:CB_MASK01 + (i + 1) * TB]
                        T.op("pool", lambda: nc.gpsimd.tensor_tensor(out=Wt[:, u, :], in0=Wt[:, u, :], in1=m01,
                                                                     op=ALU.mult),
                             rd=[("ml_Wt", u), "cb"], wr=[("ml_Wt", u)])
                for u, (idx, kb) in enumerate(grp):
                    T.op("pe", lambda: nc.tensor.matmul(ps[pN][:], lhsT=vc[:, sl, kb, :], rhs=Wt[:, u, :],
                                                        start=(idx == 0), stop=(idx == nk - 1)),
                         rd=[("sb_vc", sl), ("ml_Wt", u)], wr=[("ps", pN)], inc=False)
                    T.op("pe", lambda: nc.tensor.matmul(ps[pD][:], lhsT=onesb, rhs=Wt[:, u, :],
                                                        start=(idx == 0), stop=(idx == nk - 1)),
                         rd=["cb", ("ml_Wt", u)], wr=[("ps", pD)])
            T.op("act", lambda: nc.scalar.activation(out=rden[:], in_=ps[pD][:], func=AF.Abs),
                 rd=[("ps", pD)], wr=["rden"])
            T.op("dve", lambda: nc.vector.tensor_scalar(out=rden[:], in0=rden[:], scalar1=1.0, scalar2=None,
                                                        op0=ALU.max),
                 rd=["rden"], wr=["rden"])
            T.op("dve", lambda: nc.vector.reciprocal(out=rden[:], in_=rden[:]), rd=["rden"], wr=["rden"])
            T.op("dve", lambda: nc.vector.tensor_tensor(out=hsb[:], in0=ps[pN][:], in1=rden[:], op=ALU.mult),
                 rd=[("ps", pN), "rden"], wr=["ml_hsb"])
            T.op("act", lambda: nc.scalar.activation(out=sq[:, 0, :], in_=hsb[:], func=AF.Square),
                 rd=["ml_hsb"], wr=[("sq", 0)])
            T.op("pe", lambda: nc.tensor.matmul(ps[pD][:], lhsT=onesb, rhs=sq[:, 0, :], start=True, stop=True),
                 rd=["cb", ("sq", 0)], wr=[("ps", pD)])
            T.op("act", lambda: nc.scalar.activation(out=rden[:], in_=ps[pD][:], func=AF.Sqrt, scale=1.0 / 128,
                                                     bias=epsb[:]),
                 rd=[("ps", pD), "epsb"], wr=["rden"])
            T.op("dve", lambda: nc.vector.reciprocal(out=rden[:], in_=rden[:]), rd=["rden"], wr=["rden"])
            T.op("dve", lambda: nc.vector.scalar_tensor_tensor(out=hsb[:], in0=hsb[:], scalar=mlst["gain"][:, 0:1],
                                                               in1=rden[:], op0=ALU.mult, op1=ALU.mult),
                 rd=["ml_hsb", "ml_gain", "rden"], wr=["ml_hsb"])
            slot = w_get([("ml_w_in", 0, 0, 8, 2048 + h * 128, 128)])
            wv = wview(slot, 0, 8, 128)
            pg = nextbank()
            for kcc in range(8):
                T.op("pe", lambda: nc.tensor.matmul(ps[pg][:], lhsT=wv[:, kcc, :], rhs=uT[:, kcc, :],
                                                    start=(kcc == 0), stop=(kcc == 7)),
                     rd=[WK(slot), ("uT", kcc)], wr=[("ps", pg)], inc=(kcc == 7))
            T.op("act", lambda: nc.scalar.activation(out=sigo[:], in_=ps[pg][:], func=AF.Sigmoid),
                 rd=[("ps", pg)], wr=["ml_sigo"])
            T.op("dve", lambda: nc.vector.tensor_tensor(out=catT[:, h, :], in0=hsb[:], in1=sigo[:], op=ALU.mult),
                 rd=["ml_hsb", "ml_sigo"], wr=[("catT", h)])

    gd = {}
    gcw_d = P.dram("gdn_cw", [2, 128, 96], F32, "ExternalInput")
    gdtb_d = P.dram("gdn_dtb", [2, 8, 1], F32, "ExternalInput")
    galog_d = P.dram("gdn_alog", [2, 8, 1], F32, "ExternalInput")
    ggain_d = P.dram("gdn_gain", [2, 128, 1], F32, "ExternalInput")
    sel8_d = P.dram("sel8", [8, 8 * 128], F32, "ExternalInput")
    gmask_d = P.dram("gdn_masks", [128, 3 * TB], BF16, "ExternalInput")
    R3 = ysb_raw.bitcast(BF16)
    R1f = R1.bitcast(F32)

    def gdn_alloc():
        gF, gR2 = LS["gF"], LS["gR2"]
        gFr = gF.bitcast(F32R)
        gd["A"] = gF[:, 0:1024].rearrange("p (s n) -> p s n", s=2)
        gd["AT"] = gF[:, 1024:2048].rearrange("p (s n) -> p s n", s=2)
        gd["X"] = gF[:, 2048:2560]
        gd["Ru"] = gF[:, 2560:3072]
        gd["Rw"] = gF[:, 3072:3584]
        gd["Ar"] = gFr[:, 0:1024].rearrange("p (s n) -> p s n", s=2)
        gd["ATr"] = gFr[:, 1024:2048].rearrange("p (s n) -> p s n", s=2)
        gd["Xr"] = gFr[:, 2048:2560]
        gd["Rur"] = gFr[:, 2560:3072]
        gd["Rwr"] = gFr[:, 3072:3584]
        o1 = 0

        def f1(n, parts=128):
            nonlocal o1
            v = R1f[0:parts, o1:o1 + n]
            o1 += n
            return v
        gd["u"] = f1(512)
        gd["knf"] = f1(512)
        gd["vsf"] = f1(512)
        gd["ra"] = f1(512, 8)
        gd["rb"] = f1(512, 8)
        gd["G"] = f1(512, 8)
        gd["NG"] = f1(512, 8)
        gd["GB"] = f1(512)
        gd["xc"] = f1(516)
        gd["xc2"] = f1(516)
        assert o1 <= 5632
        o = 0

        def f2(n, parts=128):
            nonlocal o
            v = gR2[0:parts, o:o + n]
            o += n
            return v
        gd["sel8"] = f2(1024, 8)
        gd["acc"] = f2(512)
        gd["S"] = f2(1024).rearrange("p (h n) -> p h n", h=8)
        gd["gcol"] = f2(64).rearrange("p (t n) -> p t n", t=4)
        gd["ngcol"] = f2(64).rearrange("p (t n) -> p t n", t=4)
        gd["bgc"] = f2(32).rearrange("p (t n) -> p t n", t=4)
        gd["egc"] = f2(32).rearrange("p (t n) -> p t n", t=4)
        gd["kgc"] = f2(4)
        gd["glc"] = f2(4)
        gd["cw"] = f2(96).rearrange("p (c k) -> p c k", k=4)
        gd["halo"] = f2(72).rearrange("p (c k) -> p c k", k=3)
        gd["gain"] = f2(1)
        gd["dtb"] = f2(1, 8)
        gd["negA"] = f2(1, 8)
        gd["rr"] = rden[:, :]
        assert o <= 2944, o
        o3 = 0

        def f3(n):
            nonlocal o3
            v = R3[:, o3:o3 + n]
            o3 += n
            return v
        for nm in ("qnT", "qgT", "knT", "kbT", "zs", "EG", "Bb", "E1", "E2", "E3", "kg", "wT", "pTg"):
            gd[nm] = f3(512)
        gd["vnb"] = f3(256).rearrange("p (s n) -> p s n", s=2)
        gd["Sb"] = f3(128)
        assert o3 <= 8192

    G_R1KEYS = ["g_u", "g_knf", "g_vsf", "g_ra", "g_rb", "g_G", "g_NG", "g_GB", "g_xc", "g_xc2"]

    def gdn_setup(l):
        jg = l // 3
        T.dma("sp", c_io, gd["cw"].rearrange("p c k -> p (c k)"), gcw_d[jg], wr=["g_cw"])
        T.dma("sp", c_io, gd["dtb"], gdtb_d[jg], wr=["g_dtb"])
        T.dma("sp", c_io, gd["negA"], galog_d[jg], wr=["g_negA"])
        T.dma("sp", c_io, gd["gain"], ggain_d[jg], wr=["g_gain"])
        T.dma("sp", c_io, gd["sel8"], sel8_d, wr=["g_sel8"])
        T.dma("sp", c_io, cb[:, CB_MASK01:CB_MASK01 + 3 * TB], gmask_d, wr=["cb"])
        T.op("act", lambda: nc.scalar.activation(out=gd["negA"], in_=gd["negA"], func=AF.Exp), rd=["g_negA"], wr=["g_negA"])
        T.op("dve", lambda: nc.vector.tensor_scalar(out=gd["negA"], in0=gd["negA"], scalar1=-1.0, scalar2=None,
                                                    op0=ALU.mult), rd=["g_negA"], wr=["g_negA"])
        T.op("pool", lambda: nc.gpsimd.memset(gd["S"].rearrange("p h n -> p (h n)"), 0.0), wr=[("g_S", h) for h in range(8)])

    def gdn_block(l, b):
        jg = l // 3
        g = gd
        M1 = cb[:, CB_MASK01:CB_MASK01 + TB]
        M2 = cb[:, CB_MASK01 + TB:CB_MASK01 + 2 * TB]
        M3 = cb[:, CB_MASK01 + 2 * TB:CB_MASK01 + 3 * TB]
        mixer_keys[:] = G_R1KEYS
        T.alias(mixer_keys, [("aT", j) for j in range(22)])
        ysb_overlay[:] = ["g_qnT", "g_qgT", "g_knT", "g_kbT", "g_zs", "g_EG", "g_Bb", "g_E1", "g_E2", "g_E3", "g_kg", "g_wT", "g_pTg", ("g_vnb", 0), ("g_vnb", 1), "g_Sb"]
        T.alias(ysb_overlay, [("ysb", c) for c in range(8)])
        slot = w_get([("gdn_w_in", jg, 0, 8, 4096, 16)])
        wv = wview(slot, 0, 8, 16)
        pa, pb_ = nextbank(), nextbank()
        for (pbank, c0) in ((pa, 0), (pb_, 8)):
            for kc in range(8):
                T.op("pe", lambda: nc.tensor.matmul(ps[pbank][0:8, :], lhsT=wv[:, kc, c0:c0 + 8], rhs=uT[:, kc, :],
                                                    start=(kc == 0), stop=(kc == 7)),
                     rd=[WK(slot), ("uT", kc)], wr=[("ps", pbank)], inc=(kc == 7))
        T.op("act", lambda: nc.scalar.activation(out=g["ra"], in_=ps[pa][0:8, :], func=AF.Exp, bias=g["dtb"]),
             rd=[("ps", pa), "g_dtb"], wr=["g_ra"])
        T.op("act", lambda: nc.scalar.activation(out=g["ra"], in_=g["ra"], func=AF.Ln, bias=oneb[0:8, :]),
             rd=["g_ra", "oneb"], wr=["g_ra"])
        T.op("dve", lambda: nc.vector.tensor_scalar(out=g["ra"], in0=g["ra"], scalar1=g["negA"], scalar2=None,
                                                    op0=ALU.mult), rd=["g_ra", "g_negA"], wr=["g_ra"])
        T.op("act", lambda: nc.scalar.activation(out=g["rb"], in_=ps[pb_][0:8, :], func=AF.Sigmoid),
             rd=[("ps", pb_)], wr=["g_rb"])
        for n in range(4):
            cs = slice(n * 128, (n + 1) * 128)
            T.op("dve", lambda: nc.vector.tensor_tensor_scan(out=g["G"][:, cs], data0=g["ra"][:, cs], data1=g["ra"][:, cs],
                                                             initial=0.0, op0=ALU.add, op1=ALU.min),
                 rd=["g_ra"], wr=["g_G"])
        T.op("dve", lambda: nc.vector.tensor_scalar(out=g["NG"], in0=g["G"], scalar1=-1.0, scalar2=None, op0=ALU.mult),
             rd=["g_G"], wr=["g_NG"])
        for tt in range(4):
            cs = slice(tt * 128, (tt + 1) * 128)
            pt = nextbank()
            T.op("pe", lambda: nc.tensor.transpose(out=ps[pt][:, 0:8], in_=g["G"][:, cs], identity=identf[0:8, 0:8]),
                 rd=["g_G", "identf"], wr=[("ps", pt)], inc=False)
            T.op("pe", lambda: nc.tensor.transpose(out=ps[pt][:, 8:16], in_=g["rb"][:, cs], identity=identf[0:8, 0:8]),
                 rd=["g_rb", "identf"], wr=[("ps", pt)])
            T.op("act", lambda: nc.scalar.copy(out=g["gcol"][:, tt, :], in_=ps[pt][:, 0:16]),
                 rd=[("ps", pt)], wr=["g_gcol"])
        T.op("dve", lambda: nc.vector.tensor_scalar(out=g["ngcol"], in0=g["gcol"], scalar1=-1.0, scalar2=None,
                                                    op0=ALU.mult), rd=["g_gcol"], wr=["g_ngcol"])
        T.op("act", lambda: nc.scalar.activation(out=g["egc"], in_=g["gcol"][:, :, 0:8], func=AF.Exp),
             rd=["g_gcol"], wr=["g_egc"])
        T.op("dve", lambda: nc.vector.tensor_tensor(out=g["bgc"], in0=g["egc"], in1=g["gcol"][:, :, 8:16], op=ALU.mult),
             rd=["g_egc", "g_gcol"], wr=["g_bgc"])

        def conv_silu(cj, pbank, dst, dkey):
            xc, acc, cw, halo = g["xc"], g["acc"], g["cw"], g["halo"]
            T.op("act", lambda: nc.scalar.copy(out=xc[:, 3:515], in_=ps[pbank][:]), rd=[("ps", pbank)], wr=["g_xc"])
            if b == 0:
                T.op("dve", lambda: nc.vector.memset(xc[:, 0:3], 0.0), wr=["g_xc"])
            else:
                T.op("dve", lambda: nc.vector.tensor_copy(out=xc[:, 0:3], in_=halo[:, cj, :]), rd=[("g_halo", cj)],
                     wr=["g_xc"])
            T.op("dve", lambda: nc.vector.tensor_copy(out=halo[:, cj, :], in_=xc[:, 512:515]), rd=["g_xc"],
                 wr=[("g_halo", cj)])
            T.op("act", lambda: nc.scalar.activation(out=acc, in_=xc[:, 0:512], func=AF.Copy, scale=cw[:, cj, 0:1]),
                 rd=["g_xc", "g_cw"], wr=["g_acc"])
            for k in range(1, 4):
                T.op("dve", lambda: nc.vector.scalar_tensor_tensor(out=acc, in0=xc[:, k:k + 512], scalar=cw[:, cj, k:k + 1],
                                                                   in1=acc, op0=ALU.mult, op1=ALU.add),
                     rd=["g_xc", "g_cw", "g_acc"], wr=["g_acc"])
            T.op("act", lambda: nc.scalar.activation(out=dst, in_=acc, func=AF.Silu), rd=["g_acc"], wr=[dkey])

        def l2_rstd(src, skey):
            T.op("act", lambda: nc.scalar.activation(out=sq[:, 0, :], in_=src, func=AF.Square), rd=[skey], wr=[("sq", 0)])
            T.op("pe", lambda: nc.tensor.matmul(ps[7][:], lhsT=onesb, rhs=sq[:, 0, :], start=True, stop=True),
                 rd=["cb", ("sq", 0)], wr=[("ps", 7)])
            T.op("act", lambda: nc.scalar.activation(out=g["rr"], in_=ps[7][:], func=AF.Sqrt, bias=epsb[:]),
                 rd=[("ps", 7), "epsb"], wr=["rden"])
            T.op("dve", lambda: nc.vector.reciprocal(out=g["rr"], in_=g["rr"]), rd=["rden"], wr=["rden"])

        for h in range(8):
            s8 = g["sel8"][:, h * 128:(h + 1) * 128]
            pG = nextbank()
            T.op("pe", lambda: nc.tensor.matmul(ps[pG][:], lhsT=s8, rhs=g["G"], start=True, stop=True),
                 rd=["g_sel8", "g_G"], wr=[("ps", pG)])
            T.op("act", lambda: nc.scalar.copy(out=g["GB"], in_=ps[pG][:]), rd=[("ps", pG)], wr=["g_GB"])
            T.op("act", lambda: nc.scalar.activation(out=g["EG"], in_=ps[pG][:], func=AF.Exp), rd=[("ps", pG)], wr=["g_EG"])
            pB = nextbank()
            T.op("pe", lambda: nc.tensor.matmul(ps[pB][:], lhsT=s8, rhs=g["rb"], start=True, stop=True),
                 rd=["g_sel8", "g_rb"], wr=[("ps", pB)])
            T.op("act", lambda: nc.scalar.copy(out=g["Bb"], in_=ps[pB][:]), rd=[("ps", pB)], wr=["g_Bb"])
            for (nm, rows, rkey, Mk, colt, off) in (("E1", g["NG"], "g_NG", M1, g["gcol"], 0),
                                                     ("E2", g["G"], "g_G", M2, g["ngcol"], 0),
                                                     ("E3", g["G"], "g_G", M3, g["ngcol"], 0)):
                pe_ = nextbank()
                T.op("pe", lambda: nc.tensor.matmul(ps[pe_][:], lhsT=s8, rhs=rows, start=True, stop=False),
                     rd=["g_sel8", rkey], wr=[("ps", pe_)], inc=False)
                T.op("pe", lambda: nc.tensor.matmul(ps[pe_][:], lhsT=identb, rhs=Mk, start=False, stop=True),
                     rd=["cb"], wr=[("ps", pe_)])
                for n in range(4):
                    cs = slice(n * 128, (n + 1) * 128)
                    T.op("act", lambda: nc.scalar.activation(out=g[nm][:, cs], in_=ps[pe_][:, cs], func=AF.Exp,
                                                             bias=colt[:, n, h:h + 1]),
                         rd=[("ps", pe_), "g_gcol", "g_ngcol"], wr=["g_" + nm])
            for n in range(4):
                T.op("act", lambda: nc.scalar.activation(out=g["kgc"][:, n:n + 1], in_=g["gcol"][:, n, h:h + 1], func=AF.Exp,
                                                         scale=-1.0, bias=g["GB"][:, n * 128 + 127:n * 128 + 128]),
                     rd=["g_gcol", "g_GB"], wr=["g_kgc"])
                T.op("act", lambda: nc.scalar.activation(out=g["glc"][:, n:n + 1],
                                                         in_=g["GB"][:, n * 128 + 127:n * 128 + 128], func=AF.Exp),
                     rd=["g_GB"], wr=["g_glc"])
            slot = w_get([("gdn_w_in", jg, 0, 8, h * 128, 128), ("gdn_w_in", jg, 0, 8, 1024 + h * 128, 128)])
            slot2 = w_get([("gdn_w_in", jg, 0, 8, 2048 + h * 128, 128), ("gdn_w_in", jg, 0, 8, 3072 + h * 128, 128)])
            banks = []
            for (sl_, piece) in ((slot, 0), (slot, 1), (slot2, 0), (slot2, 1)):
                wv4 = WT(sl_).rearrange("p (g k n) -> p g k n", g=2, k=8)
                pbank = nextbank()
                for kc in range(8):
                    T.op("pe", lambda: nc.tensor.matmul(ps[pbank][:], lhsT=wv4[:, piece, kc, :], rhs=uT[:, kc, :],
                                                        start=(kc == 0), stop=(kc == 7)),
                         rd=[WK(sl_), ("uT", kc)], wr=[("ps", pbank)], inc=(kc == 7))
                banks.append(pbank)
            SA = dict(xc=g["xc"], xk="g_xc", acc=g["acc"], ak="g_acc", rr=g["rr"], rk="rden", sqs=0, pb=7, cj=h, bank=banks[0])
            SB_ = dict(xc=g["xc2"], xk="g_xc2", acc=tmpn[:, 0, :], ak=("tmpn", 0), rr=rstd[:, :], rk="rstd", sqs=1, pb=6,
                       cj=8 + h, bank=banks[1])
            cw, halo = g["cw"], g["halo"]

            def st_load(S_):
                T.op("act", lambda: nc.scalar.copy(out=S_["xc"][:, 3:515], in_=ps[S_["bank"]][:]), rd=[("ps", S_["bank"])],
                     wr=[S_["xk"]])
                if b == 0:
                    T.op("dve", lambda: nc.vector.memset(S_["xc"][:, 0:3], 0.0), wr=[S_["xk"]])
                else:
                    T.op("dve", lambda: nc.vector.tensor_copy(out=S_["xc"][:, 0:3], in_=halo[:, S_["cj"], :]),
                         rd=[("g_halo", S_["cj"])], wr=[S_["xk"]])
                T.op("dve", lambda: nc.vector.tensor_copy(out=halo[:, S_["cj"], :], in_=S_["xc"][:, 512:515]), rd=[S_["xk"]],
                     wr=[("g_halo", S_["cj"])])

            def st_tap0(S_):
                T.op("act", lambda: nc.scalar.activation(out=S_["acc"], in_=S_["xc"][:, 0:512], func=AF.Copy,
                                                         scale=cw[:, S_["cj"], 0:1]),
                     rd=[S_["xk"], "g_cw"], wr=[S_["ak"]])

            def st_taps(S_):
                for k in range(1, 4):
                    T.op("dve", lambda: nc.vector.scalar_tensor_tensor(out=S_["acc"], in0=S_["xc"][:, k:k + 512],
                                                                       scalar=cw[:, S_["cj"], k:k + 1], in1=S_["acc"],
                                                                       op0=ALU.mult, op1=ALU.add),
                         rd=[S_["xk"], "g_cw", S_["ak"]], wr=[S_["ak"]])

            def st_silu(S_, dst=None, dkey=None):
                d_ = S_["acc"] if dst is None else dst
                k_ = S_["ak"] if dkey is None else dkey
                T.op("act", lambda: nc.scalar.activation(out=d_, in_=S_["acc"], func=AF.Silu), rd=[S_["ak"]], wr=[k_])

            def st_sq(S_):
                T.op("act", lambda: nc.scalar.activation(out=sq[:, S_["sqs"], :], in_=S_["acc"], func=AF.Square),
                     rd=[S_["ak"]], wr=[("sq", S_["sqs"])])
                T.op("pe", lambda: nc.tensor.matmul(ps[S_["pb"]][:], lhsT=onesb, rhs=sq[:, S_["sqs"], :], start=True, stop=True),
                     rd=["cb", ("sq", S_["sqs"])], wr=[("ps", S_["pb"])])

            def st_sqrt(S_):
                T.op("act", lambda: nc.scalar.activation(out=S_["rr"], in_=ps[S_["pb"]][:], func=AF.Sqrt, bias=epsb[:]),
                     rd=[("ps", S_["pb"]), "epsb"], wr=[S_["rk"]])

            def st_recip(S_):
                T.op("dve", lambda: nc.vector.reciprocal(out=S_["rr"], in_=S_["rr"]), rd=[S_["rk"]], wr=[S_["rk"]])

            for fn_ in (st_load, st_tap0, st_taps, st_silu, st_sq, st_sqrt, st_recip):
                fn_(SA)
                fn_(SB_)
            T.op("dve", lambda: nc.vector.scalar_tensor_tensor(out=g["qnT"], in0=SA["acc"], scalar=128.0 ** -0.5, in1=SA["rr"],
                                                               op0=ALU.mult, op1=ALU.mult),
                 rd=[SA["ak"], SA["rk"]], wr=["g_qnT"])
            T.op("dve", lambda: nc.vector.tensor_tensor(out=g["knf"], in0=SB_["acc"], in1=SB_["rr"], op=ALU.mult),
                 rd=[SB_["ak"], SB_["rk"]], wr=["g_knf"])
            T.op("pool", lambda: nc.gpsimd.tensor_tensor(out=g["qgT"], in0=g["qnT"], in1=g["EG"], op=ALU.mult),
                 rd=["g_qnT", "g_EG"], wr=["g_qgT"])
            T.op("act", lambda: nc.scalar.copy(out=g["knT"], in_=g["knf"]), rd=["g_knf"], wr=["g_knT"])
            T.op("pool", lambda: nc.gpsimd.tensor_tensor(out=g["kbT"], in0=g["knf"], in1=g["Bb"], op=ALU.mult),
                 rd=["g_knf", "g_Bb"], wr=["g_kbT"])
            SV = dict(SA)
            SV["cj"] = 16 + h
            SV["bank"] = banks[2]
            st_load(SV)
            st_tap0(SV)
            st_taps(SV)
            st_silu(SV, g["vsf"], "g_vsf")
            T.op("act", lambda: nc.scalar.activation(out=g["zs"], in_=ps[banks[3]][:], func=AF.Silu),
                 rd=[("ps", banks[3])], wr=["g_zs"])
            pL, pLT = nextbank(), nextbank()
            for n in range(4):
                cs = slice(n * 128, (n + 1) * 128)
                T.op("pe", lambda: nc.tensor.matmul(ps[pL][:, cs], lhsT=g["kbT"][:, cs], rhs=g["knT"][:, cs],
                                                    start=True, stop=True),
                     rd=["g_kbT", "g_knT"], wr=[("ps", pL)], inc=(n == 3))
            for n in range(4):
                cs = slice(n * 128, (n + 1) * 128)
                T.op("pe", lambda: nc.tensor.matmul(ps[pLT][:, cs], lhsT=g["knT"][:, cs], rhs=g["kbT"][:, cs],
                                                    start=True, stop=True),
                     rd=["g_kbT", "g_knT"], wr=[("ps", pLT)], inc=(n == 3))
            T.op("dve", lambda: nc.vector.tensor_tensor(out=g["Ar"][:, 0, :], in0=ps[pL][:], in1=g["E1"], op=ALU.mult),
                 rd=[("ps", pL), "g_E1"], wr=[("g_A", 0)])
            T.op("dve", lambda: nc.vector.tensor_tensor(out=g["ATr"][:, 0, :], in0=ps[pLT][:], in1=g["E2"], op=ALU.mult),
                 rd=[("ps", pLT), "g_E2"], wr=[("g_AT", 0)])
            for n in range(4):
                cs = slice(n * 128, (n + 1) * 128)
                T.op("dve", lambda: nc.vector.tensor_tensor(out=g["Xr"][:, cs], in0=identf[:], in1=g["AT"][:, 0, cs],
                                                            op=ALU.subtract),
                     rd=["identf", ("g_AT", 0)], wr=["g_X"])
            for k in range(1, 7):
                cur, prv = k % 2, (k - 1) % 2
                p1 = nextbank()
                for n in range(4):
                    cs = slice(n * 128, (n + 1) * 128)
                    T.op("pe", lambda: nc.tensor.matmul(ps[p1][:, cs], lhsT=g["ATr"][:, prv, cs], rhs=g["Ar"][:, prv, cs],
                                                        start=True, stop=True),
                         rd=[("g_A", prv), ("g_AT", prv)], wr=[("ps", p1)], inc=(n == 3))
                if k < 6:
                    p2 = nextbank()
                    for n in range(4):
                        cs = slice(n * 128, (n + 1) * 128)
                        T.op("pe", lambda: nc.tensor.matmul(ps[p2][:, cs], lhsT=g["Ar"][:, prv, cs], rhs=g["ATr"][:, prv, cs],
                                                            start=True, stop=True),
                             rd=[("g_A", prv), ("g_AT", prv)], wr=[("ps", p2)], inc=(n == 3))
                T.op("act", lambda: nc.scalar.copy(out=g["Ar"][:, cur, :], in_=ps[p1][:]), rd=[("ps", p1)], wr=[("g_A", cur)])
                if k < 6:
                    T.op("dve", lambda: nc.vector.tensor_copy(out=g["ATr"][:, cur, :], in_=ps[p2][:]), rd=[("ps", p2)],
                         wr=[("g_AT", cur)])
                p3 = nextbank()
                for n in range(4):
                    cs = slice(n * 128, (n + 1) * 128)
                    T.op("pe", lambda: nc.tensor.matmul(ps[p3][:, cs], lhsT=g["Ar"][:, cur, cs], rhs=g["Xr"][:, cs],
                                                        start=True, stop=True),
                         rd=[("g_A", cur), "g_X"], wr=[("ps", p3)], inc=(n == 3))
                T.op("dve", lambda: nc.vector.tensor_tensor(out=g["Xr"], in0=g["X"], in1=ps[p3][:], op=ALU.add),
                     rd=["g_X", ("ps", p3)], wr=["g_X"])
            pk_, pv_ = nextbank(), nextbank()
            for n in range(4):
                cs = slice(n * 128, (n + 1) * 128)
                T.op("pe", lambda: nc.tensor.transpose(out=ps[pk_][:, cs], in_=g["knf"][:, cs], identity=identf[:]),
                     rd=["g_knf", "identf"], wr=[("ps", pk_)], inc=(n == 3))
            for n in range(4):
                cs = slice(n * 128, (n + 1) * 128)
                T.op("pe", lambda: nc.tensor.transpose(out=ps[pv_][:, cs], in_=g["vsf"][:, cs], identity=identf[:]),
                     rd=["g_vsf", "identf"], wr=[("ps", pv_)], inc=(n == 3))
            for n in range(4):
                cs = slice(n * 128, (n + 1) * 128)
                T.op("dve", lambda: nc.vector.tensor_scalar(out=g["Rwr"][:, cs], in0=ps[pk_][:, cs], scalar1=g["bgc"][:, n, h:h + 1],
                                                            scalar2=None, op0=ALU.mult),
                     rd=[("ps", pk_), "g_bgc"], wr=["g_Rw"])
                T.op("dve", lambda: nc.vector.tensor_scalar(out=g["kg"][:, cs], in0=ps[pk_][:, cs], scalar1=g["kgc"][:, n:n + 1],
                                                            scalar2=None, op0=ALU.mult),
                     rd=[("ps", pk_), "g_kgc"], wr=["g_kg"])
                T.op("act", lambda: nc.scalar.activation(out=g["Rur"][:, cs], in_=ps[pv_][:, cs], func=AF.Copy,
                                                         scale=g["gcol"][:, n, 8 + h:9 + h]),
                     rd=[("ps", pv_), "g_gcol"], wr=["g_Ru"])
            pu, pw = nextbank(), nextbank()
            for n in range(4):
                cs = slice(n * 128, (n + 1) * 128)
                T.op("pe", lambda: nc.tensor.matmul(ps[pu][:, cs], lhsT=g["Xr"][:, cs], rhs=g["Rur"][:, cs], start=True, stop=True),
                     rd=["g_X", "g_Ru"], wr=[("ps", pu)], inc=(n == 3))
            for n in range(4):
                cs = slice(n * 128, (n + 1) * 128)
                T.op("pe", lambda: nc.tensor.matmul(ps[pw][:, cs], lhsT=g["Rwr"][:, cs], rhs=g["Xr"][:, cs], start=True, stop=True),
                     rd=["g_X", "g_Rw"], wr=[("ps", pw)], inc=(n == 3))
            T.op("act", lambda: nc.scalar.copy(out=g["u"], in_=ps[pu][:]), rd=[("ps", pu)], wr=["g_u"])
            T.op("dve", lambda: nc.vector.tensor_copy(out=g["wT"], in_=ps[pw][:]), rd=[("ps", pw)], wr=["g_wT"])
            pp = nextbank()
            for n in range(4):
                cs = slice(n * 128, (n + 1) * 128)
                T.op("pe", lambda: nc.tensor.matmul(ps[pp][:, cs], lhsT=g["knT"][:, cs], rhs=g["qnT"][:, cs], start=True, stop=True),
                     rd=["g_knT", "g_qnT"], wr=[("ps", pp)], inc=(n == 3))
            T.op("dve", lambda: nc.vector.tensor_tensor(out=g["pTg"], in0=ps[pp][:], in1=g["E3"], op=ALU.mult),
                 rd=[("ps", pp), "g_E3"], wr=["g_pTg"])
            pO = 5
            T.op("act", lambda: nc.scalar.copy(out=g["Sb"], in_=g["S"][:, h, :]), rd=[("g_S", h)], wr=["g_Sb"])
            for n in range(4):
                cs = slice(n * 128, (n + 1) * 128)
                v2 = n % 2
                pvn = nextbank()
                T.op("pe", lambda: nc.tensor.matmul(ps[pvn][:, 0:128], lhsT=g["wT"][:, cs], rhs=g["Sb"],
                                                    start=True, stop=True),
                     rd=["g_wT", "g_Sb"], wr=[("ps", pvn)])
                T.op("dve", lambda: nc.vector.tensor_tensor(out=g["vnb"][:, v2, :], in0=g["u"][:, cs], in1=ps[pvn][:, 0:128],
                                                            op=ALU.subtract),
                     rd=["g_u", ("ps", pvn)], wr=[("g_vnb", v2)])
                T.op("pe", lambda: nc.tensor.matmul(ps[pO][:, cs], lhsT=g["Sb"], rhs=g["qgT"][:, cs],
                                                    start=True, stop=False),
                     rd=["g_Sb", "g_qgT"], wr=[("ps", pO)], inc=False)
                T.op("pe", lambda: nc.tensor.matmul(ps[pO][:, cs], lhsT=g["vnb"][:, v2, :], rhs=g["pTg"][:, cs],
                                                    start=False, stop=True),
                     rd=[("g_vnb", v2), "g_pTg"], wr=[("ps", pO)])
                pst = nextbank()
                T.op("pe", lambda: nc.tensor.matmul(ps[pst][:, 0:128], lhsT=g["kg"][:, cs], rhs=g["vnb"][:, v2, :],
                                                    start=True, stop=True),
                     rd=["g_kg", ("g_vnb", v2)], wr=[("ps", pst)])
                T.op("dve", lambda: nc.vector.scalar_tensor_tensor(out=g["S"][:, h, :], in0=g["S"][:, h, :],
                                                                   scalar=g["glc"][:, n:n + 1], in1=ps[pst][:, 0:128],
                                                                   op0=ALU.mult, op1=ALU.add),
                     rd=[("g_S", h), "g_glc", ("ps", pst)], wr=[("g_S", h)])
                T.op("act", lambda: nc.scalar.copy(out=g["Sb"], in_=g["S"][:, h, :]), rd=[("g_S", h)],
                     wr=["g_Sb"])
            T.op("act", lambda: nc.scalar.activation(out=sq[:, 0, :], in_=ps[pO][:], func=AF.Square), rd=[("ps", pO)],
                 wr=[("sq", 0)])
            T.op("pe", lambda: nc.tensor.matmul(ps[7][:], lhsT=onesb, rhs=sq[:, 0, :], start=True, stop=True),
                 rd=["cb", ("sq", 0)], wr=[("ps", 7)])
            T.op("act", lambda: nc.scalar.activation(out=g["rr"], in_=ps[7][:], func=AF.Sqrt, scale=1.0 / 128, bias=epsb[:]),
                 rd=[("ps", 7), "epsb"], wr=["rden"])
            T.op("dve", lambda: nc.vector.reciprocal(out=g["rr"], in_=g["rr"]), rd=["rden"], wr=["rden"])
            T.op("dve", lambda: nc.vector.scalar_tensor_tensor(out=g["acc"], in0=ps[pO][:], scalar=g["gain"], in1=g["rr"],
                                                               op0=ALU.mult, op1=ALU.mult),
                 rd=[("ps", pO), "g_gain", "rden"], wr=["g_acc"])
            T.op("dve", lambda: nc.vector.tensor_tensor(out=catT[:, h, :], in0=g["acc"], in1=g["zs"], op=ALU.mult),
                 rd=["g_acc", "g_zs"], wr=[("catT", h)])
        proj_fm("gdn_w_in", jg, 4112, 4, uT, "uT", TB, qmem_handler)

    def emit():
        T.dma("sp", c_io, identf[:], identf_d, wr=["identf"])
        T.dma("sp", c_io, gains[:], gains_d, wr=["gains"])
        T.dma("sp", c_io, cb[:], cb_d, wr=["cb"])
        T.op("pool", lambda: nc.gpsimd.memset(epsb[:], EPS), wr=["epsb"])
        T.op("pool", lambda: nc.gpsimd.memset(oneb[:], 1.0), wr=["oneb"])
        prep_mem()
        for t in range(16):
            sl = t % 2
            T.dma("sp", c_io, xin(sl), x_d[t * 128:(t + 1) * 128, :], wr=xink(sl))
            for half in range(2):
                pbn = nextbank()
                pk = ("ps", pbn)
                for q in range(4):
                    c = half * 4 + q
                    T.op("pe", lambda: nc.tensor.transpose(out=ps[pbn][:, q * 128:(q + 1) * 128],
                                                           in_=xin(sl)[:, c * 128:(c + 1) * 128],
                                                           identity=identf[:]),
                         rd=xink(sl) + ["identf"], wr=[pk], inc=(q == 3))
                dst = hT[:, half * 4:(half + 1) * 4, t * 128:(t + 1) * 128]
                src = ps[pbn][:].rearrange("p (q n) -> p q n", q=4)
                wr = [("hT", c, t // 4) for c in range(half * 4, half * 4 + 4)]
                if half == 0:
                    T.op("act", lambda: nc.scalar.copy(out=dst, in_=src), rd=[pk], wr=wr)
                else:
                    T.op("dve", lambda: nc.vector.tensor_copy(out=dst, in_=src), rd=[pk], wr=wr)
        for l in layers:
            kind = l % 3
            T.barrier()
            layer_alloc(kind)
            w_block(l, None)
            mem_kv(l)
            for b in range(NB):
                w_block(l, b)
                pre_norm(l, 0, b)
                if kind == 2:
                    if b == 0:
                        sb_setup()
                    sb_inproj(l, b)
                    sb_attn(b)
                elif kind == 1:
                    if b == 0:
                        ml_setup()
                    ml_inproj(l, b)
                    ml_attn(l, b)
                else:
                    if b == 0:
                        gdn_setup(l)
                    gdn_block(l, b)
                mem_attn(b)
                out_proj(l, b)
                ffn(l, b)
            T.barrier()
            layer_free()
        for t in range(16):
            sl = t % 2
            for half in range(2):
                pbn = nextbank()
                pk = ("ps", pbn)
                for q in range(4):
                    c = half * 4 + q
                    T.op("pe", lambda: nc.tensor.transpose(out=ps[pbn][:, q * 128:(q + 1) * 128],
                                                           in_=hT[:, c, t * 128:(t + 1) * 128],
                                                           identity=identf[:]),
                         rd=[("hT", c, t // 4), "identf"], wr=[pk], inc=(q == 3))
                dst = xin(sl)[:, half * 512:(half + 1) * 512]
                if half == 0:
                    T.op("act", lambda: nc.scalar.copy(out=dst, in_=ps[pbn][:]), rd=[pk], wr=[("ysb", 2 * sl + half)])
                else:
                    T.op("dve", lambda: nc.vector.tensor_copy(out=dst, in_=ps[pbn][:]), rd=[pk],
                         wr=[("ysb", 2 * sl + half)])
            T.dma("sp", c_io, out_d[t * 128:(t + 1) * 128, :], xin(sl), rd=xink(sl), wr=[("out", t)])
        T.wait_key("sp", [("out", t) for t in range(16)])

    T.dry = True
    emit()
    T.dry = False
    ring["i"] = 0
    wstate["nf32"] = 0
    wstate["nb16"] = 0
    wstate["slot_of"] = {}
    assert max(x[2] for x in wplan) < NWC
    emit()
    P.finish()
    return nc, T


CB_ONES = 0
CB_NEGONES = 128
CB_IDENT = 256
CB_NEGMINCL = 384
CB_MASK01 = 512
NCB = CB_MASK01 + 4 * TB
G_MEM = DEPTH * 4 * 8
NG = G_MEM + 8


def host_consts(inputs):
    g = np.stack([inputs["norm_pre_mix"], inputs["norm_post_mix"], inputs["norm_pre_ffn"],
                  inputs["norm_post_ffn"]], axis=1)
    g = g.reshape(DEPTH, 4, 8, 128).transpose(3, 0, 1, 2).reshape(128, DEPTH * 4 * 8)
    gm = inputs["mem_norm"].reshape(8, 128).T
    gains = np.concatenate([g, gm], axis=1).astype(np.float32)
    cbf = np.zeros((128, NCB), np.float32)
    cbf[:, CB_ONES:CB_ONES + 128] = 1.0
    cbf[:, CB_NEGONES:CB_NEGONES + 128] = -1.0
    cbf[:, CB_IDENT:CB_IDENT + 128] = np.eye(128)
    jj, ss = np.meshgrid(np.arange(128), np.arange(128), indexing="ij")
    cbf[:, CB_NEGMINCL:CB_NEGMINCL + 128] = -(jj >= ss).astype(np.float32)
    sidx = np.arange(128)[:, None]
    tidx = np.arange(TB)[None, :]
    for i in range(4):
        m = ((sidx + 128 * i) < tidx).astype(np.float32)
        cbf[:, CB_MASK01 + i * TB:CB_MASK01 + (i + 1) * TB] = m
    sel = np.zeros((16, 8, 128), np.float32)
    for h in range(8):
        sel[8 + h, h, :] = 1.0
    m01i = np.zeros((128, 4 * TB), np.float32)
    for i in range(4):
        m01i[:, i * TB:(i + 1) * TB] = ((sidx + 128 * i) <= tidx)
    mlbias = np.concatenate([inputs["ml_i_bias"][0], inputs["ml_f_bias"][0]]).reshape(16, 1).astype(np.float32)
    sel8 = np.zeros((8, 8, 128), np.float32)
    for h in range(8):
        sel8[h, h, :] = 1.0
    pidx = np.arange(128)[:, None]
    fidx = np.arange(128)[None, :]
    gm = np.concatenate([np.tile(np.where(fidx < pidx, 0.0, NEGBIG), (1, 4)),
                         np.tile(np.where(pidx < fidx, 0.0, NEGBIG), (1, 4)),
                         np.tile(np.where(pidx <= fidx, 0.0, NEGBIG), (1, 4))], axis=1).astype(np.float32)
    gcw = inputs["gdn_conv"].reshape(2, 4, 24, 128).transpose(0, 3, 2, 1).reshape(2, 128, 96)
    return {
        "sel8": sel8.reshape(8, 8 * 128),
        "gdn_masks": gm.astype(ml_dtypes.bfloat16),
        "gdn_cw": np.ascontiguousarray(gcw.astype(np.float32)),
        "gdn_dtb": np.ascontiguousarray(inputs["gdn_dt_bias"].reshape(2, 8, 1).astype(np.float32)),
        "gdn_alog": np.ascontiguousarray(inputs["gdn_a_log"].reshape(2, 8, 1).astype(np.float32)),
        "gdn_gain": np.ascontiguousarray(inputs["gdn_out_norm"].reshape(2, 128, 1).astype(np.float32)),
        "selc": sel.reshape(16, 8 * 128),
        "mlbias": mlbias,
        "mlgain": np.ascontiguousarray(inputs["ml_out_norm"][0].reshape(128, 1).astype(np.float32)),
        "mask01incl": m01i.astype(ml_dtypes.bfloat16),
        "identf": np.eye(128, dtype=np.float32),
        "gains": np.ascontiguousarray(gains),
        "cbf": cbf.astype(ml_dtypes.bfloat16),
    }


WNAMES = ["w_ffn_in", "w_ffn_out", "w_mem_kv", "w_out", "gdn_w_in", "ml_w_in", "sb_w_in"]


def make_in_maps(inputs, cores, x_override=None):
    consts = host_consts(inputs)
    in_maps = []
    for core in cores:
        m = dict(consts)
        m["x"] = np.ascontiguousarray(inputs["x"][core] if x_override is None else x_override[core])
        m["mem"] = np.ascontiguousarray(inputs["mem"][core])
        for w in WNAMES:
            m[w] = inputs[w]
        in_maps.append(m)
    return in_maps


def kernel(**inputs):
    inputs = {k: np.asarray(v) for k, v in inputs.items()}
    nc, _ = build(list(range(DEPTH)))
    in_maps = make_in_maps(inputs, list(range(8)))
    res = run_bass_kernel_spmd(nc, in_maps, core_ids=list(range(8)))
    return np.stack([r["out"] for r in res.results], axis=0)
```

```python
import numpy as np
import ml_dtypes
import concourse.bass as bass
import concourse.mybir as mybir
from concourse.bass_utils import run_bass_kernel_spmd

F32 = mybir.dt.float32
BF16 = mybir.dt.bfloat16
F32R = mybir.dt.float32r
AF = mybir.ActivationFunctionType
ALU = mybir.AluOpType

S = 2048
D = 1024
NB = 4
TB = 512
DEPTH = 4
N_MEM = 256
D_FF = 2816
EPS = 1e-6
SEM_LIMIT = 30000


class Counter:
    def __init__(self, tr, name, inorder):
        self.tr = tr
        self.name = name
        self.inorder = inorder
        self.sems = []
        self.final = {}
        self.epoch = -1
        self.val = 0
        self._new_epoch()

    def _new_epoch(self):
        if self.epoch >= 0:
            self.final[self.epoch] = self.val
        self.epoch += 1
        self.val = 0
        self.sems.append(self.tr.new_sem(f"{self.name}_{self.epoch}"))

    def reserve(self, amount):
        if self.val + amount > SEM_LIMIT:
            self._new_epoch()
        self.val += amount
        return (self.epoch, self.val)

    def peek_next(self, amount=1):
        if self.val + amount > SEM_LIMIT:
            return (self.epoch + 1, amount)
        return (self.epoch, self.val + amount)


class Tracker:
    def __init__(self, nc):
        self.nc = nc
        self._sem_stack = []
        self.eng = {"pe": nc.tensor, "act": nc.scalar, "dve": nc.vector, "pool": nc.gpsimd, "sp": nc.sync}
        self.cnt = {k: Counter(self, k, True) for k in self.eng}
        self.waited = {k: {} for k in self.eng}
        self.last_w = {}
        self.readers = {}
        self.pending_noinc = {k: False for k in self.eng}
        self.n_inst = {k: 0 for k in self.eng}
        self.dry = False

    def new_sem(self, name):
        cm = self.nc.semaphore(name)
        h = cm.__enter__()
        self._sem_stack.append(cm)
        return h

    def new_counter(self, name):
        c = Counter(self, name, False)
        self.cnt[name] = c
        return c

    def _need(self, deps, cnt, tok):
        k = (cnt.name, tok[0])
        if k not in deps or deps[k][1] < tok[1]:
            deps[k] = (cnt, tok[1])

    def _wait_all(self, e, rd, wr):
        deps = {}
        for r in rd:
            lw = self.last_w.get(r)
            if lw is not None:
                self._need(deps, lw[0], lw[1])
        for w in wr:
            lw = self.last_w.get(w)
            if lw is not None:
                self._need(deps, lw[0], lw[1])
            for (c, tok) in self.readers.get(w, {}).values():
                self._need(deps, c, tok)
        wd = self.waited[e]
        for (cname, ep), (cnt, val) in deps.items():
            if cname == e and e == "pe":
                continue
            if wd.get((cname, ep), 0) >= val:
                continue
            if cnt.inorder:
                if any(k[0] == cname and k[1] > ep for k in wd):
                    continue
            self.eng[e].wait_ge(cnt.sems[ep], val)
            wd[(cname, ep)] = val

    def _record(self, cnt, tok, rd, wr):
        for r in rd:
            self.readers.setdefault(r, {})[cnt.name] = (cnt, tok)
        for w in wr:
            self.last_w[w] = (cnt, tok)
            self.readers[w] = {}

    def op(self, e, fn, rd=(), wr=(), inc=True):
        if self.dry:
            return None
        self._wait_all(e, rd, wr)
        inst = fn()
        self.n_inst[e] += 1
        cnt = self.cnt[e]
        if inc:
            tok = cnt.reserve(1)
            inst.then_inc(cnt.sems[tok[0]], 1)
        else:
            tok = cnt.peek_next(1)
        self._record(cnt, tok, rd, wr)
        return inst

    def dma(self, q, cnt, out, in_, rd=(), wr=()):
        if self.dry:
            return None
        self._wait_all(q, rd, wr)
        inst = self.eng[q].dma_start(out=out, in_=in_)
        tok = cnt.reserve(16)
        inst.then_inc(cnt.sems[tok[0]], 16)
        self.n_inst[q] += 1
        self._record(cnt, tok, rd, wr)
        return inst

    def alias(self, dst_keys, src_keys):
        if self.dry:
            return
        acc = {}
        for s in src_keys:
            lw = self.last_w.get(s)
            items = list(self.readers.get(s, {}).values())
            if lw is not None:
                items.append(lw)
            for (c, tok) in items:
                k = (c.name, tok[0])
                if k not in acc or acc[k][1][1] < tok[1]:
                    acc[k] = (c, tok)
        for d in dst_keys:
            rd = self.readers.setdefault(d, {})
            for (cname, ep), (c, tok) in acc.items():
                nm = f"{cname}@{ep}"
                if nm not in rd or rd[nm][1][1] < tok[1]:
                    rd[nm] = (c, tok)

    def barrier(self):
        if self.dry:
            return
        for e in self.eng:
            wd = self.waited[e]
            for c in self.cnt.values():
                eps = range(c.epoch + 1) if not c.inorder else [c.epoch]
                for ep in eps:
                    val = c.val if ep == c.epoch else c.final[ep]
                    if val <= 0 or wd.get((c.name, ep), 0) >= val:
                        continue
                    if c.name == e and e == "pe":
                        continue
                    self.eng[e].wait_ge(c.sems[ep], val)
                    wd[(c.name, ep)] = val

    def wait_key(self, e, keys):
        if self.dry:
            return
        self._wait_all(e, keys, ())

    def close(self):
        for cm in reversed(self._sem_stack):
            cm.__exit__(None, None, None)


class Prog:
    def __init__(self, layers, load_x=True):
        self.layers = layers
        self.nc = bass.Bass("TRN2", target_bir_lowering=False)
        self._stack = []
        self.T = Tracker(self.nc)

    def dram(self, name, shape, dt, kind):
        return self.nc.dram_tensor(name, list(shape), dt, kind=kind).ap()

    def sb(self, name, shape, dt):
        cm = self.nc.sbuf_tensor(name, list(shape), dt)
        t = cm.__enter__()
        self._stack.append(cm)
        return t

    def psum(self, name, shape, dt):
        cm = self.nc.psum_tensor(name, list(shape), dt)
        t = cm.__enter__()
        self._stack.append(cm)
        return t

    def finish(self):
        for cm in reversed(self._stack):
            cm.__exit__(None, None, None)
        self.T.close()


GDN_IN = 4624
ML_IN = 3600
SB_IN = 3584
NWS = 3
NWST = 2
NEGBIG = -30000.0


def build(layers, first=True, last=True):
    P = Prog(layers)
    nc, T = P.nc, P.T
    kinds = [l % 3 for l in layers]
    x_d = P.dram("x", [S, D], F32, "ExternalInput")
    out_d = P.dram("out", [S, D], F32, "ExternalOutput")
    mem_d = P.dram("mem", [N_MEM, D], F32, "ExternalInput")
    identf_d = P.dram("identf", [128, 128], F32, "ExternalInput")
    cb_d = P.dram("cbf", [128, NCB], BF16, "ExternalInput")
    gains_d = P.dram("gains", [128, NG], F32, "ExternalInput")
    W = {
        "w_ffn_in": P.dram("w_ffn_in", [DEPTH, D, 2 * D_FF], F32, "ExternalInput"),
        "w_ffn_out": P.dram("w_ffn_out", [DEPTH, D_FF, D], F32, "ExternalInput"),
        "w_mem_kv": P.dram("w_mem_kv", [DEPTH, D, 1024], F32, "ExternalInput"),
        "w_out": P.dram("w_out", [DEPTH, 1536, D], F32, "ExternalInput"),
        "gdn_w_in": P.dram("gdn_w_in", [2, D, GDN_IN], F32, "ExternalInput"),
        "ml_w_in": P.dram("ml_w_in", [1, D, ML_IN], F32, "ExternalInput"),
        "sb_w_in": P.dram("sb_w_in", [1, D, SB_IN], F32, "ExternalInput"),
    }

    hT = P.sb("hT", [128, 8, S], F32)
    uT = P.sb("uT", [128, 8, TB], BF16)
    sq = P.sb("sq", [128, 2, TB], BF16)
    rstd = P.sb("rstd", [128, TB], F32)
    ysb_raw = P.sb("ysb", [128, 8 * TB], F32)
    ysb = ysb_raw[:, :].rearrange("p (c n) -> p c n", c=8)
    R1 = P.sb("R1", [128, 22 * TB], BF16)
    aT = R1[:, :].rearrange("p (j n) -> p j n", j=22)
    sg = P.sb("sg", [128, 1, TB], BF16)
    tmpn = P.sb("tmpn", [128, 1, TB], F32)
    LS = {}
    identf = P.sb("identf_sb", [128, 128], F32)
    cb = P.sb("cb_sb", [128, NCB], BF16)
    gains = P.sb("gains_sb", [128, NG], F32)
    wst = P.sb("wst", [128, NWST, 2048], F32)
    wbf = P.sb("wbf", [128, NWS, 2048], BF16)
    catT = P.sb("catT", [128, 12, TB], BF16)
    qmT = P.sb("qmT", [128, 4, TB], BF16)
    memnT = P.sb("memnT", [128, 8, N_MEM], BF16)
    memkT = P.sb("memkT", [128, 4, N_MEM], BF16)
    memv = P.sb("memv", [128, 2, 512], BF16)
    negkmax = P.sb("negkmax", [128, 4], F32)
    cq = P.sb("cq", [1, TB], F32)
    negc = P.sb("negc", [1, TB], BF16)
    pT = P.sb("pT", [128, 2, TB], BF16)
    rden = P.sb("rden", [128, TB], F32)
    epsb = P.sb("epsb", [128, 1], F32)
    oneb = P.sb("oneb", [128, 1], F32)
    ps = [P.psum(f"ps{i}", [128, TB], F32) for i in range(8)]

    onesb = cb[:, CB_ONES:CB_ONES + 128]
    negones = cb[:, CB_NEGONES:CB_NEGONES + 128]
    identb = cb[:, CB_IDENT:CB_IDENT + 128]
    negMincl = cb[:, CB_NEGMINCL:CB_NEGMINCL + 128]

    c_io = T.new_counter("io")
    c_w = [T.new_counter(f"w{i}") for i in range(NWST)]

    def gain(l, which, c):
        i = (l * 4 + which) * 8 + c
        return gains[:, i:i + 1]

    def xin(sl):
        return ysb[:, 2 * sl:2 * sl + 2, :].rearrange("p a b -> p (a b)")

    def xink(sl):
        return [("ysb", 2 * sl), ("ysb", 2 * sl + 1)]

    ring = {"i": 0}

    def nextbank():
        ring["i"] = (ring["i"] + 1) % 5
        return ring["i"]

    wplan = []
    wstate = {"issued": 0, "next": 0, "lb": None, "idx": 0}
    NWC = 72
    wcache_d = P.dram("wcache", [NWC, 128, 2048], BF16, "Internal")
    c_wb = [T.new_counter(f"wb{i}") for i in range(NWS + 2 * NWST)]
    c_wc = T.new_counter("wc")

    wst_b = wst.bitcast(BF16)
    NSLOT = NWS + 2 * NWST

    def WT(slot):
        if slot < NWS:
            return wbf[:, slot, :]
        j = slot - NWS
        return wst_b[:, j // 2, (j % 2) * 2048:(j % 2 + 1) * 2048]

    def WK(slot):
        if slot < NWS:
            return ("wbf", slot)
        j = slot - NWS
        return ("wst", j // 2, (j % 2) * 1024)

    def w_issue(i):
        req, lb, idx = wplan[i]
        n = sum(kc * ncols for (_, _, _, kc, _, ncols) in req)
        if lb is not None and lb[1] > 0:
            slot = wstate["nb16"] % NSLOT
            wstate["nb16"] += 1
            wstate["slot_of"][i] = slot
            T.dma("sp", c_wb[slot], WT(slot)[:, 0:n], wcache_d[idx, :, 0:n], rd=[("wc", idx)], wr=[WK(slot)])
            return
        slot = wstate["nf32"] % NWS
        st = wstate["nf32"] % NWST
        wstate["nf32"] += 1
        wstate["slot_of"][i] = slot
        off = 0
        for pi, (name, l, r0, kc, c0, ncols) in enumerate(req):
            dst = wst[:, st, off:off + kc * ncols].rearrange("p (k n) -> p k n", k=kc)
            src = W[name][l, r0:r0 + kc * 128, c0:c0 + ncols].rearrange("(k p) n -> p k n", p=128)
            if len(req) == 1:
                wk = [("wst", st, 0), ("wst", st, 1024)]
            else:
                assert kc * ncols <= 1024 and off == pi * 1024
                wk = [("wst", st, pi * 1024)]
            T.dma("sp", c_w[st], dst, src, wr=wk)
            off += kc * ncols
        T.op("act", lambda: nc.scalar.copy(out=wbf[:, slot, 0:n], in_=wst[:, st, 0:n]),
             rd=[("wst", st, 0), ("wst", st, 1024)], wr=[("wbf", slot)])
        if lb is not None:
            T.dma("pool", c_wc, wcache_d[idx, :, 0:n], wbf[:, slot, 0:n], rd=[("wbf", slot)], wr=[("wc", idx)])

    def w_get(req):
        req = tuple(req)
        if T.dry:
            wplan.append((req, wstate["lb"], wstate["idx"]))
            wstate["idx"] += 1
            return 0
        i = wstate["next"]
        assert wplan[i][0] == req, (wplan[i], req)

        def depth(k):
            lb = wplan[k][1]
            return (NSLOT - 1) if (lb is not None and lb[1] > 0) else (NWS - 1)
        def isb16(k):
            lb = wplan[k][1]
            return lb is not None and lb[1] > 0
        while wstate["issued"] < len(wplan):
            k = wstate["issued"]
            if k > i + depth(k) - 1:
                break
            if k > i and isb16(k) != isb16(i):
                break
            w_issue(k)
            wstate["issued"] += 1
        wstate["next"] += 1
        return wstate["slot_of"][i]

    def w_block(l, b):
        wstate["lb"] = (l, b) if b is not None else None
        wstate["idx"] = 0

    def wview(slot, off, kc, ncols):
        return WT(slot)[:, off:off + kc * ncols].rearrange("p (k n) -> p k n", k=kc)

    lcount = {"n": 0}

    def layer_alloc(kind):
        lcount["n"] += 1
        tag = lcount["n"]
        cms = []

        def mk(name, shape, dt):
            cm = nc.sbuf_tensor(f"{name}_{tag}", list(shape), dt)
            t = cm.__enter__()
            cms.append(cm)
            return t
        if kind == 0:
            LS["gF"] = mk("gF", [128, 3584], F32)
            LS["gR2"] = mk("gR2", [128, 2944], F32)
            gdn_alloc()
        else:
            LS["R2"] = mk("R2", [128, 12544], BF16)
            LS["R2f"] = LS["R2"].bitcast(F32)
            if kind == 2:
                sb_alloc()
            else:
                ml_alloc()
        LS["cms"] = cms

    def layer_free():
        for cm in reversed(LS["cms"]):
            cm.__exit__(None, None, None)
        LS["cms"] = []

    def rms_stats(src_fn, src_keys, n):
        pk = ("ps", 7)
        for c in range(8):
            k = c % 2
            T.op("act", lambda: nc.scalar.activation(out=sq[:, k, 0:n], in_=src_fn(c), func=AF.Square),
                 rd=[src_keys(c)], wr=[("sq", k)])
            T.op("pe", lambda: nc.tensor.matmul(ps[7][:, 0:n], lhsT=onesb, rhs=sq[:, k, 0:n],
                                                start=(c == 0), stop=(c == 7)),
                 rd=["cb", ("sq", k)], wr=[pk])
        T.op("act", lambda: nc.scalar.activation(out=rstd[:, 0:n], in_=ps[7][:, 0:n], func=AF.Sqrt,
                                                 scale=1.0 / D, bias=epsb[:]),
             rd=[pk, "epsb"], wr=["rstd"])
        T.op("dve", lambda: nc.vector.reciprocal(out=rstd[:, 0:n], in_=rstd[:, 0:n]), rd=["rstd"], wr=["rstd"])

    def pre_norm(l, which, b):
        rms_stats(lambda c: hT[:, c, b * TB:(b + 1) * TB], lambda c: ("hT", c, b), TB)
        for c in range(8):
            T.op("dve", lambda: nc.vector.scalar_tensor_tensor(
                out=uT[:, c, :], in0=hT[:, c, b * TB:(b + 1) * TB], scalar=gain(l, which, c),
                in1=rstd[:], op0=ALU.mult, op1=ALU.mult),
                rd=[("hT", c, b), "gains", "rstd"], wr=[("uT", c)])

    def post_norm_add(l, which, b):
        rms_stats(lambda c: ysb[:, c, :], lambda c: ("ysb", c), TB)
        for c in range(8):
            k = 0
            T.op("dve", lambda: nc.vector.scalar_tensor_tensor(
                out=tmpn[:, k, :], in0=ysb[:, c, :], scalar=gain(l, which, c),
                in1=rstd[:], op0=ALU.mult, op1=ALU.mult),
                rd=[("ysb", c), "gains", "rstd"], wr=[("tmpn", k)])
            T.op("pool", lambda: nc.gpsimd.tensor_tensor(
                out=hT[:, c, b * TB:(b + 1) * TB], in0=hT[:, c, b * TB:(b + 1) * TB],
                in1=tmpn[:, k, :], op=ALU.add),
                rd=[("hT", c, b), ("tmpn", k)], wr=[("hT", c, b)])

    def proj_fm(wname, wl, c0, nchunks, xT, xkey, n, handler):
        j = 0
        while j < nchunks:
            nn = min(2, nchunks - j)
            slot = w_get([(wname, wl, 0, 8, c0 + j * 128, nn * 128)])
            wv = wview(slot, 0, 8, nn * 128)
            for jj in range(nn):
                pb = nextbank()
                for kc in range(8):
                    T.op("pe", lambda: nc.tensor.matmul(ps[pb][:, 0:n], lhsT=wv[:, kc, jj * 128:(jj + 1) * 128],
                                                        rhs=xT[:, kc, 0:n], start=(kc == 0), stop=(kc == 7)),
                         rd=[WK(slot), (xkey, kc)], wr=[("ps", pb)], inc=(kc == 7))
                handler(j + jj, pb)
            j += nn

    def proj_tm(wname, wl, c0, ncols, xT, xkey, ntt, handler):
        slot = w_get([(wname, wl, 0, 8, c0, ncols)])
        wv = wview(slot, 0, 8, ncols)
        for tt in range(ntt):
            pb = nextbank()
            for kc in range(8):
                T.op("pe", lambda: nc.tensor.matmul(ps[pb][:, 0:ncols], lhsT=xT[:, kc, tt * 128:(tt + 1) * 128],
                                                    rhs=wv[:, kc, :], start=(kc == 0), stop=(kc == 7)),
                     rd=[WK(slot), (xkey, kc)], wr=[("ps", pb)], inc=(kc == 7))
            handler(tt, pb)

    mixer_keys = []
    ysb_overlay = []

    def ffn(l, b):
        T.alias([("aT", j) for j in range(22)], mixer_keys)
        pre_norm(l, 2, b)
        for j in range(22):
            slot = w_get([("w_ffn_in", l, 0, 8, j * 128, 128), ("w_ffn_in", l, 0, 8, D_FF + j * 128, 128)])
            wv = WT(slot).rearrange("p (g k n) -> p g k n", g=2, k=8)
            pg, pu = nextbank(), nextbank()
            for gi, pbank in ((0, pg), (1, pu)):
                for kc in range(8):
                    T.op("pe", lambda: nc.tensor.matmul(ps[pbank][:], lhsT=wv[:, gi, kc, :], rhs=uT[:, kc, :],
                                                        start=(kc == 0), stop=(kc == 7)),
                         rd=[WK(slot), ("uT", kc)], wr=[("ps", pbank)], inc=(kc == 7))
            k = 0
            T.op("act", lambda: nc.scalar.activation(out=sg[:, k, :], in_=ps[pg][:], func=AF.Silu),
                 rd=[("ps", pg)], wr=[("sg", k)])
            T.op("dve", lambda: nc.vector.tensor_tensor(out=aT[:, j, :], in0=sg[:, k, :], in1=ps[pu][:],
                                                        op=ALU.mult),
                 rd=[("sg", k), ("ps", pu)], wr=[("aT", j)])
        for c in range(8):
            pbank = nextbank()
            for hh in range(2):
                slot = w_get([("w_ffn_out", l, hh * 1408, 11, c * 128, 128)])
                wv = wview(slot, 0, 11, 128)
                for kc in range(11):
                    kk = hh * 11 + kc
                    T.op("pe", lambda: nc.tensor.matmul(ps[pbank][:], lhsT=wv[:, kc, :], rhs=aT[:, kk, :],
                                                        start=(kk == 0), stop=(kk == 21)),
                         rd=[WK(slot), ("aT", kk)], wr=[("ps", pbank)], inc=(kc == 10))
            T.op("act", lambda: nc.scalar.copy(out=ysb[:, c, :], in_=ps[pbank][:]),
                 rd=[("ps", pbank)], wr=[("ysb", c)])
        post_norm_add(l, 3, b)

    def out_proj(l, b):
        T.alias([("ysb", c) for c in range(8)], ysb_overlay)
        for c in range(8):
            slot = w_get([("w_out", l, 0, 12, c * 128, 128)])
            wv = wview(slot, 0, 12, 128)
            pbank = nextbank()
            for kc in range(12):
                T.op("pe", lambda: nc.tensor.matmul(ps[pbank][:], lhsT=wv[:, kc, :], rhs=catT[:, kc, :],
                                                    start=(kc == 0), stop=(kc == 11)),
                     rd=[WK(slot), ("catT", kc)], wr=[("ps", pbank)], inc=(kc == 11))
            T.op("act", lambda: nc.scalar.copy(out=ysb[:, c, :], in_=ps[pbank][:]),
                 rd=[("ps", pbank)], wr=[("ysb", c)])
        post_norm_add(l, 1, b)

    def prep_mem():
        for t in range(2):
            sl = t % 2
            T.dma("sp", c_io, xin(sl), mem_d[t * 128:(t + 1) * 128, :], wr=xink(sl))
            for half in range(2):
                pb = nextbank()
                pk = ("ps", pb)
                for q in range(4):
                    c = half * 4 + q
                    T.op("pe", lambda: nc.tensor.transpose(out=ps[pb][:, q * 128:(q + 1) * 128],
                                                           in_=xin(sl)[:, c * 128:(c + 1) * 128],
                                                           identity=identf[:]),
                         rd=xink(sl) + ["identf"], wr=[pk], inc=(q == 3))
                T.op("dve", lambda: nc.vector.tensor_copy(
                    out=hT[:, half * 4:(half + 1) * 4, t * 128:(t + 1) * 128],
                    in_=ps[pb][:].rearrange("p (q n) -> p q n", q=4)), rd=[pk],
                    wr=[("hT", c, 0) for c in range(half * 4, half * 4 + 4)])
        rms_stats(lambda c: hT[:, c, 0:N_MEM], lambda c: ("hT", c, 0), N_MEM)
        for c in range(8):
            T.op("dve", lambda: nc.vector.scalar_tensor_tensor(
                out=memnT[:, c, :], in0=hT[:, c, 0:N_MEM], scalar=gains[:, G_MEM + c:G_MEM + c + 1],
                in1=rstd[:, 0:N_MEM], op0=ALU.mult, op1=ALU.mult),
                rd=[("hT", c, 0), "gains", "rstd"], wr=[("memnT", c)])

    def mem_kv(l):
        def hk(j, pb):
            T.op("act", lambda: nc.scalar.copy(out=memkT[:, j, :], in_=ps[pb][:, 0:N_MEM]),
                 rd=[("ps", pb)], wr=[("memkT", j)])
            T.op("act", lambda: nc.scalar.activation(out=sq[:, j % 2, 0:N_MEM], in_=ps[pb][:, 0:N_MEM], func=AF.Square),
                 rd=[("ps", pb)], wr=[("sq", j % 2)])
            T.op("pe", lambda: nc.tensor.matmul(ps[6][:, 0:N_MEM], lhsT=onesb, rhs=sq[:, j % 2, 0:N_MEM],
                                                start=True, stop=True),
                 rd=["cb", ("sq", j % 2)], wr=[("ps", 6)])
            T.op("dve", lambda: nc.vector.reduce_max(out=negkmax[:, j:j + 1], in_=ps[6][:, 0:N_MEM],
                                                     axis=mybir.AxisListType.X),
                 rd=[("ps", 6)], wr=[("negkmax", j)])
            T.op("act", lambda: nc.scalar.activation(out=negkmax[:, j:j + 1], in_=negkmax[:, j:j + 1], func=AF.Sqrt),
                 rd=[("negkmax", j)], wr=[("negkmax", j)])
            T.op("dve", lambda: nc.vector.tensor_scalar(out=negkmax[:, j:j + 1], in0=negkmax[:, j:j + 1],
                                                        scalar1=-1.0, scalar2=None, op0=ALU.mult),
                 rd=[("negkmax", j)], wr=[("negkmax", j)])
        proj_fm("w_mem_kv", l, 0, 4, memnT, "memnT", N_MEM, hk)
        for half in range(2):
            def hv(tt, pb):
                T.op("act", lambda: nc.scalar.copy(out=memv[:, tt, half * 256:(half + 1) * 256], in_=ps[pb][:, 0:256]),
                     rd=[("ps", pb)], wr=[("memv", tt, half)])
            proj_tm("w_mem_kv", l, 512 + half * 256, 256, memnT, "memnT", 2, hv)

    def qmem_handler(j, pb):
        T.op("act", lambda: nc.scalar.activation(out=qmT[:, j, :], in_=ps[pb][:], func=AF.Copy, scale=128.0 ** -0.5),
             rd=[("ps", pb)], wr=[("qmT", j)])

    def mem_attn(b):
        for hm in range(4):
            T.op("act", lambda: nc.scalar.activation(out=sq[:, hm % 2, :], in_=qmT[:, hm, :], func=AF.Square),
                 rd=[("qmT", hm)], wr=[("sq", hm % 2)])
            T.op("pe", lambda: nc.tensor.matmul(ps[6][:], lhsT=onesb, rhs=sq[:, hm % 2, :], start=True, stop=True),
                 rd=["cb", ("sq", hm % 2)], wr=[("ps", 6)])
            T.op("act", lambda: nc.scalar.activation(out=cq[0:1, :], in_=ps[6][0:1, :], func=AF.Sqrt),
                 rd=[("ps", 6)], wr=["cq"])
            T.op("dve", lambda: nc.vector.tensor_scalar(out=negc[0:1, :], in0=cq[0:1, :],
                                                        scalar1=negkmax[0:1, hm:hm + 1], scalar2=None, op0=ALU.mult),
                 rd=["cq", ("negkmax", hm)], wr=["negc"])
            for mt in range(2):
                pb = nextbank()
                T.op("pe", lambda: nc.tensor.matmul(ps[pb][:], lhsT=memkT[:, hm, mt * 128:(mt + 1) * 128],
                                                    rhs=qmT[:, hm, :], start=True, stop=False),
                     rd=[("memkT", hm), ("qmT", hm)], wr=[("ps", pb)], inc=False)
                T.op("pe", lambda: nc.tensor.matmul(ps[pb][:], lhsT=onesb[0:1, :], rhs=negc[0:1, :],
                                                    start=False, stop=True),
                     rd=["cb", "negc"], wr=[("ps", pb)])
                T.op("act", lambda: nc.scalar.activation(out=pT[:, mt, :], in_=ps[pb][:], func=AF.Exp),
                     rd=[("ps", pb)], wr=[("pT", mt)])
            po, pd = nextbank(), 6
            for mt in range(2):
                T.op("pe", lambda: nc.tensor.matmul(ps[po][:], lhsT=memv[:, mt, hm * 128:(hm + 1) * 128],
                                                    rhs=pT[:, mt, :], start=(mt == 0), stop=(mt == 1)),
                     rd=[("memv", mt, hm // 2), ("pT", mt)], wr=[("ps", po)], inc=(mt == 1))
            for mt in range(2):
                T.op("pe", lambda: nc.tensor.matmul(ps[pd][:], lhsT=onesb, rhs=pT[:, mt, :],
                                                    start=(mt == 0), stop=(mt == 1)),
                     rd=["cb", ("pT", mt)], wr=[("ps", pd)], inc=(mt == 1))
            T.op("dve", lambda: nc.vector.reciprocal(out=rden[:], in_=ps[pd][:]), rd=[("ps", pd)], wr=["rden"])
            T.op("dve", lambda: nc.vector.tensor_tensor(out=catT[:, 8 + hm, :], in0=ps[po][:], in1=rden[:],
                                                        op=ALU.mult),
                 rd=[("ps", po), "rden"], wr=[("catT", 8 + hm)])

    sbst = {}
    kT_d = P.dram("sb_kT_scr", [8, 128, S], BF16, "Internal")
    v_d = P.dram("sb_v_scr", [8, S, 128], BF16, "Internal")
    c_kv = T.new_counter("kv")

    def sb_alloc():
        R2, R2f = LS["R2"], LS["R2f"]
        def v3(lo):
            return R2[:, lo:lo + 1536].rearrange("p (a n) -> p a n", a=3)
        sbst["qT"] = R2[:, 0:4096].rearrange("p (a n) -> p a n", a=8)
        sbst["Lp"] = v3(4096)
        sbst["attB"] = v3(5632)
        sbst["Pm"] = v3(7168)
        sbst["att"] = v3(8704)
        sbst["e"] = v3(10240)
        sbst["kc"] = R1[:, 0:4096].rearrange("p (a n) -> p a n", a=2)
        sbst["vc"] = R1[:, 4096:8192].rearrange("p (a k n) -> p a k n", a=2, k=16)
        sbst["kst"] = R1[:, 8192:9216].rearrange("p (a n) -> p a n", a=2)
        sbst["vst"] = R1[:, 9216:9728].rearrange("p (a n) -> p a n", a=2)

    def sb_setup():
        T.dma("sp", c_io, cb[:, CB_MASK01:CB_MASK01 + 4 * TB], cb_d[:, CB_MASK01:CB_MASK01 + 4 * TB], wr=["cb"])

    def sb_inproj(l, b):
        qT, kst, vst = sbst["qT"], sbst["kst"], sbst["vst"]
        mixer_keys[:] = [("sb_kc", 0), ("sb_kc", 1), ("sb_vc", 0), ("sb_vc", 1), ("sb_kst", 0), ("sb_kst", 1), ("sb_vst", 0), ("sb_vst", 1)]
        T.alias(mixer_keys, [("aT", j) for j in range(22)])

        def hq(j, pb):
            T.op("act", lambda: nc.scalar.activation(out=qT[:, j, :], in_=ps[pb][:], func=AF.Copy, scale=0.125),
                 rd=[("ps", pb)], wr=[("sb_qT", j)])

        def hk(j, pb):
            k2 = j % 2
            T.op("dve", lambda: nc.vector.tensor_copy(out=kst[:, k2, :], in_=ps[pb][:]),
                 rd=[("ps", pb)], wr=[("sb_kst", k2)])
            T.dma("sp", c_kv, kT_d[j, :, b * TB:(b + 1) * TB], kst[:, k2, :], rd=[("sb_kst", k2)],
                  wr=[("kT_d", j, b)])
        proj_fm("sb_w_in", 0, 0, 8, uT, "uT", TB, hq)
        proj_fm("sb_w_in", 0, 1024, 8, uT, "uT", TB, hk)
        for q4 in range(4):
            def hv(tt, pb):
                k2 = tt % 2
                T.op("act", lambda: nc.scalar.copy(out=vst[:, k2, :], in_=ps[pb][:, 0:256]),
                     rd=[("ps", pb)], wr=[("sb_vst", k2)])
                for pp in range(2):
                    cpair = 2 * q4 + pp
                    t0 = b * TB + tt * 128
                    T.dma("sp", c_kv, v_d[cpair, t0:t0 + 128, :], vst[:, k2, pp * 128:(pp + 1) * 128],
                          rd=[("sb_vst", k2)], wr=[("v_d", cpair, b)])
            proj_tm("sb_w_in", 0, 2048 + q4 * 256, 256, uT, "uT", 4, hv)
        proj_fm("sb_w_in", 0, 3072, 4, uT, "uT", TB, qmem_handler)

    def sb_load_pair(c, b):
        sl = c % 2
        n = (b + 1) * TB
        T.dma("sp", c_kv, sbst["kc"][:, sl, 0:n], kT_d[c, :, 0:n], rd=[("kT_d", c, bb) for bb in range(b + 1)],
              wr=[("sb_kc", sl)])
        T.dma("sp", c_kv, sbst["vc"][:, sl, 0:4 * (b + 1), :],
              v_d[c, 0:n, :].rearrange("(k p) n -> p k n", p=128),
              rd=[("v_d", c, bb) for bb in range(b + 1)], wr=[("sb_vc", sl)])

    def sb_attn(b):
        qT = sbst["qT"]
        e, Lp, attB, Pm, att = sbst["e"], sbst["Lp"], sbst["attB"], sbst["Pm"], sbst["att"]
        sb_load_pair(0, b)
        nk = 4 * b + 4
        rot = {"i": 0}

        def sbbank():
            rot["i"] = (rot["i"] + 1) % 6
            return (0, 1, 2, 3, 4, 7)[rot["i"]]
        for c in range(8):
            if c + 1 < 8:
                sb_load_pair(c + 1, b)
            sl = c % 2
            kc, vc = sbst["kc"], sbst["vc"]
            pO = 5
            for hh in range(2):
                po = hh * 64
                qview = qT[po:po + 64, c, :]
                order = list(range(nk - 1, -1, -1))
                for g0 in range(0, nk, 3):
                    grp = [(g0 + u, order[g0 + u]) for u in range(3) if g0 + u < nk]
                    banks_a, banks_b = {}, {}
                    for u, (idx, kb) in enumerate(grp):
                        kview = kc[po:po + 64, sl, kb * 128:(kb + 1) * 128]
                        pa = sbbank()
                        banks_a[u] = pa
                        T.op("pe", lambda: nc.tensor.matmul(ps[pa][:], lhsT=kview, rhs=qview, start=True, stop=True),
                             rd=[("sb_kc", sl), ("sb_qT", c)], wr=[("ps", pa)])
                    for u, (idx, kb) in enumerate(grp):
                        pa = banks_a[u]
                        T.op("act", lambda: nc.scalar.activation(out=e[:, u, :], in_=ps[pa][:], func=AF.Exp),
                             rd=[("ps", pa)], wr=[("sb_e", u)])
                    for u, (idx, kb) in enumerate(grp):
                        T.op("act", lambda: nc.scalar.activation(out=Lp[:, u, :], in_=e[:, u, :], func=AF.Ln,
                                                                 bias=oneb[:]),
                             rd=[("sb_e", u), "oneb"], wr=[("sb_Lp", u)])
                        i = kb - 4 * b
                        if i >= 0:
                            m01 = cb[:, CB_MASK01 + i * TB:CB_MASK01 + (i + 1) * TB]
                            T.op("pool", lambda: nc.gpsimd.tensor_tensor(out=Lp[:, u, :], in0=Lp[:, u, :], in1=m01,
                                                                         op=ALU.mult),
                                 rd=[("sb_Lp", u), "cb"], wr=[("sb_Lp", u)])
                    for u, (idx, kb) in enumerate(grp):
                        kview = kc[po:po + 64, sl, kb * 128:(kb + 1) * 128]
                        pbk = sbbank()
                        banks_b[u] = pbk
                        T.op("pe", lambda: nc.tensor.matmul(ps[pbk][:], lhsT=kview, rhs=qview, start=True, stop=False),
                             rd=[("sb_kc", sl), ("sb_qT", c)], wr=[("ps", pbk)], inc=False)
                        T.op("pe", lambda: nc.tensor.matmul(ps[pbk][:], lhsT=negMincl, rhs=Lp[:, u, :],
                                                            start=False, stop=True),
                             rd=["cb", ("sb_Lp", u)], wr=[("ps", pbk)])
                    for u, (idx, kb) in enumerate(grp):
                        pbk = banks_b[u]
                        T.op("act", lambda: nc.scalar.activation(out=attB[:, u, :], in_=ps[pbk][:], func=AF.Exp),
                             rd=[("ps", pbk)], wr=[("sb_attB", u)])
                        i = kb - 4 * b
                        if i >= 0:
                            m01 = cb[:, CB_MASK01 + i * TB:CB_MASK01 + (i + 1) * TB]
                            T.op("pool", lambda: nc.gpsimd.tensor_tensor(out=attB[:, u, :], in0=attB[:, u, :], in1=m01,
                                                                         op=ALU.mult),
                                 rd=[("sb_attB", u), "cb"], wr=[("sb_attB", u)])
                    for u, (idx, kb) in enumerate(grp):
                        if idx > 0:
                            T.op("act", lambda: nc.scalar.activation(out=Pm[:, u, :], in_=ps[6][:], func=AF.Exp),
                                 rd=[("ps", 6)], wr=[("sb_Pm", u)])
                            T.op("dve", lambda: nc.vector.tensor_tensor(out=att[:, u, :], in0=attB[:, u, :],
                                                                        in1=Pm[:, u, :], op=ALU.mult),
                                 rd=[("sb_attB", u), ("sb_Pm", u)], wr=[("sb_att", u)])
                            a_ap, a_key = att[:, u, :], ("sb_att", u)
                        else:
                            a_ap, a_key = attB[:, u, :], ("sb_attB", u)
                        if idx < nk - 1:
                            T.op("pe", lambda: nc.tensor.matmul(ps[6][:], lhsT=negones, rhs=Lp[:, u, :],
                                                                start=(idx == 0), stop=True),
                                 rd=["cb", ("sb_Lp", u)], wr=[("ps", 6)])
                        T.op("pe", lambda: nc.tensor.matmul(ps[pO][po:po + 64, :], lhsT=vc[:, sl, kb, po:po + 64],
                                                            rhs=a_ap, start=(idx == 0), stop=(idx == nk - 1),
                                                            tile_position=(0, po)),
                             rd=[("sb_vc", sl), a_key], wr=[("ps", pO, hh)])
            T.op("dve", lambda: nc.vector.tensor_copy(out=catT[:, c, :], in_=ps[pO][:]),
                 rd=[("ps", pO, 0), ("ps", pO, 1)], wr=[("catT", c)])

    mlst = {}
    sel_d = P.dram("selc", [16, 8 * 128], F32, "ExternalInput")
    mlb_d = P.dram("mlbias", [16, 1], F32, "ExternalInput")
    mlg_d = P.dram("mlgain", [128, 1], F32, "ExternalInput")
    m01i_d = P.dram("mask01incl", [128, 4 * TB], BF16, "ExternalInput")

    def ml_alloc():
        R2, R2f = LS["R2"], LS["R2f"]
        mlst["qT"] = R2[:, 0:2048].rearrange("p (a n) -> p a n", a=4)
        mlst["sigo"] = R2[:, 2048:2560]
        mlst["Dm"] = R2[:, 2560:3584].rearrange("p (a n) -> p a n", a=2)
        mlst["Wt"] = R2[:, 3584:4608].rearrange("p (a n) -> p a n", a=2)
        mlst["hsb"] = R2f[:, 2304:2816]
        mlst["gsb"] = R2f[0:16, 2816:3328]
        mlst["lp"] = R2f[0:16, 3328:3840]
        mlst["cum"] = R2f[0:16, 3840:4352]
        mlst["NF"] = R2f[0:16, 4352:4864]
        mlst["sel"] = R2f[0:16, 4864:5888]
        mlst["Acol"] = R2f[:, 5888:6016].rearrange("p (a n) -> p a n", a=16)
        mlst["tT"] = R2f[:, 6016:6048].rearrange("p (a n) -> p a n", a=2)
        mlst["carry"] = R2f[0:16, 6048:6049]
        mlst["bias"] = R2f[0:16, 6049:6050]
        mlst["gain"] = R2f[:, 6050:6051]
        mlst["kc"] = R1[:, 0:4096].rearrange("p (a n) -> p a n", a=2)
        mlst["vc"] = R1[:, 4096:8192].rearrange("p (a k n) -> p a k n", a=2, k=16)
        mlst["kst"] = R1[:, 8192:9216].rearrange("p (a n) -> p a n", a=2)
        mlst["vst"] = R1[:, 9216:9728].rearrange("p (a n) -> p a n", a=2)

    def ml_setup():
        T.dma("sp", c_io, mlst["sel"][:], sel_d, wr=["ml_sel"])
        T.dma("sp", c_io, mlst["bias"][:], mlb_d, wr=["ml_bias"])
        T.dma("sp", c_io, mlst["gain"][:], mlg_d, wr=["ml_gain"])
        T.dma("sp", c_io, cb[:, CB_MASK01:CB_MASK01 + 4 * TB], m01i_d, wr=["cb"])

    def ml_inproj(l, b):
        qT, kst, vst = mlst["qT"], mlst["kst"], mlst["vst"]
        gsb, lp, cum, carry, NF, Acol, tT = (mlst[k] for k in ("gsb", "lp", "cum", "carry", "NF", "Acol", "tT"))
        mixer_keys[:] = [("sb_kc", 0), ("sb_kc", 1), ("sb_vc", 0), ("sb_vc", 1), ("sb_kst", 0), ("sb_kst", 1),
                         ("sb_vst", 0), ("sb_vst", 1)]
        T.alias(mixer_keys, [("aT", j) for j in range(22)])

        def hq(j, pb):
            T.op("act", lambda: nc.scalar.copy(out=qT[:, j, :], in_=ps[pb][:]), rd=[("ps", pb)], wr=[("ml_qT", j)])

        def hk(j, pb):
            k2 = j % 2
            T.op("dve", lambda: nc.vector.tensor_scalar(out=kst[:, k2, :], in0=ps[pb][:], scalar1=0.125, scalar2=None,
                                                        op0=ALU.mult),
                 rd=[("ps", pb)], wr=[("sb_kst", k2)])
            T.dma("sp", c_kv, kT_d[j, :, b * TB:(b + 1) * TB], kst[:, k2, :], rd=[("sb_kst", k2)],
                  wr=[("kT_d", j, b)])
        proj_fm("ml_w_in", 0, 0, 4, uT, "uT", TB, hq)
        proj_fm("ml_w_in", 0, 512, 4, uT, "uT", TB, hk)
        for q4 in range(4):
            def hv(tt, pb):
                k2 = tt % 2
                T.op("act", lambda: nc.scalar.copy(out=vst[:, k2, :], in_=ps[pb][:, 0:256]),
                     rd=[("ps", pb)], wr=[("sb_vst", k2)])
                for pp in range(2):
                    hd = 2 * q4 + pp
                    t0 = b * TB + tt * 128
                    T.dma("sp", c_kv, v_d[hd, t0:t0 + 128, :], vst[:, k2, pp * 128:(pp + 1) * 128],
                          rd=[("sb_vst", k2)], wr=[("v_d", hd, b)])
            proj_tm("ml_w_in", 0, 1024 + q4 * 256, 256, uT, "uT", 4, hv)
        slot = w_get([("ml_w_in", 0, 0, 8, 3072, 16)])
        wv = wview(slot, 0, 8, 16)
        pg = nextbank()
        for kc in range(8):
            T.op("pe", lambda: nc.tensor.matmul(ps[pg][0:16, :], lhsT=wv[:, kc, :], rhs=uT[:, kc, :],
                                                start=(kc == 0), stop=(kc == 7)),
                 rd=[WK(slot), ("uT", kc)], wr=[("ps", pg)], inc=(kc == 7))
        T.op("act", lambda: nc.scalar.activation(out=gsb[:], in_=ps[pg][0:16, :], func=AF.Identity,
                                                 bias=mlst["bias"][:]),
             rd=[("ps", pg), "ml_bias"], wr=["ml_gsb"])
        T.op("act", lambda: nc.scalar.activation(out=lp[:], in_=gsb[:], func=AF.Exp, scale=-1.0),
             rd=["ml_gsb"], wr=["ml_lp"])
        T.op("act", lambda: nc.scalar.activation(out=lp[:], in_=lp[:], func=AF.Ln, bias=oneb[0:16, :]),
             rd=["ml_lp", "oneb"], wr=["ml_lp"])
        if b == 0:
            T.op("dve", lambda: nc.vector.memset(carry[:], 0.0), wr=["ml_carry"])
        T.op("dve", lambda: nc.vector.tensor_tensor_scan(out=cum[:], data0=lp[:], data1=lp[:], initial=carry[:, 0:1],
                                                         op0=ALU.add, op1=ALU.max),
             rd=["ml_lp", "ml_carry"], wr=["ml_cum"])
        T.op("dve", lambda: nc.vector.tensor_copy(out=carry[:], in_=cum[:, TB - 1:TB]), rd=["ml_cum"], wr=["ml_carry"])
        T.op("dve", lambda: nc.vector.tensor_scalar(out=NF[:], in0=cum[:], scalar1=-1.0, scalar2=None, op0=ALU.mult),
             rd=["ml_cum"], wr=["ml_NF"])
        for tt in range(4):
            pt = nextbank()
            T.op("pe", lambda: nc.tensor.transpose(out=ps[pt][:, 0:16], in_=gsb[:, tt * 128:(tt + 1) * 128],
                                                   identity=identf[0:16, 0:16]),
                 rd=["ml_gsb", "identf"], wr=[("ps", pt)], inc=False)
            T.op("pe", lambda: nc.tensor.transpose(out=ps[pt][:, 16:32], in_=cum[:, tt * 128:(tt + 1) * 128],
                                                   identity=identf[0:16, 0:16]),
                 rd=["ml_cum", "identf"], wr=[("ps", pt)])
            T.op("act", lambda: nc.scalar.copy(out=tT[:, tt % 2, :], in_=ps[pt][:, 16:32]),
                 rd=[("ps", pt)], wr=[("ml_tT", tt % 2)])
            T.op("dve", lambda: nc.vector.tensor_tensor(out=Acol[:, b * 4 + tt, :], in0=ps[pt][:, 0:8],
                                                        in1=tT[:, tt % 2, 8:16], op=ALU.add),
                 rd=[("ps", pt), ("ml_tT", tt % 2)], wr=[("ml_Acol", b * 4 + tt)])
        proj_fm("ml_w_in", 0, 3088, 4, uT, "uT", TB, qmem_handler)

    def ml_attn(l, b):
        qT, Dm, Wt, hsb, Acol, NF, sel, sigo = (mlst[k] for k in ("qT", "Dm", "Wt", "hsb", "Acol", "NF", "sel", "sigo"))
        kc, vc = mlst["kc"], mlst["vc"]
        it = 0
        nk = 4 * b + 4

        def load(h):
            sl = h % 2
            n = (b + 1) * TB
            if h % 2 == 0:
                T.dma("sp", c_kv, kc[:, (h // 2) % 2, 0:n], kT_d[h // 2, :, 0:n],
                      rd=[("kT_d", h // 2, bb) for bb in range(b + 1)], wr=[("sb_kc", (h // 2) % 2)])
            T.dma("sp", c_kv, vc[:, sl, 0:4 * (b + 1), :], v_d[h, 0:n, :].rearrange("(k p) n -> p k n", p=128),
                  rd=[("v_d", h, bb) for bb in range(b + 1)], wr=[("sb_vc", sl)])
        load(0)
        for h in range(8):
            if h + 1 < 8:
                load(h + 1)
            c, po, sl, ksl = h // 2, (h % 2) * 64, h % 2, (h // 2) % 2
            T.op("pe", lambda: nc.tensor.matmul(ps[6][:], lhsT=sel[:, h * 128:(h + 1) * 128], rhs=NF[:],
                                                start=True, stop=True),
                 rd=["ml_sel", "ml_NF"], wr=[("ps", 6)])
            pN, pD = 5, 7
            order = list(range(nk - 1, -1, -1))
            for g0 in range(0, nk, 2):
                grp = [(g0 + u, order[g0 + u]) for u in range(2) if g0 + u < nk]
                pas = {}
                for u, (idx, kb) in enumerate(grp):
                    pa = nextbank()
                    pas[u] = pa
                    T.op("pe", lambda: nc.tensor.matmul(ps[pa][:], lhsT=kc[po:po + 64, ksl, kb * 128:(kb + 1) * 128],
                                                        rhs=qT[po:po + 64, c, :], start=True, stop=True),
                         rd=[("sb_kc", ksl), ("ml_qT", c)], wr=[("ps", pa)])
                for u, (idx, kb) in enumerate(grp):
                    T.op("act", lambda: nc.scalar.activation(out=Dm[:, u, :], in_=ps[6][:], func=AF.Exp,
                                                             bias=Acol[:, kb, h:h + 1]),
                         rd=[("ps", 6), ("ml_Acol", kb)], wr=[("ml_Dm", u)])
                for u, (idx, kb) in enumerate(grp):
                    pa = pas[u]
                    i = kb - 4 * b
                    T.op("dve", lambda: nc.vector.tensor_tensor(out=Wt[:, u, :], in0=ps[pa][:], in1=Dm[:, u, :],
                                                                op=ALU.mult),
                         rd=[("ps", pa), ("ml_Dm", u)], wr=[("ml_Wt", u)])
                    if i >= 0:
                        m01 = cb[:, CB_MASK01 + i * TB:CB_MASK01 + (i + 1) * TB]
                        T.op("pool", lambda: nc.gpsimd.tensor_tensor(out=Wt[:, u, :], in0=Wt[:, u, :], in1=m01,
                                                                     op=ALU.mult),
                             rd=[("ml_Wt", u), "cb"], wr=[("ml_Wt", u)])
                for u, (idx, kb) in enumerate(grp):
                    T.op("pe", lambda: nc.tensor.matmul(ps[pN][:], lhsT=vc[:, sl, kb, :], rhs=Wt[:, u, :],
                                                        start=(idx == 0), stop=(idx == nk - 1)),
                         rd=[("sb_vc", sl), ("ml_Wt", u)], wr=[("ps", pN)], inc=False)
                    T.op("pe", lambda: nc.tensor.matmul(ps[pD][:], lhsT=onesb, rhs=Wt[:, u, :],
                                                        start=(idx == 0), stop=(idx == nk - 1)),
                         rd=["cb", ("ml_Wt", u)], wr=[("ps", pD)])
            T.op("act", lambda: nc.scalar.activation(out=rden[:], in_=ps[pD][:], func=AF.Abs),
                 rd=[("ps", pD)], wr=["rden"])
            T.op("dve", lambda: nc.vector.tensor_scalar(out=rden[:], in0=rden[:], scalar1=1.0, scalar2=None,
                                                        op0=ALU.max),
                 rd=["rden"], wr=["rden"])
            T.op("dve", lambda: nc.vector.reciprocal(out=rden[:], in_=rden[:]), rd=["rden"], wr=["rden"])
            T.op("dve", lambda: nc.vector.tensor_tensor(out=hsb[:], in0=ps[pN][:], in1=rden[:], op=ALU.mult),
                 rd=[("ps", pN), "rden"], wr=["ml_hsb"])
            T.op("act", lambda: nc.scalar.activation(out=sq[:, 0, :], in_=hsb[:], func=AF.Square),
                 rd=["ml_hsb"], wr=[("sq", 0)])
            T.op("pe", lambda: nc.tensor.matmul(ps[pD][:], lhsT=onesb, rhs=sq[:, 0, :], start=True, stop=True),
                 rd=["cb", ("sq", 0)], wr=[("ps", pD)])
            T.op("act", lambda: nc.scalar.activation(out=rden[:], in_=ps[pD][:], func=AF.Sqrt, scale=1.0 / 128,
                                                     bias=epsb[:]),
                 rd=[("ps", pD), "epsb"], wr=["rden"])
            T.op("dve", lambda: nc.vector.reciprocal(out=rden[:], in_=rden[:]), rd=["rden"], wr=["rden"])
            T.op("dve", lambda: nc.vector.scalar_tensor_tensor(out=hsb[:], in0=hsb[:], scalar=mlst["gain"][:, 0:1],
                                                               in1=rden[:], op0=ALU.mult, op1=ALU.mult),
                 rd=["ml_hsb", "ml_gain", "rden"], wr=["ml_hsb"])
            slot = w_get([("ml_w_in", 0, 0, 8, 2048 + h * 128, 128)])
            wv = wview(slot, 0, 8, 128)
            pg = nextbank()
            for kcc in range(8):
                T.op("pe", lambda: nc.tensor.matmul(ps[pg][:], lhsT=wv[:, kcc, :], rhs=uT[:, kcc, :],
                                                    start=(kcc == 0), stop=(kcc == 7)),
                     rd=[WK(slot), ("uT", kcc)], wr=[("ps", pg)], inc=(kcc == 7))
            T.op("act", lambda: nc.scalar.activation(out=sigo[:], in_=ps[pg][:], func=AF.Sigmoid),
                 rd=[("ps", pg)], wr=["ml_sigo"])
            T.op("dve", lambda: nc.vector.tensor_tensor(out=catT[:, h, :], in0=hsb[:], in1=sigo[:], op=ALU.mult),
                 rd=["ml_hsb", "ml_sigo"], wr=[("catT", h)])

    gd = {}
    gcw_d = P.dram("gdn_cw", [2, 128, 96], F32, "ExternalInput")
    gdtb_d = P.dram("gdn_dtb", [2, 8, 1], F32, "ExternalInput")
    galog_d = P.dram("gdn_alog", [2, 8, 1], F32, "ExternalInput")
    ggain_d = P.dram("gdn_gain", [2, 128, 1], F32, "ExternalInput")
    sel8_d = P.dram("sel8", [8, 8 * 128], F32, "ExternalInput")
    gmask_d = P.dram("gdn_masks", [128, 3 * TB], BF16, "ExternalInput")
    R3 = ysb_raw.bitcast(BF16)
    R1f = R1.bitcast(F32)

    def gdn_alloc():
        gF, gR2 = LS["gF"], LS["gR2"]
        gFr = gF.bitcast(F32R)
        gd["A"] = gF[:, 0:1024].rearrange("p (s n) -> p s n", s=2)
        gd["AT"] = gF[:, 1024:2048].rearrange("p (s n) -> p s n", s=2)
        gd["X"] = gF[:, 2048:2560]
        gd["Ru"] = gF[:, 2560:3072]
        gd["Rw"] = gF[:, 3072:3584]
        gd["Ar"] = gFr[:, 0:1024].rearrange("p (s n) -> p s n", s=2)
        gd["ATr"] = gFr[:, 1024:2048].rearrange("p (s n) -> p s n", s=2)
        gd["Xr"] = gFr[:, 2048:2560]
        gd["Rur"] = gFr[:, 2560:3072]
        gd["Rwr"] = gFr[:, 3072:3584]
        o1 = 0

        def f1(n, parts=128):
            nonlocal o1
            v = R1f[0:parts, o1:o1 + n]
            o1 += n
            return v
        gd["u"] = f1(512)
        gd["knf"] = f1(512)
        gd["vsf"] = f1(512)
        gd["ra"] = f1(512, 8)
        gd["rb"] = f1(512, 8)
        gd["G"] = f1(512, 8)
        gd["NG"] = f1(512, 8)
        gd["GB"] = f1(512)
        gd["xc"] = f1(516)
        gd["xc2"] = f1(516)
        assert o1 <= 5632
        o = 0

        def f2(n, parts=128):
            nonlocal o
            v = gR2[0:parts, o:o + n]
            o += n
            return v
        gd["sel8"] = f2(1024, 8)
        gd["acc"] = f2(512)
        gd["S"] = f2(1024).rearrange("p (h n) -> p h n", h=8)
        gd["gcol"] = f2(64).rearrange("p (t n) -> p t n", t=4)
        gd["ngcol"] = f2(64).rearrange("p (t n) -> p t n", t=4)
        gd["bgc"] = f2(32).rearrange("p (t n) -> p t n", t=4)
        gd["egc"] = f2(32).rearrange("p (t n) -> p t n", t=4)
        gd["kgc"] = f2(4)
        gd["glc"] = f2(4)
        gd["cw"] = f2(96).rearrange("p (c k) -> p c k", k=4)
        gd["halo"] = f2(72).rearrange("p (c k) -> p c k", k=3)
        gd["gain"] = f2(1)
        gd["dtb"] = f2(1, 8)
        gd["negA"] = f2(1, 8)
        gd["rr"] = rden[:, :]
        assert o <= 2944, o
        o3 = 0

        def f3(n):
            nonlocal o3
            v = R3[:, o3:o3 + n]
            o3 += n
            return v
        for nm in ("qnT", "qgT", "knT", "kbT", "zs", "EG", "Bb", "E1", "E2", "E3", "kg", "wT", "pTg"):
            gd[nm] = f3(512)
        gd["vnb"] = f3(256).rearrange("p (s n) -> p s n", s=2)
        gd["Sb"] = f3(128)
        assert o3 <= 8192

    G_R1KEYS = ["g_u", "g_knf", "g_vsf", "g_ra", "g_rb", "g_G", "g_NG", "g_GB", "g_xc", "g_xc2"]

    def gdn_setup(l):
        jg = l // 3
        T.dma("sp", c_io, gd["cw"].rearrange("p c k -> p (c k)"), gcw_d[jg], wr=["g_cw"])
        T.dma("sp", c_io, gd["dtb"], gdtb_d[jg], wr=["g_dtb"])
        T.dma("sp", c_io, gd["negA"], galog_d[jg], wr=["g_negA"])
        T.dma("sp", c_io, gd["gain"], ggain_d[jg], wr=["g_gain"])
        T.dma("sp", c_io, gd["sel8"], sel8_d, wr=["g_sel8"])
        T.dma("sp", c_io, cb[:, CB_MASK01:CB_MASK01 + 3 * TB], gmask_d, wr=["cb"])
        T.op("act", lambda: nc.scalar.activation(out=gd["negA"], in_=gd["negA"], func=AF.Exp), rd=["g_negA"], wr=["g_negA"])
        T.op("dve", lambda: nc.vector.tensor_scalar(out=gd["negA"], in0=gd["negA"], scalar1=-1.0, scalar2=None,
                                                    op0=ALU.mult), rd=["g_negA"], wr=["g_negA"])
        T.op("pool", lambda: nc.gpsimd.memset(gd["S"].rearrange("p h n -> p (h n)"), 0.0), wr=[("g_S", h) for h in range(8)])

    def gdn_block(l, b):
        jg = l // 3
        g = gd
        M1 = cb[:, CB_MASK01:CB_MASK01 + TB]
        M2 = cb[:, CB_MASK01 + TB:CB_MASK01 + 2 * TB]
        M3 = cb[:, CB_MASK01 + 2 * TB:CB_MASK01 + 3 * TB]
        mixer_keys[:] = G_R1KEYS
        T.alias(mixer_keys, [("aT", j) for j in range(22)])
        ysb_overlay[:] = ["g_qnT", "g_qgT", "g_knT", "g_kbT", "g_zs", "g_EG", "g_Bb", "g_E1", "g_E2", "g_E3", "g_kg", "g_wT", "g_pTg", ("g_vnb", 0), ("g_vnb", 1), "g_Sb"]
        T.alias(ysb_overlay, [("ysb", c) for c in range(8)])
        slot = w_get([("gdn_w_in", jg, 0, 8, 4096, 16)])
        wv = wview(slot, 0, 8, 16)
        pa, pb_ = nextbank(), nextbank()
        for (pbank, c0) in ((pa, 0), (pb_, 8)):
            for kc in range(8):
                T.op("pe", lambda: nc.tensor.matmul(ps[pbank][0:8, :], lhsT=wv[:, kc, c0:c0 + 8], rhs=uT[:, kc, :],
                                                    start=(kc == 0), stop=(kc == 7)),
                     rd=[WK(slot), ("uT", kc)], wr=[("ps", pbank)], inc=(kc == 7))
        T.op("act", lambda: nc.scalar.activation(out=g["ra"], in_=ps[pa][0:8, :], func=AF.Exp, bias=g["dtb"]),
             rd=[("ps", pa), "g_dtb"], wr=["g_ra"])
        T.op("act", lambda: nc.scalar.activation(out=g["ra"], in_=g["ra"], func=AF.Ln, bias=oneb[0:8, :]),
             rd=["g_ra", "oneb"], wr=["g_ra"])
        T.op("dve", lambda: nc.vector.tensor_scalar(out=g["ra"], in0=g["ra"], scalar1=g["negA"], scalar2=None,
                                                    op0=ALU.mult), rd=["g_ra", "g_negA"], wr=["g_ra"])
        T.op("act", lambda: nc.scalar.activation(out=g["rb"], in_=ps[pb_][0:8, :], func=AF.Sigmoid),
             rd=[("ps", pb_)], wr=["g_rb"])
        for n in range(4):
            cs = slice(n * 128, (n + 1) * 128)
            T.op("dve", lambda: nc.vector.tensor_tensor_scan(out=g["G"][:, cs], data0=g["ra"][:, cs], data1=g["ra"][:, cs],
                                                             initial=0.0, op0=ALU.add, op1=ALU.min),
                 rd=["g_ra"], wr=["g_G"])
        T.op("dve", lambda: nc.vector.tensor_scalar(out=g["NG"], in0=g["G"], scalar1=-1.0, scalar2=None, op0=ALU.mult),
             rd=["g_G"], wr=["g_NG"])
        for tt in range(4):
            cs = slice(tt * 128, (tt + 1) * 128)
            pt = nextbank()
            T.op("pe", lambda: nc.tensor.transpose(out=ps[pt][:, 0:8], in_=g["G"][:, cs], identity=identf[0:8, 0:8]),
                 rd=["g_G", "identf"], wr=[("ps", pt)], inc=False)
            T.op("pe", lambda: nc.tensor.transpose(out=ps[pt][:, 8:16], in_=g["rb"][:, cs], identity=identf[0:8, 0:8]),
                 rd=["g_rb", "identf"], wr=[("ps", pt)])
            T.op("act", lambda: nc.scalar.copy(out=g["gcol"][:, tt, :], in_=ps[pt][:, 0:16]),
                 rd=[("ps", pt)], wr=["g_gcol"])
        T.op("dve", lambda: nc.vector.tensor_scalar(out=g["ngcol"], in0=g["gcol"], scalar1=-1.0, scalar2=None,
                                                    op0=ALU.mult), rd=["g_gcol"], wr=["g_ngcol"])
        T.op("act", lambda: nc.scalar.activation(out=g["egc"], in_=g["gcol"][:, :, 0:8], func=AF.Exp),
             rd=["g_gcol"], wr=["g_egc"])
        T.op("dve", lambda: nc.vector.tensor_tensor(out=g["bgc"], in0=g["egc"], in1=g["gcol"][:, :, 8:16], op=ALU.mult),
             rd=["g_egc", "g_gcol"], wr=["g_bgc"])

        def conv_silu(cj, pbank, dst, dkey):
            xc, acc, cw, halo = g["xc"], g["acc"], g["cw"], g["halo"]
            T.op("act", lambda: nc.scalar.copy(out=xc[:, 3:515], in_=ps[pbank][:]), rd=[("ps", pbank)], wr=["g_xc"])
            if b == 0:
                T.op("dve", lambda: nc.vector.memset(xc[:, 0:3], 0.0), wr=["g_xc"])
            else:
                T.op("dve", lambda: nc.vector.tensor_copy(out=xc[:, 0:3], in_=halo[:, cj, :]), rd=[("g_halo", cj)],
                     wr=["g_xc"])
            T.op("dve", lambda: nc.vector.tensor_copy(out=halo[:, cj, :], in_=xc[:, 512:515]), rd=["g_xc"],
                 wr=[("g_halo", cj)])
            T.op("act", lambda: nc.scalar.activation(out=acc, in_=xc[:, 0:512], func=AF.Copy, scale=cw[:, cj, 0:1]),
                 rd=["g_xc", "g_cw"], wr=["g_acc"])
            for k in range(1, 4):
                T.op("dve", lambda: nc.vector.scalar_tensor_tensor(out=acc, in0=xc[:, k:k + 512], scalar=cw[:, cj, k:k + 1],
                                                                   in1=acc, op0=ALU.mult, op1=ALU.add),
                     rd=["g_xc", "g_cw", "g_acc"], wr=["g_acc"])
            T.op("act", lambda: nc.scalar.activation(out=dst, in_=acc, func=AF.Silu), rd=["g_acc"], wr=[dkey])

        def l2_rstd(src, skey):
            T.op("act", lambda: nc.scalar.activation(out=sq[:, 0, :], in_=src, func=AF.Square), rd=[skey], wr=[("sq", 0)])
            T.op("pe", lambda: nc.tensor.matmul(ps[7][:], lhsT=onesb, rhs=sq[:, 0, :], start=True, stop=True),
                 rd=["cb", ("sq", 0)], wr=[("ps", 7)])
            T.op("act", lambda: nc.scalar.activation(out=g["rr"], in_=ps[7][:], func=AF.Sqrt, bias=epsb[:]),
                 rd=[("ps", 7), "epsb"], wr=["rden"])
            T.op("dve", lambda: nc.vector.reciprocal(out=g["rr"], in_=g["rr"]), rd=["rden"], wr=["rden"])

        for h in range(8):
            s8 = g["sel8"][:, h * 128:(h + 1) * 128]
            pE2 = nextbank()
            T.op("pe", lambda: nc.tensor.matmul(ps[pE2][:], lhsT=s8, rhs=g["G"], start=True, stop=True),
                 rd=["g_sel8", "g_G"], wr=[("ps", pE2)])
            T.op("act", lambda: nc.scalar.copy(out=g["GB"], in_=ps[pE2][:]), rd=[("ps", pE2)], wr=["g_GB"])
            T.op("act", lambda: nc.scalar.activation(out=g["EG"], in_=ps[pE2][:], func=AF.Exp), rd=[("ps", pE2)], wr=["g_EG"])
            pB = nextbank()
            T.op("pe", lambda: nc.tensor.matmul(ps[pB][:], lhsT=s8, rhs=g["rb"], start=True, stop=True),
                 rd=["g_sel8", "g_rb"], wr=[("ps", pB)])
            T.op("act", lambda: nc.scalar.copy(out=g["Bb"], in_=ps[pB][:]), rd=[("ps", pB)], wr=["g_Bb"])
            T.op("pe", lambda: nc.tensor.matmul(ps[pE2][:], lhsT=identb, rhs=M2, start=False, stop=True),
                 rd=["cb"], wr=[("ps", pE2)])
            for n in range(4):
                cs = slice(n * 128, (n + 1) * 128)
                T.op("act", lambda: nc.scalar.activation(out=g["E2"][:, cs], in_=ps[pE2][:, cs], func=AF.Exp,
                                                         bias=g["ngcol"][:, n, h:h + 1]),
                     rd=[("ps", pE2), "g_ngcol"], wr=["g_E2"])
            for n in range(4):
                cs = slice(n * 128, (n + 1) * 128)
                T.op("pool", lambda: nc.gpsimd.tensor_tensor(out=g["E3"][:, cs], in0=g["E2"][:, cs], in1=identb, op=ALU.add),
                     rd=["g_E2", "cb"], wr=["g_E3"])
            pe_ = nextbank()
            T.op("pe", lambda: nc.tensor.matmul(ps[pe_][:], lhsT=s8, rhs=g["NG"], start=True, stop=False),
                 rd=["g_sel8", "g_NG"], wr=[("ps", pe_)], inc=False)
            T.op("pe", lambda: nc.tensor.matmul(ps[pe_][:], lhsT=identb, rhs=M1, start=False, stop=True),
                 rd=["cb"], wr=[("ps", pe_)])
            for n in range(4):
                cs = slice(n * 128, (n + 1) * 128)
                T.op("act", lambda: nc.scalar.activation(out=g["E1"][:, cs], in_=ps[pe_][:, cs], func=AF.Exp,
                                                         bias=g["gcol"][:, n, h:h + 1]),
                     rd=[("ps", pe_), "g_gcol"], wr=["g_E1"])
            for n in range(4):
                T.op("act", lambda: nc.scalar.activation(out=g["kgc"][:, n:n + 1], in_=g["gcol"][:, n, h:h + 1], func=AF.Exp,
                                                         scale=-1.0, bias=g["GB"][:, n * 128 + 127:n * 128 + 128]),
                     rd=["g_gcol", "g_GB"], wr=["g_kgc"])
                T.op("act", lambda: nc.scalar.activation(out=g["glc"][:, n:n + 1],
                                                         in_=g["GB"][:, n * 128 + 127:n * 128 + 128], func=AF.Exp),
                     rd=["g_GB"], wr=["g_glc"])
            slot = w_get([("gdn_w_in", jg, 0, 8, h * 128, 128), ("gdn_w_in", jg, 0, 8, 1024 + h * 128, 128)])
            slot2 = w_get([("gdn_w_in", jg, 0, 8, 2048 + h * 128, 128), ("gdn_w_in", jg, 0, 8, 3072 + h * 128, 128)])
            banks = []
            for (sl_, piece) in ((slot, 0), (slot, 1), (slot2, 0), (slot2, 1)):
                wv4 = WT(sl_).rearrange("p (g k n) -> p g k n", g=2, k=8)
                pbank = nextbank()
                for kc in range(8):
                    T.op("pe", lambda: nc.tensor.matmul(ps[pbank][:], lhsT=wv4[:, piece, kc, :], rhs=uT[:, kc, :],
                                                        start=(kc == 0), stop=(kc == 7)),
                         rd=[WK(sl_), ("uT", kc)], wr=[("ps", pbank)], inc=(kc == 7))
                banks.append(pbank)
            SA = dict(xc=g["xc"], xk="g_xc", acc=g["acc"], ak="g_acc", rr=g["rr"], rk="rden", sqs=0, pb=7, cj=h, bank=banks[0])
            SB_ = dict(xc=g["xc2"], xk="g_xc2", acc=tmpn[:, 0, :], ak=("tmpn", 0), rr=rstd[:, :], rk="rstd", sqs=1, pb=6,
                       cj=8 + h, bank=banks[1])
            cw, halo = g["cw"], g["halo"]

            def st_load(S_):
                T.op("act", lambda: nc.scalar.copy(out=S_["xc"][:, 3:515], in_=ps[S_["bank"]][:]), rd=[("ps", S_["bank"])],
                     wr=[S_["xk"]])
                if b == 0:
                    T.op("dve", lambda: nc.vector.memset(S_["xc"][:, 0:3], 0.0), wr=[S_["xk"]])
                else:
                    T.op("dve", lambda: nc.vector.tensor_copy(out=S_["xc"][:, 0:3], in_=halo[:, S_["cj"], :]),
                         rd=[("g_halo", S_["cj"])], wr=[S_["xk"]])
                T.op("dve", lambda: nc.vector.tensor_copy(out=halo[:, S_["cj"], :], in_=S_["xc"][:, 512:515]), rd=[S_["xk"]],
                     wr=[("g_halo", S_["cj"])])

            def st_tap0(S_):
                T.op("act", lambda: nc.scalar.activation(out=S_["acc"], in_=S_["xc"][:, 0:512], func=AF.Copy,
                                                         scale=cw[:, S_["cj"], 0:1]),
                     rd=[S_["xk"], "g_cw"], wr=[S_["ak"]])

            def st_taps(S_):
                for k in range(1, 4):
                    T.op("dve", lambda: nc.vector.scalar_tensor_tensor(out=S_["acc"], in0=S_["xc"][:, k:k + 512],
                                                                       scalar=cw[:, S_["cj"], k:k + 1], in1=S_["acc"],
                                                                       op0=ALU.mult, op1=ALU.add),
                         rd=[S_["xk"], "g_cw", S_["ak"]], wr=[S_["ak"]])

            def st_silu(S_, dst=None, dkey=None):
                d_ = S_["acc"] if dst is None else dst
                k_ = S_["ak"] if dkey is None else dkey
                T.op("act", lambda: nc.scalar.activation(out=d_, in_=S_["acc"], func=AF.Silu), rd=[S_["ak"]], wr=[k_])

            def st_sq(S_):
                T.op("act", lambda: nc.scalar.activation(out=sq[:, S_["sqs"], :], in_=S_["acc"], func=AF.Square),
                     rd=[S_["ak"]], wr=[("sq", S_["sqs"])])
                T.op("pe", lambda: nc.tensor.matmul(ps[S_["pb"]][:], lhsT=onesb, rhs=sq[:, S_["sqs"], :], start=True, stop=True),
                     rd=["cb", ("sq", S_["sqs"])], wr=[("ps", S_["pb"])])

            def st_sqrt(S_):
                T.op("act", lambda: nc.scalar.activation(out=S_["rr"], in_=ps[S_["pb"]][:], func=AF.Sqrt, bias=epsb[:]),
                     rd=[("ps", S_["pb"]), "epsb"], wr=[S_["rk"]])

            def st_recip(S_):
                T.op("dve", lambda: nc.vector.reciprocal(out=S_["rr"], in_=S_["rr"]), rd=[S_["rk"]], wr=[S_["rk"]])

            for fn_ in (st_load, st_tap0, st_taps, st_silu, st_sq, st_sqrt, st_recip):
                fn_(SA)
                fn_(SB_)
            T.op("dve", lambda: nc.vector.scalar_tensor_tensor(out=g["qnT"], in0=SA["acc"], scalar=128.0 ** -0.5, in1=SA["rr"],
                                                               op0=ALU.mult, op1=ALU.mult),
                 rd=[SA["ak"], SA["rk"]], wr=["g_qnT"])
            T.op("dve", lambda: nc.vector.tensor_tensor(out=g["knf"], in0=SB_["acc"], in1=SB_["rr"], op=ALU.mult),
                 rd=[SB_["ak"], SB_["rk"]], wr=["g_knf"])
            T.op("pool", lambda: nc.gpsimd.tensor_tensor(out=g["qgT"], in0=g["qnT"], in1=g["EG"], op=ALU.mult),
                 rd=["g_qnT", "g_EG"], wr=["g_qgT"])
            T.op("act", lambda: nc.scalar.copy(out=g["knT"], in_=g["knf"]), rd=["g_knf"], wr=["g_knT"])
            T.op("pool", lambda: nc.gpsimd.tensor_tensor(out=g["kbT"], in0=g["knf"], in1=g["Bb"], op=ALU.mult),
                 rd=["g_knf", "g_Bb"], wr=["g_kbT"])
            SV = dict(SA)
            SV["cj"] = 16 + h
            SV["bank"] = banks[2]
            st_load(SV)
            st_tap0(SV)
            st_taps(SV)
            st_silu(SV, g["vsf"], "g_vsf")
            T.op("act", lambda: nc.scalar.activation(out=g["zs"], in_=ps[banks[3]][:], func=AF.Silu),
                 rd=[("ps", banks[3])], wr=["g_zs"])
            pL, pLT = nextbank(), nextbank()
            for n in range(4):
                cs = slice(n * 128, (n + 1) * 128)
                T.op("pe", lambda: nc.tensor.matmul(ps[pL][:, cs], lhsT=g["kbT"][:, cs], rhs=g["knT"][:, cs],
                                                    start=True, stop=True),
                     rd=["g_kbT", "g_knT"], wr=[("ps", pL)], inc=(n == 3))
            for n in range(4):
                cs = slice(n * 128, (n + 1) * 128)
                T.op("pe", lambda: nc.tensor.matmul(ps[pLT][:, cs], lhsT=g["knT"][:, cs], rhs=g["kbT"][:, cs],
                                                    start=True, stop=True),
                     rd=["g_kbT", "g_knT"], wr=[("ps", pLT)], inc=(n == 3))
            T.op("dve", lambda: nc.vector.tensor_tensor(out=g["Ar"][:, 0, :], in0=ps[pL][:], in1=g["E1"], op=ALU.mult),
                 rd=[("ps", pL), "g_E1"], wr=[("g_A", 0)])
            T.op("dve", lambda: nc.vector.tensor_tensor(out=g["ATr"][:, 0, :], in0=ps[pLT][:], in1=g["E2"], op=ALU.mult),
                 rd=[("ps", pLT), "g_E2"], wr=[("g_AT", 0)])
            for n in range(4):
                cs = slice(n * 128, (n + 1) * 128)
                T.op("dve", lambda: nc.vector.tensor_tensor(out=g["Xr"][:, cs], in0=identf[:], in1=g["AT"][:, 0, cs],
                                                            op=ALU.subtract),
                     rd=["identf", ("g_AT", 0)], wr=["g_X"])
            for k in range(1, 7):
                cur, prv = k % 2, (k - 1) % 2
                p1 = nextbank()
                for n in range(4):
                    cs = slice(n * 128, (n + 1) * 128)
                    T.op("pe", lambda: nc.tensor.matmul(ps[p1][:, cs], lhsT=g["ATr"][:, prv, cs], rhs=g["Ar"][:, prv, cs],
                                                        start=True, stop=True),
                         rd=[("g_A", prv), ("g_AT", prv)], wr=[("ps", p1)], inc=(n == 3))
                if k < 6:
                    p2 = nextbank()
                    for n in range(4):
                        cs = slice(n * 128, (n + 1) * 128)
                        T.op("pe", lambda: nc.tensor.matmul(ps[p2][:, cs], lhsT=g["Ar"][:, prv, cs], rhs=g["ATr"][:, prv, cs],
                                                            start=True, stop=True),
                             rd=[("g_A", prv), ("g_AT", prv)], wr=[("ps", p2)], inc=(n == 3))
                T.op("act", lambda: nc.scalar.copy(out=g["Ar"][:, cur, :], in_=ps[p1][:]), rd=[("ps", p1)], wr=[("g_A", cur)])
                if k < 6:
                    T.op("dve", lambda: nc.vector.tensor_copy(out=g["ATr"][:, cur, :], in_=ps[p2][:]), rd=[("ps", p2)],
                         wr=[("g_AT", cur)])
                p3 = nextbank()
                for n in range(4):
                    cs = slice(n * 128, (n + 1) * 128)
                    T.op("pe", lambda: nc.tensor.matmul(ps[p3][:, cs], lhsT=g["Ar"][:, cur, cs], rhs=g["Xr"][:, cs],
                                                        start=True, stop=True),
                         rd=[("g_A", cur), "g_X"], wr=[("ps", p3)], inc=(n == 3))
                T.op("dve", lambda: nc.vector.tensor_tensor(out=g["Xr"], in0=g["X"], in1=ps[p3][:], op=ALU.add),
                     rd=["g_X", ("ps", p3)], wr=["g_X"])
            pk_, pv_ = nextbank(), nextbank()
            for n in range(4):
                cs = slice(n * 128, (n + 1) * 128)
                T.op("pe", lambda: nc.tensor.transpose(out=ps[pk_][:, cs], in_=g["knf"][:, cs], identity=identf[:]),
                     rd=["g_knf", "identf"], wr=[("ps", pk_)], inc=(n == 3))
            for n in range(4):
                cs = slice(n * 128, (n + 1) * 128)
                T.op("pe", lambda: nc.tensor.transpose(out=ps[pv_][:, cs], in_=g["vsf"][:, cs], identity=identf[:]),
                     rd=["g_vsf", "identf"], wr=[("ps", pv_)], inc=(n == 3))
            for n in range(4):
                cs = slice(n * 128, (n + 1) * 128)
                T.op("dve", lambda: nc.vector.tensor_scalar(out=g["Rwr"][:, cs], in0=ps[pk_][:, cs], scalar1=g["bgc"][:, n, h:h + 1],
                                                            scalar2=None, op0=ALU.mult),
                     rd=[("ps", pk_), "g_bgc"], wr=["g_Rw"])
                T.op("dve", lambda: nc.vector.tensor_scalar(out=g["kg"][:, cs], in0=ps[pk_][:, cs], scalar1=g["kgc"][:, n:n + 1],
                                                            scalar2=None, op0=ALU.mult),
                     rd=[("ps", pk_), "g_kgc"], wr=["g_kg"])
                T.op("act", lambda: nc.scalar.activation(out=g["Rur"][:, cs], in_=ps[pv_][:, cs], func=AF.Copy,
                                                         scale=g["gcol"][:, n, 8 + h:9 + h]),
                     rd=[("ps", pv_), "g_gcol"], wr=["g_Ru"])
            pu, pw = nextbank(), nextbank()
            for n in range(4):
                cs = slice(n * 128, (n + 1) * 128)
                T.op("pe", lambda: nc.tensor.matmul(ps[pu][:, cs], lhsT=g["Xr"][:, cs], rhs=g["Rur"][:, cs], start=True, stop=True),
                     rd=["g_X", "g_Ru"], wr=[("ps", pu)], inc=(n == 3))
            for n in range(4):
                cs = slice(n * 128, (n + 1) * 128)
                T.op("pe", lambda: nc.tensor.matmul(ps[pw][:, cs], lhsT=g["Rwr"][:, cs], rhs=g["Xr"][:, cs], start=True, stop=True),
                     rd=["g_X", "g_Rw"], wr=[("ps", pw)], inc=(n == 3))
            T.op("act", lambda: nc.scalar.copy(out=g["u"], in_=ps[pu][:]), rd=[("ps", pu)], wr=["g_u"])
            T.op("dve", lambda: nc.vector.tensor_copy(out=g["wT"], in_=ps[pw][:]), rd=[("ps", pw)], wr=["g_wT"])
            pp = nextbank()
            for n in range(4):
                cs = slice(n * 128, (n + 1) * 128)
                T.op("pe", lambda: nc.tensor.matmul(ps[pp][:, cs], lhsT=g["knT"][:, cs], rhs=g["qnT"][:, cs], start=True, stop=True),
                     rd=["g_knT", "g_qnT"], wr=[("ps", pp)], inc=(n == 3))
            T.op("dve", lambda: nc.vector.tensor_tensor(out=g["pTg"], in0=ps[pp][:], in1=g["E3"], op=ALU.mult),
                 rd=[("ps", pp), "g_E3"], wr=["g_pTg"])
            pO = 5
            T.op("act", lambda: nc.scalar.copy(out=g["Sb"], in_=g["S"][:, h, :]), rd=[("g_S", h)], wr=["g_Sb"])
            for n in range(4):
                cs = slice(n * 128, (n + 1) * 128)
                v2 = n % 2
                pvn = nextbank()
                T.op("pe", lambda: nc.tensor.matmul(ps[pvn][:, 0:128], lhsT=g["wT"][:, cs], rhs=g["Sb"],
                                                    start=True, stop=True),
                     rd=["g_wT", "g_Sb"], wr=[("ps", pvn)])
                T.op("dve", lambda: nc.vector.tensor_tensor(out=g["vnb"][:, v2, :], in0=g["u"][:, cs], in1=ps[pvn][:, 0:128],
                                                            op=ALU.subtract),
                     rd=["g_u", ("ps", pvn)], wr=[("g_vnb", v2)])
                T.op("pe", lambda: nc.tensor.matmul(ps[pO][:, cs], lhsT=g["Sb"], rhs=g["qgT"][:, cs],
                                                    start=True, stop=False),
                     rd=["g_Sb", "g_qgT"], wr=[("ps", pO)], inc=False)
                T.op("pe", lambda: nc.tensor.matmul(ps[pO][:, cs], lhsT=g["vnb"][:, v2, :], rhs=g["pTg"][:, cs],
                                                    start=False, stop=True),
                     rd=[("g_vnb", v2), "g_pTg"], wr=[("ps", pO)])
                pst = nextbank()
                T.op("pe", lambda: nc.tensor.matmul(ps[pst][:, 0:128], lhsT=g["kg"][:, cs], rhs=g["vnb"][:, v2, :],
                                                    start=True, stop=True),
                     rd=["g_kg", ("g_vnb", v2)], wr=[("ps", pst)])
                T.op("dve", lambda: nc.vector.scalar_tensor_tensor(out=g["S"][:, h, :], in0=g["S"][:, h, :],
                                                                   scalar=g["glc"][:, n:n + 1], in1=ps[pst][:, 0:128],
                                                                   op0=ALU.mult, op1=ALU.add),
                     rd=[("g_S", h), "g_glc", ("ps", pst)], wr=[("g_S", h)])
                T.op("act", lambda: nc.scalar.copy(out=g["Sb"], in_=g["S"][:, h, :]), rd=[("g_S", h)],
                     wr=["g_Sb"])
            T.op("act", lambda: nc.scalar.activation(out=sq[:, 0, :], in_=ps[pO][:], func=AF.Square), rd=[("ps", pO)],
                 wr=[("sq", 0)])
            T.op("pe", lambda: nc.tensor.matmul(ps[7][:], lhsT=onesb, rhs=sq[:, 0, :], start=True, stop=True),
                 rd=["cb", ("sq", 0)], wr=[("ps", 7)])
            T.op("act", lambda: nc.scalar.activation(out=g["rr"], in_=ps[7][:], func=AF.Sqrt, scale=1.0 / 128, bias=epsb[:]),
                 rd=[("ps", 7), "epsb"], wr=["rden"])
            T.op("dve", lambda: nc.vector.reciprocal(out=g["rr"], in_=g["rr"]), rd=["rden"], wr=["rden"])
            T.op("dve", lambda: nc.vector.scalar_tensor_tensor(out=g["acc"], in0=ps[pO][:], scalar=g["gain"], in1=g["rr"],
                                                               op0=ALU.mult, op1=ALU.mult),
                 rd=[("ps", pO), "g_gain", "rden"], wr=["g_acc"])
            T.op("dve", lambda: nc.vector.tensor_tensor(out=catT[:, h, :], in0=g["acc"], in1=g["zs"], op=ALU.mult),
                 rd=["g_acc", "g_zs"], wr=[("catT", h)])
        proj_fm("gdn_w_in", jg, 4112, 4, uT, "uT", TB, qmem_handler)

    def emit():
        T.dma("sp", c_io, identf[:], identf_d, wr=["identf"])
        T.dma("sp", c_io, gains[:], gains_d, wr=["gains"])
        T.dma("sp", c_io, cb[:], cb_d, wr=["cb"])
        T.op("pool", lambda: nc.gpsimd.memset(epsb[:], EPS), wr=["epsb"])
        T.op("pool", lambda: nc.gpsimd.memset(oneb[:], 1.0), wr=["oneb"])
        prep_mem()
        for t in range(16):
            sl = t % 2
            T.dma("sp", c_io, xin(sl), x_d[t * 128:(t + 1) * 128, :], wr=xink(sl))
            for half in range(2):
                pbn = nextbank()
                pk = ("ps", pbn)
                for q in range(4):
                    c = half * 4 + q
                    T.op("pe", lambda: nc.tensor.transpose(out=ps[pbn][:, q * 128:(q + 1) * 128],
                                                           in_=xin(sl)[:, c * 128:(c + 1) * 128],
                                                           identity=identf[:]),
                         rd=xink(sl) + ["identf"], wr=[pk], inc=(q == 3))
                dst = hT[:, half * 4:(half + 1) * 4, t * 128:(t + 1) * 128]
                src = ps[pbn][:].rearrange("p (q n) -> p q n", q=4)
                wr = [("hT", c, t // 4) for c in range(half * 4, half * 4 + 4)]
                if half == 0:
                    T.op("act", lambda: nc.scalar.copy(out=dst, in_=src), rd=[pk], wr=wr)
                else:
                    T.op("dve", lambda: nc.vector.tensor_copy(out=dst, in_=src), rd=[pk], wr=wr)
        for l in layers:
            kind = l % 3
            T.barrier()
            layer_alloc(kind)
            w_block(l, None)
            mem_kv(l)
            for b in range(NB):
                w_block(l, b)
                pre_norm(l, 0, b)
                if kind == 2:
                    if b == 0:
                        sb_setup()
                    sb_inproj(l, b)
                    sb_attn(b)
                elif kind == 1:
                    if b == 0:
                        ml_setup()
                    ml_inproj(l, b)
                    ml_attn(l, b)
                else:
                    if b == 0:
                        gdn_setup(l)
                    gdn_block(l, b)
                mem_attn(b)
                out_proj(l, b)
                ffn(l, b)
            T.barrier()
            layer_free()
        for t in range(16):
            sl = t % 2
            for half in range(2):
                pbn = nextbank()
                pk = ("ps", pbn)
                for q in range(4):
                    c = half * 4 + q
                    T.op("pe", lambda: nc.tensor.transpose(out=ps[pbn][:, q * 128:(q + 1) * 128],
                                                           in_=hT[:, c, t * 128:(t + 1) * 128],
                                                           identity=identf[:]),
                         rd=[("hT", c, t // 4), "identf"], wr=[pk], inc=(q == 3))
                dst = xin(sl)[:, half * 512:(half + 1) * 512]
                if half == 0:
                    T.op("act", lambda: nc.scalar.copy(out=dst, in_=ps[pbn][:]), rd=[pk], wr=[("ysb", 2 * sl + half)])
                else:
                    T.op("dve", lambda: nc.vector.tensor_copy(out=dst, in_=ps[pbn][:]), rd=[pk],
                         wr=[("ysb", 2 * sl + half)])
            T.dma("sp", c_io, out_d[t * 128:(t + 1) * 128, :], xin(sl), rd=xink(sl), wr=[("out", t)])
        T.wait_key("sp", [("out", t) for t in range(16)])

    T.dry = True
    emit()
    T.dry = False
    ring["i"] = 0
    wstate["nf32"] = 0
    wstate["nb16"] = 0
    wstate["slot_of"] = {}
    assert max(x[2] for x in wplan) < NWC
    emit()
    P.finish()
    return nc, T


CB_ONES = 0
CB_NEGONES = 128
CB_IDENT = 256
CB_NEGMINCL = 384
CB_MASK01 = 512
NCB = CB_MASK01 + 4 * TB
G_MEM = DEPTH * 4 * 8
NG = G_MEM + 8


def host_consts(inputs):
    g = np.stack([inputs["norm_pre_mix"], inputs["norm_post_mix"], inputs["norm_pre_ffn"],
                  inputs["norm_post_ffn"]], axis=1)
    g = g.reshape(DEPTH, 4, 8, 128).transpose(3, 0, 1, 2).reshape(128, DEPTH * 4 * 8)
    gm = inputs["mem_norm"].reshape(8, 128).T
    gains = np.concatenate([g, gm], axis=1).astype(np.float32)
    cbf = np.zeros((128, NCB), np.float32)
    cbf[:, CB_ONES:CB_ONES + 128] = 1.0
    cbf[:, CB_NEGONES:CB_NEGONES + 128] = -1.0
    cbf[:, CB_IDENT:CB_IDENT + 128] = np.eye(128)
    jj, ss = np.meshgrid(np.arange(128), np.arange(128), indexing="ij")
    cbf[:, CB_NEGMINCL:CB_NEGMINCL + 128] = -(jj >= ss).astype(np.float32)
    sidx = np.arange(128)[:, None]
    tidx = np.arange(TB)[None, :]
    for i in range(4):
        m = ((sidx + 128 * i) < tidx).astype(np.float32)
        cbf[:, CB_MASK01 + i * TB:CB_MASK01 + (i + 1) * TB] = m
    sel = np.zeros((16, 8, 128), np.float32)
    for h in range(8):
        sel[8 + h, h, :] = 1.0
    m01i = np.zeros((128, 4 * TB), np.float32)
    for i in range(4):
        m01i[:, i * TB:(i + 1) * TB] = ((sidx + 128 * i) <= tidx)
    mlbias = np.concatenate([inputs["ml_i_bias"][0], inputs["ml_f_bias"][0]]).reshape(16, 1).astype(np.float32)
    sel8 = np.zeros((8, 8, 128), np.float32)
    for h in range(8):
        sel8[h, h, :] = 1.0
    pidx = np.arange(128)[:, None]
    fidx = np.arange(128)[None, :]
    gm = np.concatenate([np.tile(np.where(fidx < pidx, 0.0, NEGBIG), (1, 4)),
                         np.tile(np.where(pidx < fidx, 0.0, NEGBIG), (1, 4)),
                         np.tile(np.where(pidx <= fidx, 0.0, NEGBIG), (1, 4))], axis=1).astype(np.float32)
    gcw = inputs["gdn_conv"].reshape(2, 4, 24, 128).transpose(0, 3, 2, 1).reshape(2, 128, 96)
    return {
        "sel8": sel8.reshape(8, 8 * 128),
        "gdn_masks": gm.astype(ml_dtypes.bfloat16),
        "gdn_cw": np.ascontiguousarray(gcw.astype(np.float32)),
        "gdn_dtb": np.ascontiguousarray(inputs["gdn_dt_bias"].reshape(2, 8, 1).astype(np.float32)),
        "gdn_alog": np.ascontiguousarray(inputs["gdn_a_log"].reshape(2, 8, 1).astype(np.float32)),
        "gdn_gain": np.ascontiguousarray(inputs["gdn_out_norm"].reshape(2, 128, 1).astype(np.float32)),
        "selc": sel.reshape(16, 8 * 128),
        "mlbias": mlbias,
        "mlgain": np.ascontiguousarray(inputs["ml_out_norm"][0].reshape(128, 1).astype(np.float32)),
        "mask01incl": m01i.astype(ml_dtypes.bfloat16),
        "identf": np.eye(128, dtype=np.float32),
        "gains": np.ascontiguousarray(gains),
        "cbf": cbf.astype(ml_dtypes.bfloat16),
    }


WNAMES = ["w_ffn_in", "w_ffn_out", "w_mem_kv", "w_out", "gdn_w_in", "ml_w_in", "sb_w_in"]


def make_in_maps(inputs, cores, x_override=None):
    consts = host_consts(inputs)
    in_maps = []
    for core in cores:
        m = dict(consts)
        m["x"] = np.ascontiguousarray(inputs["x"][core] if x_override is None else x_override[core])
        m["mem"] = np.ascontiguousarray(inputs["mem"][core])
        for w in WNAMES:
            m[w] = inputs[w]
        in_maps.append(m)
    return in_maps


def kernel(**inputs):
    inputs = {k: np.asarray(v) for k, v in inputs.items()}
    nc, _ = build(list(range(DEPTH)))
    in_maps = make_in_maps(inputs, list(range(8)))
    res = run_bass_kernel_spmd(nc, in_maps, core_ids=list(range(8)))
    return np.stack([r["out"] for r in res.results], axis=0)
```

```python
import numpy as np
import ml_dtypes
import concourse.bass as bass
import concourse.mybir as mybir
from concourse.bass_utils import run_bass_kernel_spmd

F32 = mybir.dt.float32
BF16 = mybir.dt.bfloat16
F32R = mybir.dt.float32r
AF = mybir.ActivationFunctionType
ALU = mybir.AluOpType

S = 2048
D = 1024
NB = 4
TB = 512
DEPTH = 4
N_MEM = 256
D_FF = 2816
EPS = 1e-6
SEM_LIMIT = 30000


class Counter:
    def __init__(self, tr, name, inorder):
        self.tr = tr
        self.name = name
        self.inorder = inorder
        self.sems = []
        self.final = {}
        self.epoch = -1
        self.val = 0
        self._new_epoch()

    def _new_epoch(self):
        if self.epoch >= 0:
            self.final[self.epoch] = self.val
        self.epoch += 1
        self.val = 0
        self.sems.append(self.tr.new_sem(f"{self.name}_{self.epoch}"))

    def reserve(self, amount):
        if self.val + amount > SEM_LIMIT:
            self._new_epoch()
        self.val += amount
        return (self.epoch, self.val)

    def peek_next(self, amount=1):
        if self.val + amount > SEM_LIMIT:
            return (self.epoch + 1, amount)
        return (self.epoch, self.val + amount)


class Tracker:
    def __init__(self, nc):
        self.nc = nc
        self._sem_stack = []
        self.eng = {"pe": nc.tensor, "act": nc.scalar, "dve": nc.vector, "pool": nc.gpsimd, "sp": nc.sync}
        self.cnt = {k: Counter(self, k, True) for k in self.eng}
        self.waited = {k: {} for k in self.eng}
        self.last_w = {}
        self.readers = {}
        self.pending_noinc = {k: False for k in self.eng}
        self.n_inst = {k: 0 for k in self.eng}
        self.dry = False

    def new_sem(self, name):
        cm = self.nc.semaphore(name)
        h = cm.__enter__()
        self._sem_stack.append(cm)
        return h

    def new_counter(self, name):
        c = Counter(self, name, False)
        self.cnt[name] = c
        return c

    def _need(self, deps, cnt, tok):
        k = (cnt.name, tok[0])
        if k not in deps or deps[k][1] < tok[1]:
            deps[k] = (cnt, tok[1])

    def _wait_all(self, e, rd, wr):
        deps = {}
        for r in rd:
            lw = self.last_w.get(r)
            if lw is not None:
                self._need(deps, lw[0], lw[1])
        for w in wr:
            lw = self.last_w.get(w)
            if lw is not None:
                self._need(deps, lw[0], lw[1])
            for (c, tok) in self.readers.get(w, {}).values():
                self._need(deps, c, tok)
        wd = self.waited[e]
        for (cname, ep), (cnt, val) in deps.items():
            if cname == e and e == "pe":
                continue
            if wd.get((cname, ep), 0) >= val:
                continue
            if cnt.inorder:
                if any(k[0] == cname and k[1] > ep for k in wd):
                    continue
            self.eng[e].wait_ge(cnt.sems[ep], val)
            wd[(cname, ep)] = val

    def _record(self, cnt, tok, rd, wr):
        for r in rd:
            self.readers.setdefault(r, {})[cnt.name] = (cnt, tok)
        for w in wr:
            self.last_w[w] = (cnt, tok)
            self.readers[w] = {}

    def op(self, e, fn, rd=(), wr=(), inc=True):
        if self.dry:
            return None
        self._wait_all(e, rd, wr)
        inst = fn()
        self.n_inst[e] += 1
        cnt = self.cnt[e]
        if inc:
            tok = cnt.reserve(1)
            inst.then_inc(cnt.sems[tok[0]], 1)
        else:
            tok = cnt.peek_next(1)
        self._record(cnt, tok, rd, wr)
        return inst

    def dma(self, q, cnt, out, in_, rd=(), wr=()):
        if self.dry:
            return None
        self._wait_all(q, rd, wr)
        inst = self.eng[q].dma_start(out=out, in_=in_)
        tok = cnt.reserve(16)
        inst.then_inc(cnt.sems[tok[0]], 16)
        self.n_inst[q] += 1
        self._record(cnt, tok, rd, wr)
        return inst

    def alias(self, dst_keys, src_keys):
        if self.dry:
            return
        acc = {}
        for s in src_keys:
            lw = self.last_w.get(s)
            items = list(self.readers.get(s, {}).values())
            if lw is not None:
                items.append(lw)
            for (c, tok) in items:
                k = (c.name, tok[0])
                if k not in acc or acc[k][1][1] < tok[1]:
                    acc[k] = (c, tok)
        for d in dst_keys:
            rd = self.readers.setdefault(d, {})
            for (cname, ep), (c, tok) in acc.items():
                nm = f"{cname}@{ep}"
                if nm not in rd or rd[nm][1][1] < tok[1]:
                    rd[nm] = (c, tok)

    def barrier(self):
        if self.dry:
            return
        for e in self.eng:
            wd = self.waited[e]
            for c in self.cnt.values():
                eps = range(c.epoch + 1) if not c.inorder else [c.epoch]
                for ep in eps:
                    val = c.val if ep == c.epoch else c.final[ep]
                    if val <= 0 or wd.get((c.name, ep), 0) >= val:
                        continue
                    if c.name == e and e == "pe":
                        continue
                    self.eng[e].wait_ge(c.sems[ep], val)
                    wd[(c.name, ep)] = val

    def wait_key(self, e, keys):
        if self.dry:
            return
        self._wait_all(e, keys, ())

    def close(self):
        for cm in reversed(self._sem_stack):
            cm.__exit__(None, None, None)


class Prog:
    def __init__(self, layers, load_x=True):
        self.layers = layers
        self.nc = bass.Bass("TRN2", target_bir_lowering=False)
        self._stack = []
        self.T = Tracker(self.nc)

    def dram(self, name, shape, dt, kind):
        return self.nc.dram_tensor(name, list(shape), dt, kind=kind).ap()

    def sb(self, name, shape, dt):
        cm = self.nc.sbuf_tensor(name, list(shape), dt)
        t = cm.__enter__()
        self._stack.append(cm)
        return t

    def psum(self, name, shape, dt):
        cm = self.nc.psum_tensor(name, list(shape), dt)
        t = cm.__enter__()
        self._stack.append(cm)
        return t

    def finish(self):
        for cm in reversed(self._stack):
            cm.__exit__(None, None, None)
        self.T.close()


GDN_IN = 4624
ML_IN = 3600
SB_IN = 3584
NWS = 3
NWST = 2
NEGBIG = -30000.0


def build(layers, first=True, last=True):
    P = Prog(layers)
    nc, T = P.nc, P.T
    kinds = [l % 3 for l in layers]
    x_d = P.dram("x", [S, D], F32, "ExternalInput")
    out_d = P.dram("out", [S, D], F32, "ExternalOutput")
    mem_d = P.dram("mem", [N_MEM, D], F32, "ExternalInput")
    identf_d = P.dram("identf", [128, 128], F32, "ExternalInput")
    cb_d = P.dram("cbf", [128, NCB], BF16, "ExternalInput")
    gains_d = P.dram("gains", [128, NG], F32, "ExternalInput")
    W = {
        "w_ffn_in": P.dram("w_ffn_in", [DEPTH, D, 2 * D_FF], F32, "ExternalInput"),
        "w_ffn_out": P.dram("w_ffn_out", [DEPTH, D_FF, D], F32, "ExternalInput"),
        "w_mem_kv": P.dram("w_mem_kv", [DEPTH, D, 1024], F32, "ExternalInput"),
        "w_out": P.dram("w_out", [DEPTH, 1536, D], F32, "ExternalInput"),
        "gdn_w_in": P.dram("gdn_w_in", [2, D, GDN_IN], F32, "ExternalInput"),
        "ml_w_in": P.dram("ml_w_in", [1, D, ML_IN], F32, "ExternalInput"),
        "sb_w_in": P.dram("sb_w_in", [1, D, SB_IN], F32, "ExternalInput"),
    }

    hT = P.sb("hT", [128, 8, S], F32)
    uT = P.sb("uT", [128, 8, TB], BF16)
    sq = P.sb("sq", [128, 2, TB], BF16)
    rstd = P.sb("rstd", [128, TB], F32)
    ysb_raw = P.sb("ysb", [128, 8 * TB], F32)
    ysb = ysb_raw[:, :].rearrange("p (c n) -> p c n", c=8)
    R1 = P.sb("R1", [128, 22 * TB], BF16)
    aT = R1[:, :].rearrange("p (j n) -> p j n", j=22)
    sg = P.sb("sg", [128, 1, TB], BF16)
    tmpn = P.sb("tmpn", [128, 1, TB], F32)
    LS = {}
    identf = P.sb("identf_sb", [128, 128], F32)
    cb = P.sb("cb_sb", [128, NCB], BF16)
    gains = P.sb("gains_sb", [128, NG], F32)
    wst = P.sb("wst", [128, NWST, 2048], F32)
    wbf = P.sb("wbf", [128, NWS, 2048], BF16)
    catT = P.sb("catT", [128, 12, TB], BF16)
    qmT = P.sb("qmT", [128, 4, TB], BF16)
    memnT = P.sb("memnT", [128, 8, N_MEM], BF16)
    memkT = P.sb("memkT", [128, 4, N_MEM], BF16)
    memv = P.sb("memv", [128, 2, 512], BF16)
    negkmax = P.sb("negkmax", [128, 4], F32)
    cq = P.sb("cq", [1, TB], F32)
    negc = P.sb("negc", [1, TB], BF16)
    pT = P.sb("pT", [128, 2, TB], BF16)
    rden = P.sb("rden", [128, TB], F32)
    epsb = P.sb("epsb", [128, 1], F32)
    oneb = P.sb("oneb", [128, 1], F32)
    ps = [P.psum(f"ps{i}", [128, TB], F32) for i in range(8)]

    onesb = cb[:, CB_ONES:CB_ONES + 128]
    negones = cb[:, CB_NEGONES:CB_NEGONES + 128]
    identb = cb[:, CB_IDENT:CB_IDENT + 128]
    negMincl = cb[:, CB_NEGMINCL:CB_NEGMINCL + 128]

    c_io = T.new_counter("io")
    c_w = [T.new_counter(f"w{i}") for i in range(NWST)]

    def gain(l, which, c):
        i = (l * 4 + which) * 8 + c
        return gains[:, i:i + 1]

    def xin(sl):
        return ysb[:, 2 * sl:2 * sl + 2, :].rearrange("p a b -> p (a b)")

    def xink(sl):
        return [("ysb", 2 * sl), ("ysb", 2 * sl + 1)]

    ring = {"i": 0}

    def nextbank():
        ring["i"] = (ring["i"] + 1) % 5
        return ring["i"]

    wplan = []
    wstate = {"issued": 0, "next": 0, "lb": None, "idx": 0}
    NWC = 72
    wcache_d = P.dram("wcache", [NWC, 128, 2048], BF16, "Internal")
    c_wb = [T.new_counter(f"wb{i}") for i in range(NWS + 2 * NWST)]
    c_wc = T.new_counter("wc")

    wst_b = wst.bitcast(BF16)
    NSLOT = NWS + 2 * NWST

    def WT(slot):
        if slot < NWS:
            return wbf[:, slot, :]
        j = slot - NWS
        return wst_b[:, j // 2, (j % 2) * 2048:(j % 2 + 1) * 2048]

    def WK(slot):
        if slot < NWS:
            return ("wbf", slot)
        j = slot - NWS
        return ("wst", j // 2, (j % 2) * 1024)

    def w_issue(i):
        req, lb, idx = wplan[i]
        n = sum(kc * ncols for (_, _, _, kc, _, ncols) in req)
        if lb is not None and lb[1] > 0:
            slot = wstate["nb16"] % NSLOT
            wstate["nb16"] += 1
            wstate["slot_of"][i] = slot
            T.dma("sp", c_wb[slot], WT(slot)[:, 0:n], wcache_d[idx, :, 0:n], rd=[("wc", idx)], wr=[WK(slot)])
            return
        slot = wstate["nf32"] % NWS
        st = wstate["nf32"] % NWST
        wstate["nf32"] += 1
        wstate["slot_of"][i] = slot
        off = 0
        for pi, (name, l, r0, kc, c0, ncols) in enumerate(req):
            dst = wst[:, st, off:off + kc * ncols].rearrange("p (k n) -> p k n", k=kc)
            src = W[name][l, r0:r0 + kc * 128, c0:c0 + ncols].rearrange("(k p) n -> p k n", p=128)
            if len(req) == 1:
                wk = [("wst", st, 0), ("wst", st, 1024)]
            else:
                assert kc * ncols <= 1024 and off == pi * 1024
                wk = [("wst", st, pi * 1024)]
            T.dma("sp", c_w[st], dst, src, wr=wk)
            off += kc * ncols
        T.op("act", lambda: nc.scalar.copy(out=wbf[:, slot, 0:n], in_=wst[:, st, 0:n]),
             rd=[("wst", st, 0), ("wst", st, 1024)], wr=[("wbf", slot)])
        if lb is not None:
            T.dma("pool", c_wc, wcache_d[idx, :, 0:n], wbf[:, slot, 0:n], rd=[("wbf", slot)], wr=[("wc", idx)])

    def w_get(req):
        req = tuple(req)
        if T.dry:
            wplan.append((req, wstate["lb"], wstate["idx"]))
            wstate["idx"] += 1
            return 0
        i = wstate["next"]
        assert wplan[i][0] == req, (wplan[i], req)

        def depth(k):
            lb = wplan[k][1]
            return (NSLOT - 1) if (lb is not None and lb[1] > 0) else (NWS - 1)
        def isb16(k):
            lb = wplan[k][1]
            return lb is not None and lb[1] > 0
        while wstate["issued"] < len(wplan):
            k = wstate["issued"]
            if k > i + depth(k) - 1:
                break
            if k > i and isb16(k) != isb16(i):
                break
            w_issue(k)
            wstate["issued"] += 1
        wstate["next"] += 1
        return wstate["slot_of"][i]

    def w_block(l, b):
        wstate["lb"] = (l, b) if b is not None else None
        wstate["idx"] = 0

    def wview(slot, off, kc, ncols):
        return WT(slot)[:, off:off + kc * ncols].rearrange("p (k n) -> p k n", k=kc)

    lcount = {"n": 0}

    def layer_alloc(kind):
        lcount["n"] += 1
        tag = lcount["n"]
        cms = []

        def mk(name, shape, dt):
            cm = nc.sbuf_tensor(f"{name}_{tag}", list(shape), dt)
            t = cm.__enter__()
            cms.append(cm)
            return t
        if kind == 0:
            LS["gF"] = mk("gF", [128, 3584], F32)
            LS["gR2"] = mk("gR2", [128, 2944], F32)
            gdn_alloc()
        else:
            LS["R2"] = mk("R2", [128, 12544], BF16)
            LS["R2f"] = LS["R2"].bitcast(F32)
            if kind == 2:
                sb_alloc()
            else:
                ml_alloc()
        LS["cms"] = cms

    def layer_free():
        for cm in reversed(LS["cms"]):
            cm.__exit__(None, None, None)
        LS["cms"] = []

    def rms_stats(src_fn, src_keys, n):
        pk = ("ps", 7)
        for c in range(8):
            k = c % 2
            T.op("act", lambda: nc.scalar.activation(out=sq[:, k, 0:n], in_=src_fn(c), func=AF.Square),
                 rd=[src_keys(c)], wr=[("sq", k)])
            T.op("pe", lambda: nc.tensor.matmul(ps[7][:, 0:n], lhsT=onesb, rhs=sq[:, k, 0:n],
                                                start=(c == 0), stop=(c == 7)),
                 rd=["cb", ("sq", k)], wr=[pk])
        T.op("act", lambda: nc.scalar.activation(out=rstd[:, 0:n], in_=ps[7][:, 0:n], func=AF.Sqrt,
                                                 scale=1.0 / D, bias=epsb[:]),
             rd=[pk, "epsb"], wr=["rstd"])
        T.op("dve", lambda: nc.vector.reciprocal(out=rstd[:, 0:n], in_=rstd[:, 0:n]), rd=["rstd"], wr=["rstd"])

    def pre_norm(l, which, b):
        rms_stats(lambda c: hT[:, c, b * TB:(b + 1) * TB], lambda c: ("hT", c, b), TB)
        for c in range(8):
            T.op("dve", lambda: nc.vector.scalar_tensor_tensor(
                out=uT[:, c, :], in0=hT[:, c, b * TB:(b + 1) * TB], scalar=gain(l, which, c),
                in1=rstd[:], op0=ALU.mult, op1=ALU.mult),
                rd=[("hT", c, b), "gains", "rstd"], wr=[("uT", c)])

    def post_norm_add(l, which, b):
        rms_stats(lambda c: ysb[:, c, :], lambda c: ("ysb", c), TB)
        for c in range(8):
            k = 0
            T.op("dve", lambda: nc.vector.scalar_tensor_tensor(
                out=tmpn[:, k, :], in0=ysb[:, c, :], scalar=gain(l, which, c),
                in1=rstd[:], op0=ALU.mult, op1=ALU.mult),
                rd=[("ysb", c), "gains", "rstd"], wr=[("tmpn", k)])
            T.op("pool", lambda: nc.gpsimd.tensor_tensor(
                out=hT[:, c, b * TB:(b + 1) * TB], in0=hT[:, c, b * TB:(b + 1) * TB],
                in1=tmpn[:, k, :], op=ALU.add),
                rd=[("hT", c, b), ("tmpn", k)], wr=[("hT", c, b)])

    def proj_fm(wname, wl, c0, nchunks, xT, xkey, n, handler):
        j = 0
        while j < nchunks:
            nn = min(2, nchunks - j)
            slot = w_get([(wname, wl, 0, 8, c0 + j * 128, nn * 128)])
            wv = wview(slot, 0, 8, nn * 128)
            for jj in range(nn):
                pb = nextbank()
                for kc in range(8):
                    T.op("pe", lambda: nc.tensor.matmul(ps[pb][:, 0:n], lhsT=wv[:, kc, jj * 128:(jj + 1) * 128],
                                                        rhs=xT[:, kc, 0:n], start=(kc == 0), stop=(kc == 7)),
                         rd=[WK(slot), (xkey, kc)], wr=[("ps", pb)], inc=(kc == 7))
                handler(j + jj, pb)
            j += nn

    def proj_tm(wname, wl, c0, ncols, xT, xkey, ntt, handler):
        slot = w_get([(wname, wl, 0, 8, c0, ncols)])
        wv = wview(slot, 0, 8, ncols)
        for tt in range(ntt):
            pb = nextbank()
            for kc in range(8):
                T.op("pe", lambda: nc.tensor.matmul(ps[pb][:, 0:ncols], lhsT=xT[:, kc, tt * 128:(tt + 1) * 128],
                                                    rhs=wv[:, kc, :], start=(kc == 0), stop=(kc == 7)),
                     rd=[WK(slot), (xkey, kc)], wr=[("ps", pb)], inc=(kc == 7))
            handler(tt, pb)

    mixer_keys = []
    ysb_overlay = []

    def ffn(l, b):
        T.alias([("aT", j) for j in range(22)], mixer_keys)
        pre_norm(l, 2, b)
        for j in range(22):
            slot = w_get([("w_ffn_in", l, 0, 8, j * 128, 128), ("w_ffn_in", l, 0, 8, D_FF + j * 128, 128)])
            wv = WT(slot).rearrange("p (g k n) -> p g k n", g=2, k=8)
            pg, pu = nextbank(), nextbank()
            for gi, pbank in ((0, pg), (1, pu)):
                for kc in range(8):
                    T.op("pe", lambda: nc.tensor.matmul(ps[pbank][:], lhsT=wv[:, gi, kc, :], rhs=uT[:, kc, :],
                                                        start=(kc == 0), stop=(kc == 7)),
                         rd=[WK(slot), ("uT", kc)], wr=[("ps", pbank)], inc=(kc == 7))
            k = 0
            T.op("act", lambda: nc.scalar.activation(out=sg[:, k, :], in_=ps[pg][:], func=AF.Silu),
                 rd=[("ps", pg)], wr=[("sg", k)])
            T.op("dve", lambda: nc.vector.tensor_tensor(out=aT[:, j, :], in0=sg[:, k, :], in1=ps[pu][:],
                                                        op=ALU.mult),
                 rd=[("sg", k), ("ps", pu)], wr=[("aT", j)])
        for c in range(8):
            pbank = nextbank()
            for hh in range(2):
                slot = w_get([("w_ffn_out", l, hh * 1408, 11, c * 128, 128)])
                wv = wview(slot, 0, 11, 128)
                for kc in range(11):
                    kk = hh * 11 + kc
                    T.op("pe", lambda: nc.tensor.matmul(ps[pbank][:], lhsT=wv[:, kc, :], rhs=aT[:, kk, :],
                                                        start=(kk == 0), stop=(kk == 21)),
                         rd=[WK(slot), ("aT", kk)], wr=[("ps", pbank)], inc=(kc == 10))
            T.op("act", lambda: nc.scalar.copy(out=ysb[:, c, :], in_=ps[pbank][:]),
                 rd=[("ps", pbank)], wr=[("ysb", c)])
        post_norm_add(l, 3, b)

    def out_proj(l, b):
        T.alias([("ysb", c) for c in range(8)], ysb_overlay)
        for c in range(8):
            slot = w_get([("w_out", l, 0, 12, c * 128, 128)])
            wv = wview(slot, 0, 12, 128)
            pbank = nextbank()
            for kc in range(12):
                T.op("pe", lambda: nc.tensor.matmul(ps[pbank][:], lhsT=wv[:, kc, :], rhs=catT[:, kc, :],
                                                    start=(kc == 0), stop=(kc == 11)),
                     rd=[WK(slot), ("catT", kc)], wr=[("ps", pbank)], inc=(kc == 11))
            T.op("act", lambda: nc.scalar.copy(out=ysb[:, c, :], in_=ps[pbank][:]),
                 rd=[("ps", pbank)], wr=[("ysb", c)])
        post_norm_add(l, 1, b)

    def prep_mem():
        for t in range(2):
            sl = t % 2
            T.dma("sp", c_io, xin(sl), mem_d[t * 128:(t + 1) * 128, :], wr=xink(sl))
            for half in range(2):
                pb = nextbank()
                pk = ("ps", pb)
                for q in range(4):
                    c = half * 4 + q
                    T.op("pe", lambda: nc.tensor.transpose(out=ps[pb][:, q * 128:(q + 1) * 128],
                                                           in_=xin(sl)[:, c * 128:(c + 1) * 128],
                                                           identity=identf[:]),
                         rd=xink(sl) + ["identf"], wr=[pk], inc=(q == 3))
                T.op("dve", lambda: nc.vector.tensor_copy(
                    out=hT[:, half * 4:(half + 1) * 4, t * 128:(t + 1) * 128],
                    in_=ps[pb][:].rearrange("p (q n) -> p q n", q=4)), rd=[pk],
                    wr=[("hT", c, 0) for c in range(half * 4, half * 4 + 4)])
        rms_stats(lambda c: hT[:, c, 0:N_MEM], lambda c: ("hT", c, 0), N_MEM)
        for c in range(8):
            T.op("dve", lambda: nc.vector.scalar_tensor_tensor(
                out=memnT[:, c, :], in0=hT[:, c, 0:N_MEM], scalar=gains[:, G_MEM + c:G_MEM + c + 1],
                in1=rstd[:, 0:N_MEM], op0=ALU.mult, op1=ALU.mult),
                rd=[("hT", c, 0), "gains", "rstd"], wr=[("memnT", c)])

    def mem_kv(l):
        def hk(j, pb):
            T.op("act", lambda: nc.scalar.copy(out=memkT[:, j, :], in_=ps[pb][:, 0:N_MEM]),
                 rd=[("ps", pb)], wr=[("memkT", j)])
            T.op("act", lambda: nc.scalar.activation(out=sq[:, j % 2, 0:N_MEM], in_=ps[pb][:, 0:N_MEM], func=AF.Square),
                 rd=[("ps", pb)], wr=[("sq", j % 2)])
            T.op("pe", lambda: nc.tensor.matmul(ps[6][:, 0:N_MEM], lhsT=onesb, rhs=sq[:, j % 2, 0:N_MEM],
                                                start=True, stop=True),
                 rd=["cb", ("sq", j % 2)], wr=[("ps", 6)])
            T.op("dve", lambda: nc.vector.reduce_max(out=negkmax[:, j:j + 1], in_=ps[6][:, 0:N_MEM],
                                                     axis=mybir.AxisListType.X),
                 rd=[("ps", 6)], wr=[("negkmax", j)])
            T.op("act", lambda: nc.scalar.activation(out=negkmax[:, j:j + 1], in_=negkmax[:, j:j + 1], func=AF.Sqrt),
                 rd=[("negkmax", j)], wr=[("negkmax", j)])
            T.op("dve", lambda: nc.vector.tensor_scalar(out=negkmax[:, j:j + 1], in0=negkmax[:, j:j + 1],
                                                        scalar1=-1.0, scalar2=None, op0=ALU.mult),
                 rd=[("negkmax", j)], wr=[("negkmax", j)])
        proj_fm("w_mem_kv", l, 0, 4, memnT, "memnT", N_MEM, hk)
        for half in range(2):
            def hv(tt, pb):
                T.op("act", lambda: nc.scalar.copy(out=memv[:, tt, half * 256:(half + 1) * 256], in_=ps[pb][:, 0:256]),
                     rd=[("ps", pb)], wr=[("memv", tt, half)])
            proj_tm("w_mem_kv", l, 512 + half * 256, 256, memnT, "memnT", 2, hv)

    def qmem_handler(j, pb):
        T.op("act", lambda: nc.scalar.activation(out=qmT[:, j, :], in_=ps[pb][:], func=AF.Copy, scale=128.0 ** -0.5),
             rd=[("ps", pb)], wr=[("qmT", j)])

    def mem_attn(b):
        for hm in range(4):
            T.op("act", lambda: nc.scalar.activation(out=sq[:, hm % 2, :], in_=qmT[:, hm, :], func=AF.Square),
                 rd=[("qmT", hm)], wr=[("sq", hm % 2)])
            T.op("pe", lambda: nc.tensor.matmul(ps[6][:], lhsT=onesb, rhs=sq[:, hm % 2, :], start=True, stop=True),
                 rd=["cb", ("sq", hm % 2)], wr=[("ps", 6)])
            T.op("act", lambda: nc.scalar.activation(out=cq[0:1, :], in_=ps[6][0:1, :], func=AF.Sqrt),
                 rd=[("ps", 6)], wr=["cq"])
            T.op("dve", lambda: nc.vector.tensor_scalar(out=negc[0:1, :], in0=cq[0:1, :],
                                                        scalar1=negkmax[0:1, hm:hm + 1], scalar2=None, op0=ALU.mult),
                 rd=["cq", ("negkmax", hm)], wr=["negc"])
            for mt in range(2):
                pb = nextbank()
                T.op("pe", lambda: nc.tensor.matmul(ps[pb][:], lhsT=memkT[:, hm, mt * 128:(mt + 1) * 128],
                                                    rhs=qmT[:, hm, :], start=True, stop=False),
                     rd=[("memkT", hm), ("qmT", hm)], wr=[("ps", pb)], inc=False)
                T.op("pe", lambda: nc.tensor.matmul(ps[pb][:], lhsT=onesb[0:1, :], rhs=negc[0:1, :],
                                                    start=False, stop=True),
                     rd=["cb", "negc"], wr=[("ps", pb)])
                T.op("act", lambda: nc.scalar.activation(out=pT[:, mt, :], in_=ps[pb][:], func=AF.Exp),
                     rd=[("ps", pb)], wr=[("pT", mt)])
            po, pd = nextbank(), 6
            for mt in range(2):
                T.op("pe", lambda: nc.tensor.matmul(ps[po][:], lhsT=memv[:, mt, hm * 128:(hm + 1) * 128],
                                                    rhs=pT[:, mt, :], start=(mt == 0), stop=(mt == 1)),
                     rd=[("memv", mt, hm // 2), ("pT", mt)], wr=[("ps", po)], inc=(mt == 1))
            for mt in range(2):
                T.op("pe", lambda: nc.tensor.matmul(ps[pd][:], lhsT=onesb, rhs=pT[:, mt, :],
                                                    start=(mt == 0), stop=(mt == 1)),
                     rd=["cb", ("pT", mt)], wr=[("ps", pd)], inc=(mt == 1))
            T.op("dve", lambda: nc.vector.reciprocal(out=rden[:], in_=ps[pd][:]), rd=[("ps", pd)], wr=["rden"])
            T.op("dve", lambda: nc.vector.tensor_tensor(out=catT[:, 8 + hm, :], in0=ps[po][:], in1=rden[:],
                                                        op=ALU.mult),
                 rd=[("ps", po), "rden"], wr=[("catT", 8 + hm)])

    sbst = {}
    kT_d = P.dram("sb_kT_scr", [8, 128, S], BF16, "Internal")
    v_d = P.dram("sb_v_scr", [8, S, 128], BF16, "Internal")
    c_kv = T.new_counter("kv")

    def sb_alloc():
        R2, R2f = LS["R2"], LS["R2f"]
        def v3(lo):
            return R2[:, lo:lo + 1536].rearrange("p (a n) -> p a n", a=3)
        sbst["qT"] = R2[:, 0:4096].rearrange("p (a n) -> p a n", a=8)
        sbst["Lp"] = v3(4096)
        sbst["attB"] = v3(5632)
        sbst["Pm"] = v3(7168)
        sbst["att"] = v3(8704)
        sbst["e"] = v3(10240)
        sbst["kc"] = R1[:, 0:4096].rearrange("p (a n) -> p a n", a=2)
        sbst["vc"] = R1[:, 4096:8192].rearrange("p (a k n) -> p a k n", a=2, k=16)
        sbst["kst"] = R1[:, 8192:9216].rearrange("p (a n) -> p a n", a=2)
        sbst["vst"] = R1[:, 9216:9728].rearrange("p (a n) -> p a n", a=2)

    def sb_setup():
        T.dma("sp", c_io, cb[:, CB_MASK01:CB_MASK01 + 4 * TB], cb_d[:, CB_MASK01:CB_MASK01 + 4 * TB], wr=["cb"])

    def sb_inproj(l, b):
        qT, kst, vst = sbst["qT"], sbst["kst"], sbst["vst"]
        mixer_keys[:] = [("sb_kc", 0), ("sb_kc", 1), ("sb_vc", 0), ("sb_vc", 1), ("sb_kst", 0), ("sb_kst", 1), ("sb_vst", 0), ("sb_vst", 1)]
        T.alias(mixer_keys, [("aT", j) for j in range(22)])

        def hq(j, pb):
            T.op("act", lambda: nc.scalar.activation(out=qT[:, j, :], in_=ps[pb][:], func=AF.Copy, scale=0.125),
                 rd=[("ps", pb)], wr=[("sb_qT", j)])

        def hk(j, pb):
            k2 = j % 2
            T.op("dve", lambda: nc.vector.tensor_copy(out=kst[:, k2, :], in_=ps[pb][:]),
                 rd=[("ps", pb)], wr=[("sb_kst", k2)])
            T.dma("sp", c_kv, kT_d[j, :, b * TB:(b + 1) * TB], kst[:, k2, :], rd=[("sb_kst", k2)],
                  wr=[("kT_d", j, b)])
        proj_fm("sb_w_in", 0, 0, 8, uT, "uT", TB, hq)
        proj_fm("sb_w_in", 0, 1024, 8, uT, "uT", TB, hk)
        for q4 in range(4):
            def hv(tt, pb):
                k2 = tt % 2
                T.op("act", lambda: nc.scalar.copy(out=vst[:, k2, :], in_=ps[pb][:, 0:256]),
                     rd=[("ps", pb)], wr=[("sb_vst", k2)])
                for pp in range(2):
                    cpair = 2 * q4 + pp
                    t0 = b * TB + tt * 128
                    T.dma("sp", c_kv, v_d[cpair, t0:t0 + 128, :], vst[:, k2, pp * 128:(pp + 1) * 128],
                          rd=[("sb_vst", k2)], wr=[("v_d", cpair, b)])
            proj_tm("sb_w_in", 0, 2048 + q4 * 256, 256, uT, "uT", 4, hv)
        proj_fm("sb_w_in", 0, 3072, 4, uT, "uT", TB, qmem_handler)

    def sb_load_pair(c, b):
        sl = c % 2
        n = (b + 1) * TB
        T.dma("sp", c_kv, sbst["kc"][:, sl, 0:n], kT_d[c, :, 0:n], rd=[("kT_d", c, bb) for bb in range(b + 1)],
              wr=[("sb_kc", sl)])
        T.dma("sp", c_kv, sbst["vc"][:, sl, 0:4 * (b + 1), :],
              v_d[c, 0:n, :].rearrange("(k p) n -> p k n", p=128),
              rd=[("v_d", c, bb) for bb in range(b + 1)], wr=[("sb_vc", sl)])

    def sb_attn(b):
        qT = sbst["qT"]
        e, Lp, attB, Pm, att = sbst["e"], sbst["Lp"], sbst["attB"], sbst["Pm"], sbst["att"]
        sb_load_pair(0, b)
        nk = 4 * b + 4
        rot = {"i": 0}

        def sbbank():
            rot["i"] = (rot["i"] + 1) % 6
            return (0, 1, 2, 3, 4, 7)[rot["i"]]
        for c in range(8):
            if c + 1 < 8:
                sb_load_pair(c + 1, b)
            sl = c % 2
            kc, vc = sbst["kc"], sbst["vc"]
            pO = 5
            for hh in range(2):
                po = hh * 64
                qview = qT[po:po + 64, c, :]
                order = list(range(nk - 1, -1, -1))
                for g0 in range(0, nk, 3):
                    grp = [(g0 + u, order[g0 + u]) for u in range(3) if g0 + u < nk]
                    banks_a, banks_b = {}, {}
                    for u, (idx, kb) in enumerate(grp):
                        kview = kc[po:po + 64, sl, kb * 128:(kb + 1) * 128]
                        pa = sbbank()
                        banks_a[u] = pa
                        T.op("pe", lambda: nc.tensor.matmul(ps[pa][:], lhsT=kview, rhs=qview, start=True, stop=True),
                             rd=[("sb_kc", sl), ("sb_qT", c)], wr=[("ps", pa)])
                    for u, (idx, kb) in enumerate(grp):
                        pa = banks_a[u]
                        T.op("act", lambda: nc.scalar.activation(out=e[:, u, :], in_=ps[pa][:], func=AF.Exp),
                             rd=[("ps", pa)], wr=[("sb_e", u)])
                    for u, (idx, kb) in enumerate(grp):
                        T.op("act", lambda: nc.scalar.activation(out=Lp[:, u, :], in_=e[:, u, :], func=AF.Ln,
                                                                 bias=oneb[:]),
                             rd=[("sb_e", u), "oneb"], wr=[("sb_Lp", u)])
                        i = kb - 4 * b
                        if i >= 0:
                            m01 = cb[:, CB_MASK01 + i * TB:CB_MASK01 + (i + 1) * TB]
                            T.op("pool", lambda: nc.gpsimd.tensor_tensor(out=Lp[:, u, :], in0=Lp[:, u, :], in1=m01,
                                                                         op=ALU.mult),
                                 rd=[("sb_Lp", u), "cb"], wr=[("sb_Lp", u)])
                    for u, (idx, kb) in enumerate(grp):
                        kview = kc[po:po + 64, sl, kb * 128:(kb + 1) * 128]
                        pbk = sbbank()
                        banks_b[u] = pbk
                        T.op("pe", lambda: nc.tensor.matmul(ps[pbk][:], lhsT=kview, rhs=qview, start=True, stop=False),
                             rd=[("sb_kc", sl), ("sb_qT", c)], wr=[("ps", pbk)], inc=False)
                        T.op("pe", lambda: nc.tensor.matmul(ps[pbk][:], lhsT=negMincl, rhs=Lp[:, u, :],
                                                            start=False, stop=True),
                             rd=["cb", ("sb_Lp", u)], wr=[("ps", pbk)])
                    for u, (idx, kb) in enumerate(grp):
                        pbk = banks_b[u]
                        T.op("act", lambda: nc.scalar.activation(out=attB[:, u, :], in_=ps[pbk][:], func=AF.Exp),
                             rd=[("ps", pbk)], wr=[("sb_attB", u)])
                        i = kb - 4 * b
                        if i >= 0:
                            m01 = cb[:, CB_MASK01 + i * TB:CB_MASK01 + (i + 1) * TB]
                            T.op("pool", lambda: nc.gpsimd.tensor_tensor(out=attB[:, u, :], in0=attB[:, u, :], in1=m01,
                                                                         op=ALU.mult),
                                 rd=[("sb_attB", u), "cb"], wr=[("sb_attB", u)])
                    for u, (idx, kb) in enumerate(grp):
                        if idx > 0:
                            T.op("act", lambda: nc.scalar.activation(out=Pm[:, u, :], in_=ps[6][:], func=AF.Exp),
                                 rd=[("ps", 6)], wr=[("sb_Pm", u)])
                            T.op("dve", lambda: nc.vector.tensor_tensor(out=att[:, u, :], in0=attB[:, u, :],
                                                                        in1=Pm[:, u, :], op=ALU.mult),
                                 rd=[("sb_attB", u), ("sb_Pm", u)], wr=[("sb_att", u)])
                            a_ap, a_key = att[:, u, :], ("sb_att", u)
                        else:
                            a_ap, a_key = attB[:, u, :], ("sb_attB", u)
                        if idx < nk - 1:
                            T.op("pe", lambda: nc.tensor.matmul(ps[6][:], lhsT=negones, rhs=Lp[:, u, :],
                                                                start=(idx == 0), stop=True),
                                 rd=["cb", ("sb_Lp", u)], wr=[("ps", 6)])
                        T.op("pe", lambda: nc.tensor.matmul(ps[pO][po:po + 64, :], lhsT=vc[:, sl, kb, po:po + 64],
                                                            rhs=a_ap, start=(idx == 0), stop=(idx == nk - 1),
                                                            tile_position=(0, po)),
                             rd=[("sb_vc", sl), a_key], wr=[("ps", pO, hh)])
            T.op("dve", lambda: nc.vector.tensor_copy(out=catT[:, c, :], in_=ps[pO][:]),
                 rd=[("ps", pO, 0), ("ps", pO, 1)], wr=[("catT", c)])

    mlst = {}
    sel_d = P.dram("selc", [16, 8 * 128], F32, "ExternalInput")
    mlb_d = P.dram("mlbias", [16, 1], F32, "ExternalInput")
    mlg_d = P.dram("mlgain", [128, 1], F32, "ExternalInput")
    m01i_d = P.dram("mask01incl", [128, 4 * TB], BF16, "ExternalInput")

    def ml_alloc():
        R2, R2f = LS["R2"], LS["R2f"]
        mlst["qT"] = R2[:, 0:2048].rearrange("p (a n) -> p a n", a=4)
        mlst["sigo"] = R2[:, 2048:2560]
        mlst["Dm"] = R2[:, 2560:3584].rearrange("p (a n) -> p a n", a=2)
        mlst["Wt"] = R2[:, 3584:4608].rearrange("p (a n) -> p a n", a=2)
        mlst["hsb"] = R2f[:, 2304:2816]
        mlst["gsb"] = R2f[0:16, 2816:3328]
        mlst["lp"] = R2f[0:16, 3328:3840]
        mlst["cum"] = R2f[0:16, 3840:4352]
        mlst["NF"] = R2f[0:16, 4352:4864]
        mlst["sel"] = R2f[0:16, 4864:5888]
        mlst["Acol"] = R2f[:, 5888:6016].rearrange("p (a n) -> p a n", a=16)
        mlst["tT"] = R2f[:, 6016:6048].rearrange("p (a n) -> p a n", a=2)
        mlst["carry"] = R2f[0:16, 6048:6049]
        mlst["bias"] = R2f[0:16, 6049:6050]
        mlst["gain"] = R2f[:, 6050:6051]
        mlst["kc"] = R1[:, 0:4096].rearrange("p (a n) -> p a n", a=2)
        mlst["vc"] = R1[:, 4096:8192].rearrange("p (a k n) -> p a k n", a=2, k=16)
        mlst["kst"] = R1[:, 8192:9216].rearrange("p (a n) -> p a n", a=2)
        mlst["vst"] = R1[:, 9216:9728].rearrange("p (a n) -> p a n", a=2)

    def ml_setup():
        T.dma("sp", c_io, mlst["sel"][:], sel_d, wr=["ml_sel"])
        T.dma("sp", c_io, mlst["bias"][:], mlb_d, wr=["ml_bias"])
        T.dma("sp", c_io, mlst["gain"][:], mlg_d, wr=["ml_gain"])
        T.dma("sp", c_io, cb[:, CB_MASK01:CB_MASK01 + 4 * TB], m01i_d, wr=["cb"])

    def ml_inproj(l, b):
        qT, kst, vst = mlst["qT"], mlst["kst"], mlst["vst"]
        gsb, lp, cum, carry, NF, Acol, tT = (mlst[k] for k in ("gsb", "lp", "cum", "carry", "NF", "Acol", "tT"))
        mixer_keys[:] = [("sb_kc", 0), ("sb_kc", 1), ("sb_vc", 0), ("sb_vc", 1), ("sb_kst", 0), ("sb_kst", 1),
                         ("sb_vst", 0), ("sb_vst", 1)]
        T.alias(mixer_keys, [("aT", j) for j in range(22)])

        def hq(j, pb):
            T.op("act", lambda: nc.scalar.copy(out=qT[:, j, :], in_=ps[pb][:]), rd=[("ps", pb)], wr=[("ml_qT", j)])

        def hk(j, pb):
            k2 = j % 2
            T.op("dve", lambda: nc.vector.tensor_scalar(out=kst[:, k2, :], in0=ps[pb][:], scalar1=0.125, scalar2=None,
                                                        op0=ALU.mult),
                 rd=[("ps", pb)], wr=[("sb_kst", k2)])
            T.dma("sp", c_kv, kT_d[j, :, b * TB:(b + 1) * TB], kst[:, k2, :], rd=[("sb_kst", k2)],
                  wr=[("kT_d", j, b)])
        proj_fm("ml_w_in", 0, 0, 4, uT, "uT", TB, hq)
        proj_fm("ml_w_in", 0, 512, 4, uT, "uT", TB, hk)
        for q4 in range(4):
            def hv(tt, pb):
                k2 = tt % 2
                T.op("act", lambda: nc.scalar.copy(out=vst[:, k2, :], in_=ps[pb][:, 0:256]),
                     rd=[("ps", pb)], wr=[("sb_vst", k2)])
                for pp in range(2):
                    hd = 2 * q4 + pp
                    t0 = b * TB + tt * 128
                    T.dma("sp", c_kv, v_d[hd, t0:t0 + 128, :], vst[:, k2, pp * 128:(pp + 1) * 128],
                          rd=[("sb_vst", k2)], wr=[("v_d", hd, b)])
            proj_tm("ml_w_in", 0, 1024 + q4 * 256, 256, uT, "uT", 4, hv)
        slot = w_get([("ml_w_in", 0, 0, 8, 3072, 16)])
        wv = wview(slot, 0, 8, 16)
        pg = nextbank()
        for kc in range(8):
            T.op("pe", lambda: nc.tensor.matmul(ps[pg][0:16, :], lhsT=wv[:, kc, :], rhs=uT[:, kc, :],
                                                start=(kc == 0), stop=(kc == 7)),
                 rd=[WK(slot), ("uT", kc)], wr=[("ps", pg)], inc=(kc == 7))
        T.op("act", lambda: nc.scalar.activation(out=gsb[:], in_=ps[pg][0:16, :], func=AF.Identity,
                                                 bias=mlst["bias"][:]),
             rd=[("ps", pg), "ml_bias"], wr=["ml_gsb"])
        T.op("act", lambda: nc.scalar.activation(out=lp[:], in_=gsb[:], func=AF.Exp, scale=-1.0),
             rd=["ml_gsb"], wr=["ml_lp"])
        T.op("act", lambda: nc.scalar.activation(out=lp[:], in_=lp[:], func=AF.Ln, bias=oneb[0:16, :]),
             rd=["ml_lp", "oneb"], wr=["ml_lp"])
        if b == 0:
            T.op("dve", lambda: nc.vector.memset(carry[:], 0.0), wr=["ml_carry"])
        T.op("dve", lambda: nc.vector.tensor_tensor_scan(out=cum[:], data0=lp[:], data1=lp[:], initial=carry[:, 0:1],
                                                         op0=ALU.add, op1=ALU.max),
             rd=["ml_lp", "ml_carry"], wr=["ml_cum"])
        T.op("dve", lambda: nc.vector.tensor_copy(out=carry[:], in_=cum[:, TB - 1:TB]), rd=["ml_cum"], wr=["ml_carry"])
        T.op("dve", lambda: nc.vector.tensor_scalar(out=NF[:], in0=cum[:], scalar1=-1.0, scalar2=None, op0=ALU.mult),
             rd=["ml_cum"], wr=["ml_NF"])
        for tt in range(4):
            pt = nextbank()
            T.op("pe", lambda: nc.tensor.transpose(out=ps[pt][:, 0:16], in_=gsb[:, tt * 128:(tt + 1) * 128],
                                                   identity=identf[0:16, 0:16]),
                 rd=["ml_gsb", "identf"], wr=[("ps", pt)], inc=False)
            T.op("pe", lambda: nc.tensor.transpose(out=ps[pt][:, 16:32], in_=cum[:, tt * 128:(tt + 1) * 128],
                                                   identity=identf[0:16, 0:16]),
                 rd=["ml_cum", "identf"], wr=[("ps", pt)])
            T.op("act", lambda: nc.scalar.copy(out=tT[:, tt % 2, :], in_=ps[pt][:, 16:32]),
                 rd=[("ps", pt)], wr=[("ml_tT", tt % 2)])
            T.op("dve", lambda: nc.vector.tensor_tensor(out=Acol[:, b * 4 + tt, :], in0=ps[pt][:, 0:8],
                                                        in1=tT[:, tt % 2, 8:16], op=ALU.add),
                 rd=[("ps", pt), ("ml_tT", tt % 2)], wr=[("ml_Acol", b * 4 + tt)])
        proj_fm("ml_w_in", 0, 3088, 4, uT, "uT", TB, qmem_handler)

    def ml_attn(l, b):
        qT, Dm, Wt, hsb, Acol, NF, sel, sigo = (mlst[k] for k in ("qT", "Dm", "Wt", "hsb", "Acol", "NF", "sel", "sigo"))
        kc, vc = mlst["kc"], mlst["vc"]
        it = 0
        nk = 4 * b + 4

        def load(h):
            sl = h % 2
            n = (b + 1) * TB
            if h % 2 == 0:
                T.dma("sp", c_kv, kc[:, (h // 2) % 2, 0:n], kT_d[h // 2, :, 0:n],
                      rd=[("kT_d", h // 2, bb) for bb in range(b + 1)], wr=[("sb_kc", (h // 2) % 2)])
            T.dma("sp", c_kv, vc[:, sl, 0:4 * (b + 1), :], v_d[h, 0:n, :].rearrange("(k p) n -> p k n", p=128),
                  rd=[("v_d", h, bb) for bb in range(b + 1)], wr=[("sb_vc", sl)])
        load(0)
        for h in range(8):
            if h + 1 < 8:
                load(h + 1)
            c, po, sl, ksl = h // 2, (h % 2) * 64, h % 2, (h // 2) % 2
            T.op("pe", lambda: nc.tensor.matmul(ps[6][:], lhsT=sel[:, h * 128:(h + 1) * 128], rhs=NF[:],
                                                start=True, stop=True),
                 rd=["ml_sel", "ml_NF"], wr=[("ps", 6)])
            pN, pD = 5, 7
            order = list(range(nk - 1, -1, -1))
            for g0 in range(0, nk, 2):
                grp = [(g0 + u, order[g0 + u]) for u in range(2) if g0 + u < nk]
                pas = {}
                for u, (idx, kb) in enumerate(grp):
                    pa = nextbank()
                    pas[u] = pa
                    T.op("pe", lambda: nc.tensor.matmul(ps[pa][:], lhsT=kc[po:po + 64, ksl, kb * 128:(kb + 1) * 128],
                                                        rhs=qT[po:po + 64, c, :], start=True, stop=True),
                         rd=[("sb_kc", ksl), ("ml_qT", c)], wr=[("ps", pa)])
                for u, (idx, kb) in enumerate(grp):
                    T.op("act", lambda: nc.scalar.activation(out=Dm[:, u, :], in_=ps[6][:], func=AF.Exp,
                                                             bias=Acol[:, kb, h:h + 1]),
                         rd=[("ps", 6), ("ml_Acol", kb)], wr=[("ml_Dm", u)])
                for u, (idx, kb) in enumerate(grp):
                    pa = pas[u]
                    i = kb - 4 * b
                    T.op("dve", lambda: nc.vector.tensor_tensor(out=Wt[:, u, :], in0=ps[pa][:], in1=Dm[:, u, :],
                                                                op=ALU.mult),
                         rd=[("ps", pa), ("ml_Dm", u)], wr=[("ml_Wt", u)])
                    if i >= 0:
                        m01 = cb[:, CB_MASK01 + i * TB:CB_MASK01 + (i + 1) * TB]
                        T.op("pool", lambda: nc.gpsimd.tensor_tensor(out=Wt[:, u, :], in0=Wt[:, u, :], in1=m01,
                                                                     op=ALU.mult),
                             rd=[("ml_Wt", u), "cb"], wr=[("ml_Wt", u)])
                for u, (idx, kb) in enumerate(grp):
                    T.op("pe", lambda: nc.tensor.matmul(ps[pN][:], lhsT=vc[:, sl, kb, :], rhs=Wt[:, u, :],
                                                        start=(idx == 0), stop=(idx == nk - 1)),
                         rd=[("sb_vc", sl), ("ml_Wt", u)], wr=[("ps", pN)], inc=False)
                    T.op("pe", lambda: nc.tensor.matmul(ps[pD][:], lhsT=onesb, rhs=Wt[:, u, :],
                                                        start=(idx == 0), stop=(idx == nk - 1)),
                         rd=["cb", ("ml_Wt", u)], wr=[("ps", pD)])
            T.op("act", lambda: nc.scalar.activation(out=rden[:], in_=ps[pD][:], func=AF.Abs),
                 rd=[("ps", pD)], wr=["rden"])
            T.op("dve", lambda: nc.vector.tensor_scalar(out=rden[:], in0=rden[:], scalar1=1.0, scalar2=None,
                                                        op0=ALU.max),
                 rd=["rden"], wr=["rden"])
            T.op("dve", lambda: nc.vector.reciprocal(out=rden[:], in_=rden[:]), rd=["rden"], wr=["rden"])
            T.op("dve", lambda: nc.vector.tensor_tensor(out=hsb[:], in0=ps[pN][:], in1=rden[:], op=ALU.mult),
                 rd=[("ps", pN), "rden"], wr=["ml_hsb"])
            T.op("act", lambda: nc.scalar.activation(out=sq[:, 0, :], in_=hsb[:], func=AF.Square),
                 rd=["ml_hsb"], wr=[("sq", 0)])
            T.op("pe", lambda: nc.tensor.matmul(ps[pD][:], lhsT=onesb, rhs=sq[:, 0, :], start=True, stop=True),
                 rd=["cb", ("sq", 0)], wr=[("ps", pD)])
            T.op("act", lambda: nc.scalar.activation(out=rden[:], in_=ps[pD][:], func=AF.Sqrt, scale=1.0 / 128,
                                                     bias=epsb[:]),
                 rd=[("ps", pD), "epsb"], wr=["rden"])
            T.op("dve", lambda: nc.vector.reciprocal(out=rden[:], in_=rden[:]), rd=["rden"], wr=["rden"])
            T.op("dve", lambda: nc.vector.scalar_tensor_tensor(out=hsb[:], in0=hsb[:], scalar=mlst["gain"][:, 0:1],
                                                               in1=rden[:], op0=ALU.mult, op1=ALU.mult),
                 rd=["ml_hsb", "ml_gain", "rden"], wr=["ml_hsb"])
            slot = w_get([("ml_w_in", 0, 0, 8, 2048 + h * 128, 128)])
            wv = wview(slot, 0, 8, 128)
            pg = nextbank()
            for kcc in range(8):
                T.op("pe", lambda: nc.tensor.matmul(ps[pg][:], lhsT=wv[:, kcc, :], rhs=uT[:, kcc, :],
                                                    start=(kcc == 0), stop=(kcc == 7)),
                     rd=[WK(slot), ("uT", kcc)], wr=[("ps", pg)], inc=(kcc == 7))
            T.op("act", lambda: nc.scalar.activation(out=sigo[:], in_=ps[pg][:], func=AF.Sigmoid),
                 rd=[("ps", pg)], wr=["ml_sigo"])
            T.op("dve", lambda: nc.vector.tensor_tensor(out=catT[:, h, :], in0=hsb[:], in1=sigo[:], op=ALU.mult),
                 rd=["ml_hsb", "ml_sigo"], wr=[("catT", h)])

    gd = {}
    gcw_d = P.dram("gdn_cw", [2, 128, 96], F32, "ExternalInput")
    gdtb_d = P.dram("gdn_dtb", [2, 8, 1], F32, "ExternalInput")
    galog_d = P.dram("gdn_alog", [2, 8, 1], F32, "ExternalInput")
    ggain_d = P.dram("gdn_gain", [2, 128, 1], F32, "ExternalInput")
    sel8_d = P.dram("sel8", [8, 8 * 128], F32, "ExternalInput")
    gmask_d = P.dram("gdn_masks", [128, 3 * TB], BF16, "ExternalInput")
    R3 = ysb_raw.bitcast(BF16)
    R1f = R1.bitcast(F32)

    def gdn_alloc():
        gF, gR2 = LS["gF"], LS["gR2"]
        gFr = gF.bitcast(F32R)
        gd["A"] = gF[:, 0:1024].rearrange("p (s n) -> p s n", s=2)
        gd["AT"] = gF[:, 1024:2048].rearrange("p (s n) -> p s n", s=2)
        gd["X"] = gF[:, 2048:2560]
        gd["Ru"] = gF[:, 2560:3072]
        gd["Rw"] = gF[:, 3072:3584]
        gd["Ar"] = gFr[:, 0:1024].rearrange("p (s n) -> p s n", s=2)
        gd["ATr"] = gFr[:, 1024:2048].rearrange("p (s n) -> p s n", s=2)
        gd["Xr"] = gFr[:, 2048:2560]
        gd["Rur"] = gFr[:, 2560:3072]
        gd["Rwr"] = gFr[:, 3072:3584]
        o1 = 0

        def f1(n, parts=128):
            nonlocal o1
            v = R1f[0:parts, o1:o1 + n]
            o1 += n
            return v
        gd["u"] = f1(512)
        gd["knf"] = f1(512)
        gd["vsf"] = f1(512)
        gd["ra"] = f1(512, 8)
        gd["rb"] = f1(512, 8)
        gd["G"] = f1(512, 8)
        gd["NG"] = f1(512, 8)
        gd["GB"] = f1(512)
        gd["xc"] = f1(516)
        gd["xc2"] = f1(516)
        assert o1 <= 5632
        o = 0

        def f2(n, parts=128):
            nonlocal o
            v = gR2[0:parts, o:o + n]
            o += n
            return v
        gd["sel8"] = f2(1024, 8)
        gd["acc"] = f2(512)
        gd["S"] = f2(1024).rearrange("p (h n) -> p h n", h=8)
        gd["gcol"] = f2(64).rearrange("p (t n) -> p t n", t=4)
        gd["ngcol"] = f2(64).rearrange("p (t n) -> p t n", t=4)
        gd["bgc"] = f2(32).rearrange("p (t n) -> p t n", t=4)
        gd["egc"] = f2(32).rearrange("p (t n) -> p t n", t=4)
        gd["kgc"] = f2(4)
        gd["glc"] = f2(4)
        gd["cw"] = f2(96).rearrange("p (c k) -> p c k", k=4)
        gd["halo"] = f2(72).rearrange("p (c k) -> p c k", k=3)
        gd["gain"] = f2(1)
        gd["dtb"] = f2(1, 8)
        gd["negA"] = f2(1, 8)
        gd["rr"] = rden[:, :]
        assert o <= 2944, o
        o3 = 0

        def f3(n):
            nonlocal o3
            v = R3[:, o3:o3 + n]
            o3 += n
            return v
        for nm in ("qnT", "qgT", "knT", "kbT", "zs", "EG", "Bb", "E1", "E2", "E3", "kg", "wT", "pTg"):
            gd[nm] = f3(512)
        gd["vnb"] = f3(256).rearrange("p (s n) -> p s n", s=2)
        gd["Sb"] = f3(128)
        assert o3 <= 8192

    G_R1KEYS = ["g_u", "g_knf", "g_vsf", "g_ra", "g_rb", "g_G", "g_NG", "g_GB", "g_xc", "g_xc2"]

    def gdn_setup(l):
        jg = l // 3
        T.dma("sp", c_io, gd["cw"].rearrange("p c k -> p (c k)"), gcw_d[jg], wr=["g_cw"])
        T.dma("sp", c_io, gd["dtb"], gdtb_d[jg], wr=["g_dtb"])
        T.dma("sp", c_io, gd["negA"], galog_d[jg], wr=["g_negA"])
        T.dma("sp", c_io, gd["gain"], ggain_d[jg], wr=["g_gain"])
        T.dma("sp", c_io, gd["sel8"], sel8_d, wr=["g_sel8"])
        T.dma("sp", c_io, cb[:, CB_MASK01:CB_MASK01 + 3 * TB], gmask_d, wr=["cb"])
        T.op("act", lambda: nc.scalar.activation(out=gd["negA"], in_=gd["negA"], func=AF.Exp), rd=["g_negA"], wr=["g_negA"])
        T.op("dve", lambda: nc.vector.tensor_scalar(out=gd["negA"], in0=gd["negA"], scalar1=-1.0, scalar2=None,
                                                    op0=ALU.mult), rd=["g_negA"], wr=["g_negA"])
        T.op("pool", lambda: nc.gpsimd.memset(gd["S"].rearrange("p h n -> p (h n)"), 0.0), wr=[("g_S", h) for h in range(8)])

    def gdn_block(l, b):
        jg = l // 3
        g = gd
        M1 = cb[:, CB_MASK01:CB_MASK01 + TB]
        M2 = cb[:, CB_MASK01 + TB:CB_MASK01 + 2 * TB]
        M3 = cb[:, CB_MASK01 + 2 * TB:CB_MASK01 + 3 * TB]
        mixer_keys[:] = G_R1KEYS
        T.alias(mixer_keys, [("aT", j) for j in range(22)])
        ysb_overlay[:] = ["g_qnT", "g_qgT", "g_knT", "g_kbT", "g_zs", "g_EG", "g_Bb", "g_E1", "g_E2", "g_E3", "g_kg", "g_wT", "g_pTg", ("g_vnb", 0), ("g_vnb", 1), "g_Sb"]
        T.alias(ysb_overlay, [("ysb", c) for c in range(8)])
        slot = w_get([("gdn_w_in", jg, 0, 8, 4096, 16)])
        wv = wview(slot, 0, 8, 16)
        pa, pb_ = nextbank(), nextbank()
        for (pbank, c0) in ((pa, 0), (pb_, 8)):
            for kc in range(8):
                T.op("pe", lambda: nc.tensor.matmul(ps[pbank][0:8, :], lhsT=wv[:, kc, c0:c0 + 8], rhs=uT[:, kc, :],
                                                    start=(kc == 0), stop=(kc == 7)),
                     rd=[WK(slot), ("uT", kc)], wr=[("ps", pbank)], inc=(kc == 7))
        T.op("act", lambda: nc.scalar.activation(out=g["ra"], in_=ps[pa][0:8, :], func=AF.Exp, bias=g["dtb"]),
             rd=[("ps", pa), "g_dtb"], wr=["g_ra"])
        T.op("act", lambda: nc.scalar.activation(out=g["ra"], in_=g["ra"], func=AF.Ln, bias=oneb[0:8, :]),
             rd=["g_ra", "oneb"], wr=["g_ra"])
        T.op("dve", lambda: nc.vector.tensor_scalar(out=g["ra"], in0=g["ra"], scalar1=g["negA"], scalar2=None,
                                                    op0=ALU.mult), rd=["g_ra", "g_negA"], wr=["g_ra"])
        T.op("act", lambda: nc.scalar.activation(out=g["rb"], in_=ps[pb_][0:8, :], func=AF.Sigmoid),
             rd=[("ps", pb_)], wr=["g_rb"])
        for n in range(4):
            cs = slice(n * 128, (n + 1) * 128)
            T.op("dve", lambda: nc.vector.tensor_tensor_scan(out=g["G"][:, cs], data0=g["ra"][:, cs], data1=g["ra"][:, cs],
                                                             initial=0.0, op0=ALU.add, op1=ALU.min),
                 rd=["g_ra"], wr=["g_G"])
        T.op("dve", lambda: nc.vector.tensor_scalar(out=g["NG"], in0=g["G"], scalar1=-1.0, scalar2=None, op0=ALU.mult),
             rd=["g_G"], wr=["g_NG"])
        for tt in range(4):
            cs = slice(tt * 128, (tt + 1) * 128)
            pt = nextbank()
            T.op("pe", lambda: nc.tensor.transpose(out=ps[pt][:, 0:8], in_=g["G"][:, cs], identity=identf[0:8, 0:8]),
                 rd=["g_G", "identf"], wr=[("ps", pt)], inc=False)
            T.op("pe", lambda: nc.tensor.transpose(out=ps[pt][:, 8:16], in_=g["rb"][:, cs], identity=identf[0:8, 0:8]),
                 rd=["g_rb", "identf"], wr=[("ps", pt)])
            T.op("act", lambda: nc.scalar.copy(out=g["gcol"][:, tt, :], in_=ps[pt][:, 0:16]),
                 rd=[("ps", pt)], wr=["g_gcol"])
        T.op("dve", lambda: nc.vector.tensor_scalar(out=g["ngcol"], in0=g["gcol"], scalar1=-1.0, scalar2=None,
                                                    op0=ALU.mult), rd=["g_gcol"], wr=["g_ngcol"])
        T.op("act", lambda: nc.scalar.activation(out=g["egc"], in_=g["gcol"][:, :, 0:8], func=AF.Exp),
             rd=["g_gcol"], wr=["g_egc"])
        T.op("dve", lambda: nc.vector.tensor_tensor(out=g["bgc"], in0=g["egc"], in1=g["gcol"][:, :, 8:16], op=ALU.mult),
             rd=["g_egc", "g_gcol"], wr=["g_bgc"])

        def conv_silu(cj, pbank, dst, dkey):
            xc, acc, cw, halo = g["xc"], g["acc"], g["cw"], g["halo"]
            T.op("act", lambda: nc.scalar.copy(out=xc[:, 3:515], in_=ps[pbank][:]), rd=[("ps", pbank)], wr=["g_xc"])
            if b == 0:
                T.op("dve", lambda: nc.vector.memset(xc[:, 0:3], 0.0), wr=["g_xc"])
            else:
                T.op("dve", lambda: nc.vector.tensor_copy(out=xc[:, 0:3], in_=halo[:, cj, :]), rd=[("g_halo", cj)],
                     wr=["g_xc"])
            T.op("dve", lambda: nc.vector.tensor_copy(out=halo[:, cj, :], in_=xc[:, 512:515]), rd=["g_xc"],
                 wr=[("g_halo", cj)])
            T.op("act", lambda: nc.scalar.activation(out=acc, in_=xc[:, 0:512], func=AF.Copy, scale=cw[:, cj, 0:1]),
                 rd=["g_xc", "g_cw"], wr=["g_acc"])
            for k in range(1, 4):
                T.op("dve", lambda: nc.vector.scalar_tensor_tensor(out=acc, in0=xc[:, k:k + 512], scalar=cw[:, cj, k:k + 1],
                                                                   in1=acc, op0=ALU.mult, op1=ALU.add),
                     rd=["g_xc", "g_cw", "g_acc"], wr=["g_acc"])
            T.op("act", lambda: nc.scalar.activation(out=dst, in_=acc, func=AF.Silu), rd=["g_acc"], wr=[dkey])

        def l2_rstd(src, skey):
            T.op("act", lambda: nc.scalar.activation(out=sq[:, 0, :], in_=src, func=AF.Square), rd=[skey], wr=[("sq", 0)])
            T.op("pe", lambda: nc.tensor.matmul(ps[7][:], lhsT=onesb, rhs=sq[:, 0, :], start=True, stop=True),
                 rd=["cb", ("sq", 0)], wr=[("ps", 7)])
            T.op("act", lambda: nc.scalar.activation(out=g["rr"], in_=ps[7][:], func=AF.Sqrt, bias=epsb[:]),
                 rd=[("ps", 7), "epsb"], wr=["rden"])
            T.op("dve", lambda: nc.vector.reciprocal(out=g["rr"], in_=g["rr"]), rd=["rden"], wr=["rden"])

        for h in range(8):
            s8 = g["sel8"][:, h * 128:(h + 1) * 128]
            pE2 = nextbank()
            T.op("pe", lambda: nc.tensor.matmul(ps[pE2][:], lhsT=s8, rhs=g["G"], start=True, stop=True),
                 rd=["g_sel8", "g_G"], wr=[("ps", pE2)])
            T.op("act", lambda: nc.scalar.copy(out=g["GB"], in_=ps[pE2][:]), rd=[("ps", pE2)], wr=["g_GB"])
            T.op("act", lambda: nc.scalar.activation(out=g["EG"], in_=ps[pE2][:], func=AF.Exp), rd=[("ps", pE2)], wr=["g_EG"])
            pB = nextbank()
            T.op("pe", lambda: nc.tensor.matmul(ps[pB][:], lhsT=s8, rhs=g["rb"], start=True, stop=True),
                 rd=["g_sel8", "g_rb"], wr=[("ps", pB)])
            T.op("act", lambda: nc.scalar.copy(out=g["Bb"], in_=ps[pB][:]), rd=[("ps", pB)], wr=["g_Bb"])
            T.op("pe", lambda: nc.tensor.matmul(ps[pE2][:], lhsT=identb, rhs=M2, start=False, stop=True),
                 rd=["cb"], wr=[("ps", pE2)])
            for n in range(4):
                cs = slice(n * 128, (n + 1) * 128)
                T.op("act", lambda: nc.scalar.activation(out=g["E2"][:, cs], in_=ps[pE2][:, cs], func=AF.Exp,
                                                         bias=g["ngcol"][:, n, h:h + 1]),
                     rd=[("ps", pE2), "g_ngcol"], wr=["g_E2"])
            for n in range(4):
                cs = slice(n * 128, (n + 1) * 128)
                T.op("pool", lambda: nc.gpsimd.tensor_tensor(out=g["E3"][:, cs], in0=g["E2"][:, cs], in1=identb, op=ALU.add),
                     rd=["g_E2", "cb"], wr=["g_E3"])
            pe_ = nextbank()
            T.op("pe", lambda: nc.tensor.matmul(ps[pe_][:], lhsT=s8, rhs=g["NG"], start=True, stop=False),
                 rd=["g_sel8", "g_NG"], wr=[("ps", pe_)], inc=False)
            T.op("pe", lambda: nc.tensor.matmul(ps[pe_][:], lhsT=identb, rhs=M1, start=False, stop=True),
                 rd=["cb"], wr=[("ps", pe_)])
            for n in range(4):
                cs = slice(n * 128, (n + 1) * 128)
                T.op("act", lambda: nc.scalar.activation(out=g["E1"][:, cs], in_=ps[pe_][:, cs], func=AF.Exp,
                                                         bias=g["gcol"][:, n, h:h + 1]),
                     rd=[("ps", pe_), "g_gcol"], wr=["g_E1"])
            for n in range(4):
                T.op("act", lambda: nc.scalar.activation(out=g["kgc"][:, n:n + 1], in_=g["gcol"][:, n, h:h + 1], func=AF.Exp,
                                                         scale=-1.0, bias=g["GB"][:, n * 128 + 127:n * 128 + 128]),
                     rd=["g_gcol", "g_GB"], wr=["g_kgc"])
                T.op("act", lambda: nc.scalar.activation(out=g["glc"][:, n:n + 1],
                                                         in_=g["GB"][:, n * 128 + 127:n * 128 + 128], func=AF.Exp),
                     rd=["g_GB"], wr=["g_glc"])
            slot = w_get([("gdn_w_in", jg, 0, 8, h * 128, 128), ("gdn_w_in", jg, 0, 8, 1024 + h * 128, 128)])
            slot2 = w_get([("gdn_w_in", jg, 0, 8, 2048 + h * 128, 128), ("gdn_w_in", jg, 0, 8, 3072 + h * 128, 128)])
            banks = []
            for (sl_, piece) in ((slot, 0), (slot, 1), (slot2, 0), (slot2, 1)):
                wv4 = WT(sl_).rearrange("p (g k n) -> p g k n", g=2, k=8)
                pbank = nextbank()
                for kc in range(8):
                    T.op("pe", lambda: nc.tensor.matmul(ps[pbank][:], lhsT=wv4[:, piece, kc, :], rhs=uT[:, kc, :],
                                                        start=(kc == 0), stop=(kc == 7)),
                         rd=[WK(sl_), ("uT", kc)], wr=[("ps", pbank)], inc=(kc == 7))
                banks.append(pbank)
            SA = dict(xc=g["xc"], xk="g_xc", acc=g["acc"], ak="g_acc", rr=g["rr"], rk="rden", sqs=0, pb=7, cj=h, bank=banks[0])
            SB_ = dict(xc=g["xc2"], xk="g_xc2", acc=tmpn[:, 0, :], ak=("tmpn", 0), rr=rstd[:, :], rk="rstd", sqs=1, pb=6,
                       cj=8 + h, bank=banks[1])
            cw, halo = g["cw"], g["halo"]

            def st_load(S_):
                T.op("act", lambda: nc.scalar.copy(out=S_["xc"][:, 3:515], in_=ps[S_["bank"]][:]), rd=[("ps", S_["bank"])],
                     wr=[S_["xk"]])
                if b == 0:
                    T.op("dve", lambda: nc.vector.memset(S_["xc"][:, 0:3], 0.0), wr=[S_["xk"]])
                else:
                    T.op("dve", lambda: nc.vector.tensor_copy(out=S_["xc"][:, 0:3], in_=halo[:, S_["cj"], :]),
                         rd=[("g_halo", S_["cj"])], wr=[S_["xk"]])
                T.op("dve", lambda: nc.vector.tensor_copy(out=halo[:, S_["cj"], :], in_=S_["xc"][:, 512:515]), rd=[S_["xk"]],
                     wr=[("g_halo", S_["cj"])])

            def st_tap0(S_):
                T.op("act", lambda: nc.scalar.activation(out=S_["acc"], in_=S_["xc"][:, 0:512], func=AF.Copy,
                                                         scale=cw[:, S_["cj"], 0:1]),
                     rd=[S_["xk"], "g_cw"], wr=[S_["ak"]])

            def st_taps(S_):
                for k in range(1, 4):
                    T.op("dve", lambda: nc.vector.scalar_tensor_tensor(out=S_["acc"], in0=S_["xc"][:, k:k + 512],
                                                                       scalar=cw[:, S_["cj"], k:k + 1], in1=S_["acc"],
                                                                       op0=ALU.mult, op1=ALU.add),
                         rd=[S_["xk"], "g_cw", S_["ak"]], wr=[S_["ak"]])

            def st_silu(S_, dst=None, dkey=None):
                d_ = S_["acc"] if dst is None else dst
                k_ = S_["ak"] if dkey is None else dkey
                T.op("act", lambda: nc.scalar.activation(out=d_, in_=S_["acc"], func=AF.Silu), rd=[S_["ak"]], wr=[k_])

            def st_sq(S_):
                T.op("act", lambda: nc.scalar.activation(out=sq[:, S_["sqs"], :], in_=S_["acc"], func=AF.Square),
                     rd=[S_["ak"]], wr=[("sq", S_["sqs"])])
                T.op("pe", lambda: nc.tensor.matmul(ps[S_["pb"]][:], lhsT=onesb, rhs=sq[:, S_["sqs"], :], start=True, stop=True),
                     rd=["cb", ("sq", S_["sqs"])], wr=[("ps", S_["pb"])])

            def st_sqrt(S_):
                T.op("act", lambda: nc.scalar.activation(out=S_["rr"], in_=ps[S_["pb"]][:], func=AF.Sqrt, bias=epsb[:]),
                     rd=[("ps", S_["pb"]), "epsb"], wr=[S_["rk"]])

            def st_recip(S_):
                T.op("dve", lambda: nc.vector.reciprocal(out=S_["rr"], in_=S_["rr"]), rd=[S_["rk"]], wr=[S_["rk"]])

            for fn_ in (st_load, st_tap0, st_taps, st_silu, st_sq, st_sqrt, st_recip):
                fn_(SA)
                fn_(SB_)
            T.op("dve", lambda: nc.vector.scalar_tensor_tensor(out=g["qnT"], in0=SA["acc"], scalar=128.0 ** -0.5, in1=SA["rr"],
                                                               op0=ALU.mult, op1=ALU.mult),
                 rd=[SA["ak"], SA["rk"]], wr=["g_qnT"])
            T.op("dve", lambda: nc.vector.tensor_tensor(out=g["knf"], in0=SB_["acc"], in1=SB_["rr"], op=ALU.mult),
                 rd=[SB_["ak"], SB_["rk"]], wr=["g_knf"])
            T.op("pool", lambda: nc.gpsimd.tensor_tensor(out=g["qgT"], in0=g["qnT"], in1=g["EG"], op=ALU.mult),
                 rd=["g_qnT", "g_EG"], wr=["g_qgT"])
            T.op("act", lambda: nc.scalar.copy(out=g["knT"], in_=g["knf"]), rd=["g_knf"], wr=["g_knT"])
            T.op("pool", lambda: nc.gpsimd.tensor_tensor(out=g["kbT"], in0=g["knf"], in1=g["Bb"], op=ALU.mult),
                 rd=["g_knf", "g_Bb"], wr=["g_kbT"])
            SV = dict(SA)
            SV["cj"] = 16 + h
            SV["bank"] = banks[2]
            st_load(SV)
            st_tap0(SV)
            st_taps(SV)
            st_silu(SV, g["vsf"], "g_vsf")
            T.op("act", lambda: nc.scalar.activation(out=g["zs"], in_=ps[banks[3]][:], func=AF.Silu),
                 rd=[("ps", banks[3])], wr=["g_zs"])
            pL, pLT = nextbank(), nextbank()
            for n in range(4):
                cs = slice(n * 128, (n + 1) * 128)
                T.op("pe", lambda: nc.tensor.matmul(ps[pL][:, cs], lhsT=g["kbT"][:, cs], rhs=g["knT"][:, cs],
                                                    start=True, stop=True),
                     rd=["g_kbT", "g_knT"], wr=[("ps", pL)], inc=(n == 3))
            for n in range(4):
                cs = slice(n * 128, (n + 1) * 128)
                T.op("pe", lambda: nc.tensor.matmul(ps[pLT][:, cs], lhsT=g["knT"][:, cs], rhs=g["kbT"][:, cs],
                                                    start=True, stop=True),
                     rd=["g_kbT", "g_knT"], wr=[("ps", pLT)], inc=(n == 3))
            T.op("dve", lambda: nc.vector.tensor_tensor(out=g["Ar"][:, 0, :], in0=ps[pL][:], in1=g["E1"], op=ALU.mult),
                 rd=[("ps", pL), "g_E1"], wr=[("g_A", 0)])
            T.op("dve", lambda: nc.vector.tensor_tensor(out=g["ATr"][:, 0, :], in0=ps[pLT][:], in1=g["E2"], op=ALU.mult),
                 rd=[("ps", pLT), "g_E2"], wr=[("g_AT", 0)])
            for n in range(4):
                cs = slice(n * 128, (n + 1) * 128)
                T.op("dve", lambda: nc.vector.tensor_tensor(out=g["Xr"][:, cs], in0=identf[:], in1=g["AT"][:, 0, cs],
                                                            op=ALU.subtract),
                     rd=["identf", ("g_AT", 0)], wr=["g_X"])
            for k in range(1, 7):
                cur, prv = k % 2, (k - 1) % 2
                p1 = nextbank()
                for n in range(4):
                    cs = slice(n * 128, (n + 1) * 128)
                    T.op("pe", lambda: nc.tensor.matmul(ps[p1][:, cs], lhsT=g["ATr"][:, prv, cs], rhs=g["Ar"][:, prv, cs],
                                                        start=True, stop=True),
                         rd=[("g_A", prv), ("g_AT", prv)], wr=[("ps", p1)], inc=(n == 3))
                if k < 6:
                    p2 = nextbank()
                    for n in range(4):
                        cs = slice(n * 128, (n + 1) * 128)
                        T.op("pe", lambda: nc.tensor.matmul(ps[p2][:, cs], lhsT=g["Ar"][:, prv, cs], rhs=g["ATr"][:, prv, cs],
                                                            start=True, stop=True),
                             rd=[("g_A", prv), ("g_AT", prv)], wr=[("ps", p2)], inc=(n == 3))
                T.op("act", lambda: nc.scalar.copy(out=g["Ar"][:, cur, :], in_=ps[p1][:]), rd=[("ps", p1)], wr=[("g_A", cur)])
                if k < 6:
                    T.op("dve", lambda: nc.vector.tensor_copy(out=g["ATr"][:, cur, :], in_=ps[p2][:]), rd=[("ps", p2)],
                         wr=[("g_AT", cur)])
                p3 = nextbank()
                for n in range(4):
                    cs = slice(n * 128, (n + 1) * 128)
                    T.op("pe", lambda: nc.tensor.matmul(ps[p3][:, cs], lhsT=g["Ar"][:, cur, cs], rhs=g["Xr"][:, cs],
                                                        start=True, stop=True),
                         rd=[("g_A", cur), "g_X"], wr=[("ps", p3)], inc=(n == 3))
                T.op("dve", lambda: nc.vector.tensor_tensor(out=g["Xr"], in0=g["X"], in1=ps[p3][:], op=ALU.add),
                     rd=["g_X", ("ps", p3)], wr=["g_X"])
            pk_, pv_ = nextbank(), nextbank()
            for n in range(4):
                cs = slice(n * 128, (n + 1) * 128)
                T.op("pe", lambda: nc.tensor.transpose(out=ps[pk_][:, cs], in_=g["knf"][:, cs], identity=identf[:]),
                     rd=["g_knf", "identf"], wr=[("ps", pk_)], inc=(n == 3))
            for n in range(4):
                cs = slice(n * 128, (n + 1) * 128)
                T.op("pe", lambda: nc.tensor.transpose(out=ps[pv_][:, cs], in_=g["vsf"][:, cs], identity=identf[:]),
                     rd=["g_vsf", "identf"], wr=[("ps", pv_)], inc=(n == 3))
            for n in range(4):
                cs = slice(n * 128, (n + 1) * 128)
                T.op("dve", lambda: nc.vector.tensor_scalar(out=g["Rwr"][:, cs], in0=ps[pk_][:, cs], scalar1=g["bgc"][:, n, h:h + 1],
                                                            scalar2=None, op0=ALU.mult),
                     rd=[("ps", pk_), "g_bgc"], wr=["g_Rw"])
                T.op("dve", lambda: nc.vector.tensor_scalar(out=g["kg"][:, cs], in0=ps[pk_][:, cs], scalar1=g["kgc"][:, n:n + 1],
                                                            scalar2=None, op0=ALU.mult),
                     rd=[("ps", pk_), "g_kgc"], wr=["g_kg"])
                T.op("act", lambda: nc.scalar.activation(out=g["Rur"][:, cs], in_=ps[pv_][:, cs], func=AF.Copy,
                                                         scale=g["gcol"][:, n, 8 + h:9 + h]),
                     rd=[("ps", pv_), "g_gcol"], wr=["g_Ru"])
            pu, pw = nextbank(), nextbank()
            for n in range(4):
                cs = slice(n * 128, (n + 1) * 128)
                T.op("pe", lambda: nc.tensor.matmul(ps[pu][:, cs], lhsT=g["Xr"][:, cs], rhs=g["Rur"][:, cs], start=True, stop=True),
                     rd=["g_X", "g_Ru"], wr=[("ps", pu)], inc=(n == 3))
            for n in range(4):
                cs = slice(n * 128, (n + 1) * 128)
                T.op("pe", lambda: nc.tensor.matmul(ps[pw][:, cs], lhsT=g["Rwr"][:, cs], rhs=g["Xr"][:, cs], start=True, stop=True),
                     rd=["g_X", "g_Rw"], wr=[("ps", pw)], inc=(n == 3))
            T.op("act", lambda: nc.scalar.copy(out=g["u"], in_=ps[pu][:]), rd=[("ps", pu)], wr=["g_u"])
            T.op("dve", lambda: nc.vector.tensor_copy(out=g["wT"], in_=ps[pw][:]), rd=[("ps", pw)], wr=["g_wT"])
            pp = nextbank()
            for n in range(4):
                cs = slice(n * 128, (n + 1) * 128)
                T.op("pe", lambda: nc.tensor.matmul(ps[pp][:, cs], lhsT=g["knT"][:, cs], rhs=g["qnT"][:, cs], start=True, stop=True),
                     rd=["g_knT", "g_qnT"], wr=[("ps", pp)], inc=(n == 3))
            T.op("dve", lambda: nc.vector.tensor_tensor(out=g["pTg"], in0=ps[pp][:], in1=g["E3"], op=ALU.mult),
                 rd=[("ps", pp), "g_E3"], wr=["g_pTg"])
            pO = 5
            T.op("act", lambda: nc.scalar.copy(out=g["Sb"], in_=g["S"][:, h, :]), rd=[("g_S", h)], wr=["g_Sb"])
            for n in range(4):
                cs = slice(n * 128, (n + 1) * 128)
                v2 = n % 2
                pvn = nextbank()
                T.op("pe", lambda: nc.tensor.matmul(ps[pvn][:, 0:128], lhsT=g["wT"][:, cs], rhs=g["Sb"],
                                                    start=True, stop=True),
                     rd=["g_wT", "g_Sb"], wr=[("ps", pvn)])
                T.op("dve", lambda: nc.vector.tensor_tensor(out=g["vnb"][:, v2, :], in0=g["u"][:, cs], in1=ps[pvn][:, 0:128],
                                                            op=ALU.subtract),
                     rd=["g_u", ("ps", pvn)], wr=[("g_vnb", v2)])
                T.op("pe", lambda: nc.tensor.matmul(ps[pO][:, cs], lhsT=g["Sb"], rhs=g["qgT"][:, cs],
                                                    start=True, stop=False),
                     rd=["g_Sb", "g_qgT"], wr=[("ps", pO)], inc=False)
                T.op("pe", lambda: nc.tensor.matmul(ps[pO][:, cs], lhsT=g["vnb"][:, v2, :], rhs=g["pTg"][:, cs],
                                                    start=False, stop=True),
                     rd=[("g_vnb", v2), "g_pTg"], wr=[("ps", pO)])
                pst = nextbank()
                T.op("pe", lambda: nc.tensor.matmul(ps[pst][:, 0:128], lhsT=g["kg"][:, cs], rhs=g["vnb"][:, v2, :],
                                                    start=True, stop=True),
                     rd=["g_kg", ("g_vnb", v2)], wr=[("ps", pst)])
                T.op("dve", lambda: nc.vector.scalar_tensor_tensor(out=g["Sb"], in0=g["S"][:, h, :],
                                                                   scalar=g["glc"][:, n:n + 1], in1=ps[pst][:, 0:128],
                                                                   op0=ALU.mult, op1=ALU.add),
                     rd=[("g_S", h), "g_glc", ("ps", pst)], wr=["g_Sb"])
                T.op("dve", lambda: nc.vector.scalar_tensor_tensor(out=g["S"][:, h, :], in0=g["S"][:, h, :],
                                                                   scalar=g["glc"][:, n:n + 1], in1=ps[pst][:, 0:128],
                                                                   op0=ALU.mult, op1=ALU.add),
                     rd=[("g_S", h), "g_glc", ("ps", pst)], wr=[("g_S", h)])
            T.op("act", lambda: nc.scalar.activation(out=sq[:, 0, :], in_=ps[pO][:], func=AF.Square), rd=[("ps", pO)],
                 wr=[("sq", 0)])
            T.op("pe", lambda: nc.tensor.matmul(ps[7][:], lhsT=onesb, rhs=sq[:, 0, :], start=True, stop=True),
                 rd=["cb", ("sq", 0)], wr=[("ps", 7)])
            T.op("act", lambda: nc.scalar.activation(out=g["rr"], in_=ps[7][:], func=AF.Sqrt, scale=1.0 / 128, bias=epsb[:]),
                 rd=[("ps", 7), "epsb"], wr=["rden"])
            T.op("dve", lambda: nc.vector.reciprocal(out=g["rr"], in_=g["rr"]), rd=["rden"], wr=["rden"])
            T.op("dve", lambda: nc.vector.scalar_tensor_tensor(out=g["acc"], in0=ps[pO][:], scalar=g["gain"], in1=g["rr"],
                                                               op0=ALU.mult, op1=ALU.mult),
                 rd=[("ps", pO), "g_gain", "rden"], wr=["g_acc"])
            T.op("dve", lambda: nc.vector.tensor_tensor(out=catT[:, h, :], in0=g["acc"], in1=g["zs"], op=ALU.mult),
                 rd=["g_acc", "g_zs"], wr=[("catT", h)])
        proj_fm("gdn_w_in", jg, 4112, 4, uT, "uT", TB, qmem_handler)

    def emit():
        T.dma("sp", c_io, identf[:], identf_d, wr=["identf"])
        T.dma("sp", c_io, gains[:], gains_d, wr=["gains"])
        T.dma("sp", c_io, cb[:], cb_d, wr=["cb"])
        T.op("pool", lambda: nc.gpsimd.memset(epsb[:], EPS), wr=["epsb"])
        T.op("pool", lambda: nc.gpsimd.memset(oneb[:], 1.0), wr=["oneb"])
        prep_mem()
        for t in range(16):
            sl = t % 2
            T.dma("sp", c_io, xin(sl), x_d[t * 128:(t + 1) * 128, :], wr=xink(sl))
            for half in range(2):
                pbn = nextbank()
                pk = ("ps", pbn)
                for q in range(4):
                    c = half * 4 + q
                    T.op("pe", lambda: nc.tensor.transpose(out=ps[pbn][:, q * 128:(q + 1) * 128],
                                                           in_=xin(sl)[:, c * 128:(c + 1) * 128],
                                                           identity=identf[:]),
                         rd=xink(sl) + ["identf"], wr=[pk], inc=(q == 3))
                dst = hT[:, half * 4:(half + 1) * 4, t * 128:(t + 1) * 128]
                src = ps[pbn][:].rearrange("p (q n) -> p q n", q=4)
                wr = [("hT", c, t // 4) for c in range(half * 4, half * 4 + 4)]
                if half == 0:
                    T.op("act", lambda: nc.scalar.copy(out=dst, in_=src), rd=[pk], wr=wr)
                else:
                    T.op("dve", lambda: nc.vector.tensor_copy(out=dst, in_=src), rd=[pk], wr=wr)
        for l in layers:
            kind = l % 3
            T.barrier()
            layer_alloc(kind)
            w_block(l, None)
            mem_kv(l)
            for b in range(NB):
                w_block(l, b)
                pre_norm(l, 0, b)
                if kind == 2:
                    if b == 0:
                        sb_setup()
                    sb_inproj(l, b)
                    sb_attn(b)
                elif kind == 1:
                    if b == 0:
                        ml_setup()
                    ml_inproj(l, b)
                    ml_attn(l, b)
                else:
                    if b == 0:
                        gdn_setup(l)
                    gdn_block(l, b)
                mem_attn(b)
                out_proj(l, b)
                ffn(l, b)
            T.barrier()
            layer_free()
        for t in range(16):
            sl = t % 2
            for half in range(2):
                pbn = nextbank()
                pk = ("ps", pbn)
                for q in range(4):
                    c = half * 4 + q
                    T.op("pe", lambda: nc.tensor.transpose(out=ps[pbn][:, q * 128:(q + 1) * 128],
                                                           in_=hT[:, c, t * 128:(t + 1) * 128],
                                                           identity=identf[:]),
                         rd=[("hT", c, t // 4), "identf"], wr=[pk], inc=(q == 3))
                dst = xin(sl)[:, half * 512:(half + 1) * 512]
                if half == 0:
                    T.op("act", lambda: nc.scalar.copy(out=dst, in_=ps[pbn][:]), rd=[pk], wr=[("ysb", 2 * sl + half)])
                else:
                    T.op("dve", lambda: nc.vector.tensor_copy(out=dst, in_=ps[pbn][:]), rd=[pk],
                         wr=[("ysb", 2 * sl + half)])
            T.dma("sp", c_io, out_d[t * 128:(t + 1) * 128, :], xin(sl), rd=xink(sl), wr=[("out", t)])
        T.wait_key("sp", [("out", t) for t in range(16)])

    T.dry = True
    emit()
    T.dry = False
    ring["i"] = 0
    wstate["nf32"] = 0
    wstate["nb16"] = 0
    wstate["slot_of"] = {}
    assert max(x[2] for x in wplan) < NWC
    emit()
    P.finish()
    return nc, T


CB_ONES = 0
CB_NEGONES = 128
CB_IDENT = 256
CB_NEGMINCL = 384
CB_MASK01 = 512
NCB = CB_MASK01 + 4 * TB
G_MEM = DEPTH * 4 * 8
NG = G_MEM + 8


def host_consts(inputs):
    g = np.stack([inputs["norm_pre_mix"], inputs["norm_post_mix"], inputs["norm_pre_ffn"],
                  inputs["norm_post_ffn"]], axis=1)
    g = g.reshape(DEPTH, 4, 8, 128).transpose(3, 0, 1, 2).reshape(128, DEPTH * 4 * 8)
    gm = inputs["mem_norm"].reshape(8, 128).T
    gains = np.concatenate([g, gm], axis=1).astype(np.float32)
    cbf = np.zeros((128, NCB), np.float32)
    cbf[:, CB_ONES:CB_ONES + 128] = 1.0
    cbf[:, CB_NEGONES:CB_NEGONES + 128] = -1.0
    cbf[:, CB_IDENT:CB_IDENT + 128] = np.eye(128)
    jj, ss = np.meshgrid(np.arange(128), np.arange(128), indexing="ij")
    cbf[:, CB_NEGMINCL:CB_NEGMINCL + 128] = -(jj >= ss).astype(np.float32)
    sidx = np.arange(128)[:, None]
    tidx = np.arange(TB)[None, :]
    for i in range(4):
        m = ((sidx + 128 * i) < tidx).astype(np.float32)
        cbf[:, CB_MASK01 + i * TB:CB_MASK01 + (i + 1) * TB] = m
    sel = np.zeros((16, 8, 128), np.float32)
    for h in range(8):
        sel[8 + h, h, :] = 1.0
    m01i = np.zeros((128, 4 * TB), np.float32)
    for i in range(4):
        m01i[:, i * TB:(i + 1) * TB] = ((sidx + 128 * i) <= tidx)
    mlbias = np.concatenate([inputs["ml_i_bias"][0], inputs["ml_f_bias"][0]]).reshape(16, 1).astype(np.float32)
    sel8 = np.zeros((8, 8, 128), np.float32)
    for h in range(8):
        sel8[h, h, :] = 1.0
    pidx = np.arange(128)[:, None]
    fidx = np.arange(128)[None, :]
    gm = np.concatenate([np.tile(np.where(fidx < pidx, 0.0, NEGBIG), (1, 4)),
                         np.tile(np.where(pidx < fidx, 0.0, NEGBIG), (1, 4)),
                         np.tile(np.where(pidx <= fidx, 0.0, NEGBIG), (1, 4))], axis=1).astype(np.float32)
    gcw = inputs["gdn_conv"].reshape(2, 4, 24, 128).transpose(0, 3, 2, 1).reshape(2, 128, 96)
    return {
        "sel8": sel8.reshape(8, 8 * 128),
        "gdn_masks": gm.astype(ml_dtypes.bfloat16),
        "gdn_cw": np.ascontiguousarray(gcw.astype(np.float32)),
        "gdn_dtb": np.ascontiguousarray(inputs["gdn_dt_bias"].reshape(2, 8, 1).astype(np.float32)),
        "gdn_alog": np.ascontiguousarray(inputs["gdn_a_log"].reshape(2, 8, 1).astype(np.float32)),
        "gdn_gain": np.ascontiguousarray(inputs["gdn_out_norm"].reshape(2, 128, 1).astype(np.float32)),
        "selc": sel.reshape(16, 8 * 128),
        "mlbias": mlbias,
        "mlgain": np.ascontiguousarray(inputs["ml_out_norm"][0].reshape(128, 1).astype(np.float32)),
        "mask01incl": m01i.astype(ml_dtypes.bfloat16),
        "identf": np.eye(128, dtype=np.float32),
        "gains": np.ascontiguousarray(gains),
        "cbf": cbf.astype(ml_dtypes.bfloat16),
    }


WNAMES = ["w_ffn_in", "w_ffn_out", "w_mem_kv", "w_out", "gdn_w_in", "ml_w_in", "sb_w_in"]


def make_in_maps(inputs, cores, x_override=None):
    consts = host_consts(inputs)
    in_maps = []
    for core in cores:
        m = dict(consts)
        m["x"] = np.ascontiguousarray(inputs["x"][core] if x_override is None else x_override[core])
        m["mem"] = np.ascontiguousarray(inputs["mem"][core])
        for w in WNAMES:
            m[w] = inputs[w]
        in_maps.append(m)
    return in_maps


def kernel(**inputs):
    inputs = {k: np.asarray(v) for k, v in inputs.items()}
    nc, _ = build(list(range(DEPTH)))
    in_maps = make_in_maps(inputs, list(range(8)))
    res = run_bass_kernel_spmd(nc, in_maps, core_ids=list(range(8)))
    return np.stack([r["out"] for r in res.results], axis=0)
```
